# Optimizing a Trainium2 kernel written in Bass

```python
import jax, jax.numpy as jnp
from jax import lax
import numpy as np

D_MODEL = 1024
BATCH = 8
SEQ = 2048
DEPTH = 4

CHUNK = 64
N_BRANCH = 4
BRANCH_WIDTH = 512
NORM_EPS = 1e-6

GMLP_BLOCK = 128
GMLP_HEADS = 4
GMLP_WIDTH = BRANCH_WIDTH
GMLP_HEAD_DIM = GMLP_WIDTH // GMLP_HEADS

LRU_WIDTH = BRANCH_WIDTH
LRU_HEADS = 4
LRU_HEAD_DIM = LRU_WIDTH // LRU_HEADS
LRU_CONV = 4
LRU_C = 8.0

MLA_HEADS = 8
MLA_Q_RANK = 384
MLA_KV_RANK = 256
MLA_NOPE = 64
MLA_ROPE = 32
MLA_V = 64
MLA_WIDTH = MLA_HEADS * MLA_V
ROPE_THETA = 10000.0
Q_BLOCK = 128

GLA_HEADS = 4
GLA_DK = 64
GLA_DV = 128
GLA_KW = GLA_HEADS * GLA_DK
GLA_VW = GLA_HEADS * GLA_DV
GLA_GATE_RANK = 16
GLA_TAU = 16.0

FFN_DIM = 2816
FFN_CONV = 3

IN_SIZES = (2 * GMLP_WIDTH, LRU_WIDTH, LRU_WIDTH, MLA_Q_RANK, MLA_KV_RANK, MLA_ROPE,
            GLA_KW, GLA_KW, GLA_VW, GLA_GATE_RANK, GLA_VW, N_BRANCH * D_MODEL)
IN_COLS = (2 * GMLP_WIDTH + 2 * LRU_WIDTH + MLA_Q_RANK + MLA_KV_RANK + MLA_ROPE
           + 2 * GLA_KW + 2 * GLA_VW + GLA_GATE_RANK + N_BRANCH * D_MODEL)

kernel_name = "hybrid_chunk_causal_gated_parallel_trunk"


def rms_norm(x, gain):
    xf = x.astype(jnp.float32)
    y = xf * lax.rsqrt(jnp.mean(xf * xf, axis=-1, keepdims=True) + NORM_EPS)
    return (y * gain.astype(jnp.float32)).astype(x.dtype)


def causal_dwconv(x, w, b):
    k = w.shape[0]
    y = lax.conv_general_dilated(x, w[:, None, :].astype(x.dtype), window_strides=(1,),
                                 padding=[(k - 1, 0)],
                                 dimension_numbers=('NWC', 'WIO', 'NWC'),
                                 feature_group_count=x.shape[-1])
    return y + b.astype(x.dtype)


def gmlp_mixer(z, v_norm, w_s, b_s):
    z = jax.nn.gelu(z)
    u, v = jnp.split(z, 2, axis=-1)
    v = rms_norm(v, v_norm)
    bsz, seq, _ = v.shape
    v = v.reshape(bsz, seq // GMLP_BLOCK, GMLP_BLOCK, GMLP_HEADS, GMLP_HEAD_DIM)
    pos_chunk = jnp.arange(GMLP_BLOCK) // CHUNK
    mask = pos_chunk[None, :] <= pos_chunk[:, None]
    w = jnp.where(mask[None], w_s, 0).astype(v.dtype)
    sv = jnp.einsum('hts,bnshc->bnthc', w, v) + b_s.T.astype(v.dtype)[None, None, :, :, None]
    return u * sv.reshape(bsz, seq, GMLP_WIDTH)


def rglru_mixer(y, u, conv_w, conv_b, w_a, b_a, w_i, b_i, lam):
    u = causal_dwconv(u, conv_w, conv_b)
    bsz, seq, _ = u.shape
    uh = u.reshape(bsz, seq, LRU_HEADS, LRU_HEAD_DIM)
    r = jax.nn.sigmoid(jnp.einsum('bshi,hij->bshj', uh, w_a).reshape(bsz, seq, LRU_WIDTH) + b_a)
    i = jax.nn.sigmoid(jnp.einsum('bshi,hij->bshj', uh, w_i).reshape(bsz, seq, LRU_WIDTH) + b_i)
    log_a = -LRU_C * r.astype(jnp.float32) * jax.nn.softplus(-lam.astype(jnp.float32))
    a = jnp.exp(log_a)
    x_in = jnp.sqrt(-jnp.expm1(2.0 * log_a)) * (i * u).astype(jnp.float32)

    def combine(c1, c2):
        a1, b1 = c1
        a2, b2 = c2
        return a1 * a2, a2 * b1 + b2

    _, h = lax.associative_scan(combine, (a, x_in), axis=1)
    return h.astype(u.dtype) * jax.nn.gelu(y)


def apply_rope(x, cos, sin):
    x1, x2 = jnp.split(x, 2, axis=-1)
    return jnp.concatenate([x1 * cos - x2 * sin, x2 * cos + x1 * sin], axis=-1)


def mla_mixer(c_q, c_kv, k_r, positions, q_norm, w_uq, kv_norm, w_ukv):
    bsz, seq, _ = c_q.shape
    dt = c_q.dtype
    q = (rms_norm(c_q, q_norm) @ w_uq).reshape(bsz, seq, MLA_HEADS, MLA_NOPE + MLA_ROPE)
    q_nope, q_rope = q[..., :MLA_NOPE], q[..., MLA_NOPE:]
    kv = (rms_norm(c_kv, kv_norm) @ w_ukv).reshape(bsz, seq, MLA_HEADS, MLA_NOPE + MLA_V)
    k_nope, v = kv[..., :MLA_NOPE], kv[..., MLA_NOPE:]
    inv_freq = ROPE_THETA ** (-jnp.arange(0, MLA_ROPE, 2, dtype=jnp.float32) / MLA_ROPE)
    ang = positions.astype(jnp.float32)[..., None] * inv_freq
    cos, sin = jnp.cos(ang).astype(dt), jnp.sin(ang).astype(dt)
    q_rope = apply_rope(q_rope, cos[:, :, None, :], sin[:, :, None, :])
    k_rope = apply_rope(k_r, cos, sin)
    scale = (MLA_NOPE + MLA_ROPE) ** -0.5
    outs = []
    for blk in range(seq // Q_BLOCK):
        q0, q1 = blk * Q_BLOCK, (blk + 1) * Q_BLOCK
        s = (jnp.einsum('bqhd,bkhd->bhqk', q_nope[:, q0:q1], k_nope[:, :q1])
             + jnp.einsum('bqhr,bkr->bhqk', q_rope[:, q0:q1], k_rope[:, :q1]))
        s = s.astype(jnp.float32) * scale
        q_chunk = (q0 + jnp.arange(Q_BLOCK)) // CHUNK
        k_chunk = jnp.arange(q1) // CHUNK
        s = jnp.where(k_chunk[None, :] <= q_chunk[:, None], s, -1e30)
        p = jax.nn.softmax(s, axis=-1).astype(v.dtype)
        outs.append(jnp.einsum('bhqk,bkhv->bqhv', p, v[:, :q1]))
    o = jnp.concatenate(outs, axis=1)
    return o.reshape(bsz, seq, MLA_WIDTH)


def gla_mixer(q, k, v, g_lr, og, w_g2, b_g, o_norm):
    dt = v.dtype
    bsz, seq, _ = q.shape
    nc = seq // CHUNK
    log_a = jax.nn.log_sigmoid((g_lr @ w_g2 + b_g).astype(jnp.float32)) / GLA_TAU

    def to_chunks(t, d):
        return t.astype(jnp.float32).reshape(bsz, nc, CHUNK, GLA_HEADS, d).transpose(1, 0, 3, 2, 4)

    qc = to_chunks(q, GLA_DK) * (GLA_DK ** -0.5)
    kc = to_chunks(k, GLA_DK)
    vc = to_chunks(v, GLA_DV)
    bcum = jnp.cumsum(to_chunks(log_a, GLA_DK), axis=3)
    b_last = bcum[:, :, :, -1:, :]
    q_in = qc * jnp.exp(bcum)
    k_in = kc * jnp.exp(-bcum)
    k_st = kc * jnp.exp(b_last - bcum)
    tri = jnp.tril(jnp.ones((CHUNK, CHUNK), dtype=bool))
    att = jnp.where(tri, jnp.einsum('cbhtd,cbhsd->cbhts', q_in, k_in), 0.0)
    o_intra = jnp.einsum('cbhts,cbhsv->cbhtv', att, vc)
    decay = jnp.exp(b_last[:, :, :, 0, :])

    def step(state, inp):
        q_c, k_c, v_c, d_c = inp
        o_c = jnp.einsum('bhtd,bhdv->bhtv', q_c, state)
        state = d_c[..., None] * state + jnp.einsum('bhtd,bhtv->bhdv', k_c, v_c)
        return state, o_c

    s0 = jnp.zeros((bsz, GLA_HEADS, GLA_DK, GLA_DV), jnp.float32)
    _, o_inter = lax.scan(step, s0, (q_in, k_st, vc, decay))
    o = (o_intra + o_inter).transpose(1, 0, 3, 2, 4).reshape(bsz, seq, GLA_HEADS, GLA_DV)
    o = rms_norm(o, o_norm.reshape(GLA_HEADS, GLA_DV)).reshape(bsz, seq, GLA_VW)
    return o.astype(dt) * jax.nn.silu(og)


def conv_ffn(h, w_up, conv_w, conv_b, w_down):
    z = causal_dwconv(h @ w_up, conv_w, conv_b)
    a, b = jnp.split(z, 2, axis=-1)
    return (jax.nn.gelu(a) * b) @ w_down


def setup_inputs(seed: int = 0) -> dict:
    key = jax.random.key(seed)
    ks = jax.random.split(key, 40)
    L = DEPTH

    def nrm(k, shape, scale):
        return jax.random.normal(k, shape, jnp.float32) * scale

    def gain(k, n):
        return 1.0 + 0.05 * jax.random.normal(k, (L, n), jnp.float32)

    x = jax.random.normal(ks[0], (BATCH, SEQ, D_MODEL), jnp.float32)
    offsets = jax.random.randint(ks[1], (BATCH, 1), 0, 4096, dtype=jnp.int32)
    positions = (jnp.arange(SEQ, dtype=jnp.int32)[None, :] + offsets).astype(jnp.int32)
    a0 = jax.random.uniform(ks[2], (L, LRU_WIDTH), jnp.float32, minval=0.9, maxval=0.999)
    s_lam = a0 ** (1.0 / LRU_C)
    lru_lambda = jnp.log(s_lam) - jnp.log1p(-s_lam)
    return {
        "x": x,
        "positions": positions,
        "norm_mix_pre": gain(ks[3], D_MODEL),
        "norm_mix_post": gain(ks[4], D_MODEL),
        "norm_ffn_pre": gain(ks[5], D_MODEL),
        "norm_ffn_post": gain(ks[6], D_MODEL),
        "w_in": nrm(ks[7], (L, D_MODEL, IN_COLS), D_MODEL ** -0.5),
        "gmlp_v_norm": gain(ks[8], GMLP_WIDTH),
        "gmlp_w_s": nrm(ks[9], (L, GMLP_HEADS, GMLP_BLOCK, GMLP_BLOCK), 0.5 * GMLP_BLOCK ** -0.5),
        "gmlp_b_s": 1.0 + nrm(ks[10], (L, GMLP_HEADS, GMLP_BLOCK), 0.1),
        "lru_conv_w": nrm(ks[11], (L, LRU_CONV, LRU_WIDTH), LRU_CONV ** -0.5),
        "lru_conv_b": nrm(ks[12], (L, LRU_WIDTH), 0.01),
        "lru_w_a": nrm(ks[13], (L, LRU_HEADS, LRU_HEAD_DIM, LRU_HEAD_DIM), LRU_HEAD_DIM ** -0.5),
        "lru_b_a": nrm(ks[14], (L, LRU_WIDTH), 0.01),
        "lru_w_i": nrm(ks[15], (L, LRU_HEADS, LRU_HEAD_DIM, LRU_HEAD_DIM), LRU_HEAD_DIM ** -0.5),
        "lru_b_i": nrm(ks[16], (L, LRU_WIDTH), 0.01),
        "lru_lambda": lru_lambda,
        "mla_q_norm": gain(ks[17], MLA_Q_RANK),
        "mla_w_uq": nrm(ks[18], (L, MLA_Q_RANK, MLA_HEADS * (MLA_NOPE + MLA_ROPE)), MLA_Q_RANK ** -0.5),
        "mla_kv_norm": gain(ks[19], MLA_KV_RANK),
        "mla_w_ukv": nrm(ks[20], (L, MLA_KV_RANK, MLA_HEADS * (MLA_NOPE + MLA_V)), MLA_KV_RANK ** -0.5),
        "gla_w_g2": nrm(ks[21], (L, GLA_GATE_RANK, GLA_KW), GLA_GATE_RANK ** -0.5),
        "gla_b_g": nrm(ks[22], (L, GLA_KW), 0.01),
        "gla_o_norm": gain(ks[23], GLA_VW),
        "w_branch": nrm(ks[24], (L, N_BRANCH, BRANCH_WIDTH, D_MODEL), BRANCH_WIDTH ** -0.5),
        "w_out": nrm(ks[25], (L, D_MODEL, D_MODEL), D_MODEL ** -0.5),
        "ffn_w_up": nrm(ks[26], (L, D_MODEL, 2 * FFN_DIM), D_MODEL ** -0.5),
        "ffn_conv_w": nrm(ks[27], (L, FFN_CONV, 2 * FFN_DIM), FFN_CONV ** -0.5),
        "ffn_conv_b": nrm(ks[28], (L, 2 * FFN_DIM), 0.01),
        "ffn_w_down": nrm(ks[29], (L, FFN_DIM, D_MODEL), FFN_DIM ** -0.5),
    }


def reference(x, positions, norm_mix_pre, norm_mix_post, norm_ffn_pre, norm_ffn_post, w_in,
              gmlp_v_norm, gmlp_w_s, gmlp_b_s,
              lru_conv_w, lru_conv_b, lru_w_a, lru_b_a, lru_w_i, lru_b_i, lru_lambda,
              mla_q_norm, mla_w_uq, mla_kv_norm, mla_w_ukv,
              gla_w_g2, gla_b_g, gla_o_norm,
              w_branch, w_out, ffn_w_up, ffn_conv_w, ffn_conv_b, ffn_w_down):
    bsz, seq, _ = x.shape
    split_at = [int(o) for o in np.cumsum(IN_SIZES)[:-1]]
    for l in range(DEPTH):
        h = rms_norm(x, norm_mix_pre[l])
        proj = h @ w_in[l]
        (z_a, y_b, u_b, c_q, c_kv, k_r, q_d, k_d, v_d, g_d, og_d, gate) = jnp.split(proj, split_at, axis=-1)
        o_a = gmlp_mixer(z_a, gmlp_v_norm[l], gmlp_w_s[l], gmlp_b_s[l])
        o_b = rglru_mixer(y_b, u_b, lru_conv_w[l], lru_conv_b[l], lru_w_a[l], lru_b_a[l],
                          lru_w_i[l], lru_b_i[l], lru_lambda[l])
        o_c = mla_mixer(c_q, c_kv, k_r, positions, mla_q_norm[l], mla_w_uq[l],
                        mla_kv_norm[l], mla_w_ukv[l])
        o_d = gla_mixer(q_d, k_d, v_d, g_d, og_d, gla_w_g2[l], gla_b_g[l], gla_o_norm[l])
        o = jnp.stack([o_a, o_b, o_c, o_d], axis=2)
        br = jnp.einsum('bskc,kcd->bskd', o, w_branch[l])
        g = jax.nn.sigmoid(gate).reshape(bsz, seq, N_BRANCH, D_MODEL)
        mixed = jnp.sum(g * br, axis=2) @ w_out[l]
        x = x + rms_norm(mixed, norm_mix_post[l])
        h = rms_norm(x, norm_ffn_pre[l])
        f = conv_ffn(h, ffn_w_up[l], ffn_conv_w[l], ffn_conv_b[l], ffn_w_down[l])
        x = x + rms_norm(f, norm_ffn_post[l])
    return x
```

```python
import numpy as np
from contextlib import ExitStack
import concourse.bass as bass
import concourse.mybir as mybir
from concourse.bass_utils import run_bass_kernel_spmd

F32 = mybir.dt.float32
BF16 = mybir.dt.bfloat16
I32 = mybir.dt.int32
AF = mybir.ActivationFunctionType
ALU = mybir.AluOpType

D = 1024
S = 2048
DEPTH = 4
NCORE = 8
TG = 512
NTG = 4
IN_COLS = 8368
FFN = 2816
EPS = 1e-6

C_ZA = 0
C_YB = 1024
C_UB = 1536
C_CQ = 2048
C_CKV = 2432
C_KR = 2688
C_QD = 2720
C_KD = 2976
C_VD = 3232
C_GD = 3744
C_OG = 3760
C_GATE = 4272

PP = {}
_o = 0
for _n, _w in [("nmp", 8), ("nmo", 8), ("nfp", 8), ("nfo", 8), ("lcw", 16), ("lcb", 4), ("lba", 4), ("lbi", 4),
               ("llam", 4), ("mqn", 3), ("mkn", 2), ("gbg", 2), ("gon", 4), ("fcw", 132), ("fcb", 44)]:
    PP[_n] = _o
    _o += _w
NPP = _o

CS_IDENT = 0
CS_ONES = 128
CS_GMASK = 256
CS_INVF = 384
CS_SIGN = 385
CS_EPS = 386
CS_ONE = 387
CS_HALFPI = 388
CS_ZERO = 389
NCST = 392


def _host_consts():
    c = np.zeros((128, NCST), np.float32)
    c[:, CS_IDENT:CS_IDENT + 128] = np.eye(128, dtype=np.float32)
    c[:, CS_ONES:CS_ONES + 128] = 1.0
    s = np.arange(128)[:, None]
    t = np.arange(128)[None, :]
    c[:, CS_GMASK:CS_GMASK + 128] = ((s // 64 == t // 64) & (s <= t)).astype(np.float32)
    invf = (10000.0 ** (-np.arange(0, 32, 2, dtype=np.float32) / np.float32(32))).astype(np.float32)
    for p in range(64, 96):
        c[p, CS_INVF] = invf[(p - 64) % 16]
        c[p, CS_SIGN] = -1.0 if p < 80 else 1.0
    c[:, CS_EPS] = EPS
    c[:, CS_ONE] = 1.0
    c[:, CS_HALFPI] = np.float32(np.pi / 2)
    return c


def _host_resetmask():
    m = np.ones((128, S), np.float32)
    m[:, 0::64] = 0.0
    return m


def _host_params(inp, l):
    p = np.zeros((128, NPP), np.float32)

    def put(name, arr):
        p[:, PP[name]:PP[name] + arr.shape[1]] = arr
    put("nmp", inp["norm_mix_pre"][l].reshape(8, 128).T)
    put("nmo", inp["norm_mix_post"][l].reshape(8, 128).T)
    put("nfp", inp["norm_ffn_pre"][l].reshape(8, 128).T)
    put("nfo", inp["norm_ffn_post"][l].reshape(8, 128).T)
    put("lcw", inp["lru_conv_w"][l].reshape(4, 4, 128).transpose(2, 1, 0).reshape(128, 16))
    put("lcb", inp["lru_conv_b"][l].reshape(4, 128).T)
    put("lba", inp["lru_b_a"][l].reshape(4, 128).T)
    put("lbi", inp["lru_b_i"][l].reshape(4, 128).T)
    put("llam", inp["lru_lambda"][l].reshape(4, 128).T)
    put("mqn", inp["mla_q_norm"][l].reshape(3, 128).T)
    put("mkn", inp["mla_kv_norm"][l].reshape(2, 128).T)
    put("gbg", inp["gla_b_g"][l].reshape(2, 128).T)
    put("gon", inp["gla_o_norm"][l].reshape(4, 128).T)
    put("fcw", inp["ffn_conv_w"][l].reshape(3, 44, 128).transpose(2, 1, 0).reshape(128, 132))
    put("fcb", inp["ffn_conv_b"][l].reshape(44, 128).T)
    return p


def _host_tile(inp, key):
    kind = key[0]
    if kind == "k":
        _, name, l, sub, row0, pn, kc, cols = key
        w = inp[name][l]
        if sub is not None:
            w = w[sub]
        idx = np.concatenate([np.arange(s, s + n) for (s, n) in cols])
        blk = w[row0:row0 + kc * pn][:, idx]
        t = blk.reshape(kc, pn, len(idx)).transpose(1, 0, 2).reshape(pn, kc * len(idx))
        if pn < 128:
            t = np.concatenate([t, np.zeros((128 - pn, t.shape[1]), np.float32)], 0)
        return np.ascontiguousarray(t, dtype=np.float32)
    if kind == "blk":
        _, name, l = key
        return np.ascontiguousarray(inp[name][l].transpose(1, 0, 2).reshape(128, 512), dtype=np.float32)
    if kind == "blkT":
        _, name, l = key
        return np.ascontiguousarray(inp[name][l].transpose(2, 0, 1).reshape(128, 512), dtype=np.float32)
    raise ValueError(key)


class Buf:
    __slots__ = ("name", "writers", "readers", "excl", "lo", "hi", "live")

    def __init__(self, name="", excl=False, lo=None, hi=None):
        self.name = name
        self.excl = excl
        self.lo = lo
        self.hi = hi
        self.live = lo is None
        self.writers = {}
        self.readers = {}


class DSem:
    __slots__ = ("h", "count", "last")

    def __init__(self, h):
        self.h = h
        self.count = 0
        self.last = None


class Op:
    __slots__ = ("eng", "fn", "deps", "is_dma", "idx", "sem", "tick", "signal", "dsem")


ENGS = ["pe", "act", "dve", "pool", "sp"]
SEM_LIMIT = 12000
SAME_ENG_SYNC = True
import os as _os
USE_BARRIERS = bool(int(_os.environ.get('KERN_BARRIERS', '0')))


class Prog:
    def __init__(self, nc, es):
        self.nc = nc
        self.es = es
        self.ops = []
        self.dry = False
        self.nsem = 0
        self.last_op = {}
        self.dmas_since_bar = []
        self.bar_deps = []
        self.bar_pending = set()
        self.live = []

    def touch(self, b):
        if b.live:
            return
        keep = []
        for o in self.live:
            if o.lo < b.hi and b.lo < o.hi:
                for k, op in o.writers.items():
                    if k not in b.writers or b.writers[k].idx < op.idx:
                        b.writers[k] = op
                for k, op in o.readers.items():
                    if k not in b.readers or b.readers[k].idx < op.idx:
                        b.readers[k] = op
                o.writers = {}
                o.readers = {}
                o.live = False
            else:
                keep.append(o)
        keep.append(b)
        b.live = True
        self.live = keep

    def new_sem(self, name="s"):
        self.nsem += 1
        return self.es.enter_context(self.nc.semaphore(f"{name}{self.nsem}"))

    def new_dsem(self):
        return DSem(self.new_sem("d"))

    def barrier(self, force=False):
        if self.dry or not (force or USE_BARRIERS):
            return
        deps = list(self.last_op.values()) + self.dmas_since_bar
        if self.bar_pending:
            deps += self.bar_deps
        self.bar_deps = deps
        self.bar_pending = set(ENGS)
        self.dmas_since_bar = []

    def add(self, eng, fn, reads=(), writes=(), dma=False, dsem=None):
        if self.dry:
            return
        op = Op()
        op.eng = eng
        op.fn = fn
        op.is_dma = dma
        op.idx = len(self.ops)
        op.signal = False
        op.sem = None
        op.tick = 0
        op.dsem = dsem
        key = ("d", op.idx) if dma else eng
        deps = {}
        for b in reads:
            self.touch(b)
        for b in writes:
            self.touch(b)
        ex = [b for b in reads if b.excl and b not in writes]
        if ex:
            writes = list(writes) + ex
            reads = [b for b in reads if not b.excl]

        def need(o):
            if o.is_dma or dma or o.eng != eng or (SAME_ENG_SYNC and eng != "pe"):
                deps[o.idx] = o
        for b in reads:
            for o in b.writers.values():
                need(o)
        for b in writes:
            for o in b.writers.values():
                need(o)
            for o in b.readers.values():
                need(o)
        if eng in self.bar_pending:
            for o in self.bar_deps:
                need(o)
            self.bar_pending.discard(eng)
        if dma:
            assert dsem is not None
            if dsem.last is not None:
                need(dsem.last)
            dsem.last = op
            dsem.count += 1
            op.sem = dsem.h
            op.tick = 16 * dsem.count
            self.dmas_since_bar.append(op)
        for b in writes:
            b.writers = {key: op}
            b.readers = {}
        for b in reads:
            if b not in writes:
                b.readers[key] = op
        op.deps = list(deps.values())
        if not dma:
            self.last_op[eng] = op
        self.ops.append(op)
        return op

    def emit(self, block):
        for op in self.ops:
            for d in op.deps:
                d.signal = True
        cur = {}
        cnt = {}
        for op in self.ops:
            if op.is_dma or not op.signal:
                continue
            e = op.eng
            if e not in cur or cnt[e] >= SEM_LIMIT:
                cur[e] = self.new_sem(e)
                cnt[e] = 0
            cnt[e] += 1
            op.sem = cur[e]
            op.tick = cnt[e]
        import os
        mx = int(os.environ.get("KERN_MAXOPS", "0"))
        ops_all = self.ops[:mx] if mx > 0 else self.ops
        if os.environ.get("KERN_DUMP"):
            for o in ops_all:
                print("OP", o.idx, o.eng, "dma" if o.is_dma else "", "deps", [d.idx for d in o.deps], flush=True)
        streams = {e: [o for o in ops_all if o.eng == e] for e in ENGS}

        def run(eng_handle, ops):
            waited = {}
            for op in ops:
                for d in op.deps:
                    k = id(d.sem)
                    if waited.get(k, 0) < d.tick:
                        eng_handle.wait_ge(d.sem, d.tick)
                        waited[k] = d.tick
                if op.fn is None:
                    continue
                ins = op.fn(eng_handle)
                if op.is_dma:
                    ins.then_inc(op.sem, 16)
                elif op.signal:
                    ins.then_inc(op.sem, 1)

        @block.tensor
        def _(e):
            run(e, streams["pe"])

        @block.scalar
        def _(e):
            run(e, streams["act"])

        @block.vector
        def _(e):
            run(e, streams["dve"])

        @block.gpsimd
        def _(e):
            run(e, streams["pool"])

        @block.sync
        def _(e):
            run(e, streams["sp"])


class T:
    __slots__ = ("ap", "buf", "off", "words")

    def __init__(self, ap, buf=None, name="", off=None, words=None):
        self.ap = ap
        self.off = off
        self.words = words
        if buf is None:
            buf = Buf(name, lo=off, hi=(off + words) if off is not None else None)
        self.buf = buf


class Arena:
    def __init__(self, ap_f32, total_words):
        self.a = ap_f32
        self.total = total_words

    def f32(self, off, n, name=""):
        assert off + n <= self.total, (off, n, self.total)
        return T(self.a[:, off:off + n], name=name, off=off, words=n)

    def bf16(self, off_words, n_bf, name=""):
        assert n_bf % 2 == 0 and off_words + n_bf // 2 <= self.total, (off_words, n_bf, self.total)
        return T(self.a[:, off_words:off_words + n_bf // 2].bitcast(BF16), name=name, off=off_words, words=n_bf // 2)

    def i32(self, off, n, name=""):
        return T(self.a[:, off:off + n].bitcast(I32), name=name, off=off, words=n)


class Bump:
    def __init__(self, arena, regions):
        self.arena = arena
        self.regions = [list(r) for r in regions]

    def _take(self, words):
        for r in self.regions:
            if r[1] - r[0] >= words:
                off = r[0]
                r[0] += words
                return off
        raise MemoryError(f"bump alloc {words} words; regions {self.regions}")

    def f32(self, n, name=""):
        return self.arena.f32(self._take(n), n, name)

    def bf16(self, n, name=""):
        return self.arena.bf16(self._take((n + 1) // 2), n, name)


class WStream:
    NSLOT = 4
    KEEP = 2
    SLOT_WORDS = 1024

    def __init__(self, P, arena, off_words, wpack_ap_getter):
        self.P = P
        self.seq = []
        self.tiles = {}
        self.tot = 0
        self.pos = 0
        self.issued = 0
        self.slots = [arena.bf16(off_words + i * self.SLOT_WORDS, 2 * self.SLOT_WORDS, f"wslot{i}") for i in range(self.NSLOT)]
        self.sems = None
        self.get_wpack = wpack_ap_getter

    def reset_real(self):
        self.pos = 0
        self.issued = 0
        self.sems = [self.P.new_dsem() for _ in range(self.NSLOT)]

    def _issue(self, i):
        key, n = self.seq[i]
        off = self.tiles[key][0]
        slot = i % self.NSLOT
        st = self.slots[slot]
        src = self.get_wpack()[:, off:off + n]
        dst = st.ap[:, 0:n]
        self.P.add("pool", lambda e, dst=dst, src=src: e.dma_start(out=dst, in_=src), reads=[], writes=[st.buf],
                   dma=True, dsem=self.sems[slot])

    def next(self, key, n):
        assert n <= 2 * self.SLOT_WORDS
        if self.P.dry:
            self.seq.append((key, n))
            if key not in self.tiles:
                self.tiles[key] = (self.tot, n)
                self.tot += n
            return self.slots[0]
        assert self.seq[self.pos] == (key, n), (self.pos, self.seq[self.pos], key, n)
        hi = min(len(self.seq), self.pos + self.NSLOT - self.KEEP + 1)
        while self.issued < hi:
            self._issue(self.issued)
            self.issued += 1
        st = self.slots[self.pos % self.NSLOT]
        self.pos += 1
        return st


class Kern:
    def __init__(self, n_layers=DEPTH, taps=(), stop_after=None, mixers=("mla", "gmlp", "lru", "gla")):
        self.n_layers = n_layers
        self.taps = set(taps)
        self.stop_after = stop_after
        self.mixers = mixers
        self.nc = bass.Bass("TRN2", target_bir_lowering=False)
        self.tap_names = []
        self._build()

    def mm(self, out, lhsT, rhs, start, stop, reads, writes, skip=False):
        self.P.add("pe", lambda e: e.matmul(out, lhsT, rhs, start=start, stop=stop, skip_group_check=skip),
                   reads, writes)

    def act(self, out, in_, func, reads, writes, bias=None, scale=None, accum=None):
        kw = {}
        if bias is not None:
            kw["bias"] = bias
        if scale is not None:
            kw["scale"] = scale
        if accum is not None:
            kw["accum_out"] = accum
        self.P.add("act", lambda e: e.activation(out=out, in_=in_, func=func, **kw), reads, writes)

    def ts(self, out, in0, s1, s2, op0, op1, reads, writes, eng="dve"):
        if op1 is None:
            self.P.add(eng, lambda e: e.tensor_scalar(out=out, in0=in0, scalar1=s1, scalar2=None, op0=op0), reads, writes)
        else:
            self.P.add(eng, lambda e: e.tensor_scalar(out=out, in0=in0, scalar1=s1, scalar2=s2, op0=op0, op1=op1), reads, writes)

    def tt(self, out, in0, in1, op, reads, writes, eng="dve"):
        self.P.add(eng, lambda e: e.tensor_tensor(out=out, in0=in0, in1=in1, op=op), reads, writes)

    def stt(self, out, in0, scalar, in1, op0, op1, reads, writes):
        self.P.add("dve", lambda e: e.scalar_tensor_tensor(out=out, in0=in0, scalar=scalar, in1=in1, op0=op0, op1=op1),
                   reads, writes)

    def cp(self, out, in_, reads, writes, eng="dve"):
        self.P.add(eng, lambda e: e.tensor_copy(out=out, in_=in_), reads, writes)

    def recip(self, out, in_, reads, writes):
        self.P.add("dve", lambda e: e.reciprocal(out=out, in_=in_), reads, writes)

    def memset(self, ap, val, writes, eng="dve"):
        self.P.add(eng, lambda e: e.memset(ap, val), [], writes)

    def dma(self, out, in_, reads, writes, dsem, q="sp"):
        self.P.add(q, lambda e: e.dma_start(out=out, in_=in_), reads, writes, dma=True, dsem=dsem)

    def cst(self, col, p0=0, p1=128):
        return self.cst_t.ap[p0:p1, col:col + 1]

    def ppc(self, l, name, idx=0, p0=0, p1=128):
        c = l * NPP + PP[name] + idx
        return self.pp_t.ap[p0:p1, c:c + 1]

    def tap(self, name, tiles, width=S):
        if name not in self.taps or self.P.dry:
            return
        n = len(tiles)
        dt = self.nc.dram_tensor(name, [n * 128, width], F32, kind="ExternalOutput").ap()
        self.tap_names.append(name)
        self.P.barrier(force=True)
        stg = [self.arena.f32(self.r3_free + i * 512, 512, f"tapstg{i}") for i in range(2)]
        k = 0
        for i, t in enumerate(tiles):
            for c0 in range(0, width, 512):
                s_ = stg[k % 2]
                k += 1
                self.cp(s_.ap, t.ap[:, c0:c0 + 512], [t.buf], [s_.buf])
                ob = Buf("tapout")
                self.outbufs.append(ob)
                self.dma(dt[i * 128:(i + 1) * 128, c0:c0 + 512], s_.ap, [s_.buf], [ob], self.msem())
        self.P.barrier(force=True)

    def _build(self):
        nc = self.nc
        self.x_d = nc.dram_tensor("xT", [8, 128, S], F32, kind="ExternalInput").ap()
        self.pos_d = nc.dram_tensor("pos", [1, S], I32, kind="ExternalInput").ap()
        self.pp_d = nc.dram_tensor("pp", [128, DEPTH * NPP], F32, kind="ExternalInput").ap()
        self.cst_d = nc.dram_tensor("cst", [128, NCST], F32, kind="ExternalInput").ap()
        self.rm_d = nc.dram_tensor("rmask", [128, S], F32, kind="ExternalInput").ap()
        self.rows_d = nc.dram_tensor("rows", [DEPTH, 2, 512], F32, kind="ExternalInput").ap()
        self.wg2_d = nc.dram_tensor("wg2", [DEPTH, 16, 256], F32, kind="ExternalInput").ap()
        self.out_d = nc.dram_tensor("outT", [8, 128, S], F32, kind="ExternalOutput").ap()
        self.xsp_d = nc.dram_tensor("xspill", [8, 128, S], F32, kind="Internal").ap()
        self.wpack_d = None

        es0 = ExitStack()
        self.P = Prog(nc, es0)
        self.P.dry = True
        self._alloc(es0, dry=True)
        self._program()
        seq, tiles, tot = self.ws.seq, self.ws.tiles, self.ws.tot
        taps_dry = list(self.tap_names)
        es0.close()
        self.tile_table = tiles
        self.wtot = tot
        self.tap_names = []

        tot = max(tot, 64)
        self.wtot = tot
        self.wpack_d = nc.dram_tensor("wpack", [128, tot], F32, kind="ExternalInput").ap()
        with ExitStack() as es:
            self.P = Prog(nc, es)
            self._alloc(es, dry=False)
            self.ws.seq, self.ws.tiles, self.ws.tot = seq, tiles, tot
            self.ws.reset_real()
            self._program()
            assert self.ws.pos == len(seq), (self.ws.pos, len(seq))
            block = es.enter_context(nc.Block())
            self.P.emit(block)
        self.n_ops = len(self.P.ops)

    ARENA_WORDS = 47 * 1024
    R0 = 0
    R1 = 16 * 1024
    R2 = 24 * 1024
    R3 = 40 * 1024

    def _alloc(self, es, dry):
        nc = self.nc
        if dry and hasattr(self, "_arena_dry"):
            pass
        at = es.enter_context(nc.sbuf_tensor("arena" + ("d" if dry else ""), [128, self.ARENA_WORDS], F32))
        self.arena = Arena(at[:, :], self.ARENA_WORDS)
        A = self.arena
        ps = [es.enter_context(nc.psum_tensor(f"ps{i}" + ("d" if dry else ""), [128, 1024], F32)) for i in range(4)]
        self.PS2 = [T(p[:, :], name=f"ps2_{i}") for i, p in enumerate(ps)]
        self.PB = []
        for i, p in enumerate(ps):
            self.PB.append(T(p[:, 0:512], buf=Buf(f"pb{2 * i}", excl=True)))
            self.PB.append(T(p[:, 512:1024], buf=Buf(f"pb{2 * i + 1}", excl=True)))
        o = self.R3
        self.ws = WStream(self.P, A, o, lambda: self.wpack_d)
        o += WStream.NSLOT * WStream.SLOT_WORDS
        self.pp_t = A.f32(o, DEPTH * NPP, "pp")
        o += DEPTH * NPP
        self.cst_t = A.f32(o, NCST, "cst")
        o += NCST
        self.ident_bf = A.bf16(o, 128, "ident")
        o += 64
        self.ones_bf = A.bf16(o, 128, "ones")
        o += 64
        self.gmask_bf = A.bf16(o, 128, "gmask")
        o += 64
        self.carry = A.f32(o, 88, "carry")
        o += 88
        self.lrud = A.f32(o, 16, "lrud")
        o += 16
        self.TAP_OFF = None
        self.r3_free = o
        assert o <= self.ARENA_WORDS, o
        self.xT = [A.f32(self.R0 + m * S, S, f"xT{m}") for m in range(8)]
        self.hT = [A.bf16(self.R1 + m * (S // 2), S, f"hT{m}") for m in range(8)]
        self.acc = [A.bf16(self.R2 + m * (S // 2), S, f"acc{m}") for m in range(8)]
        self.misc_sems = [self.P.new_dsem() for _ in range(6)] if not dry else [None] * 6
        self.misc_i = 0
        self.outbufs = []

    def msem(self):
        s = self.misc_sems[self.misc_i % len(self.misc_sems)]
        self.misc_i += 1
        return s

    def _program(self):
        P = self.P
        A = self.arena
        self.dma(self.pp_t.ap, self.pp_d, [], [self.pp_t.buf], self.msem())
        self.dma(self.cst_t.ap, self.cst_d, [], [self.cst_t.buf], self.msem())
        for m in range(8):
            self.dma(self.xT[m].ap, self.x_d[m], [], [self.xT[m].buf], self.msem())
        c = self.cst_t
        self.cp(self.ident_bf.ap, c.ap[:, CS_IDENT:CS_IDENT + 128], [c.buf], [self.ident_bf.buf])
        self.cp(self.ones_bf.ap, c.ap[:, CS_ONES:CS_ONES + 128], [c.buf], [self.ones_bf.buf])
        self.cp(self.gmask_bf.ap, c.ap[:, CS_GMASK:CS_GMASK + 128], [c.buf], [self.gmask_bf.buf])
        self.carrybufs = [Buf(f"carry{i}") for i in range(44)]

        for l in range(self.n_layers):
            self.layer(l)
            if self.stop_after is not None and self.stop_after[0] == l:
                break
        P.barrier()
        for m in range(8):
            ob = Buf("outb")
            self.outbufs.append(ob)
            self.dma(self.out_d[m], self.xT[m].ap, [self.xT[m].buf], [ob], self.msem())
        P.add("sp", None, reads=list(self.outbufs), writes=[])

    def prenorm(self, l, gname, src, dst, tgs, tmp, dst_off=0):
        sq = [tmp.bf16(512, f"sq{i}") for i in range(8)]
        rt = tmp.f32(512, "rt")
        rstd = tmp.f32(512, "rstd")
        bank = self.PB[7]
        for tg in tgs:
            sl = slice(tg * TG, (tg + 1) * TG)
            dl = slice(tg * TG - dst_off, (tg + 1) * TG - dst_off)
            for m in range(8):
                s = sq[m]
                self.act(s.ap, src[m].ap[:, sl], AF.Square, [src[m].buf], [s.buf])
            for m in range(8):
                s = sq[m]
                self.mm(bank.ap, self.ones_bf.ap, s.ap, m == 0, m == 7, [self.ones_bf.buf, s.buf], [bank.buf])
            self.act(rt.ap, bank.ap, AF.Ln, [bank.buf, self.cst_t.buf], [rt.buf], bias=self.cst(CS_EPS), scale=1.0 / D)
            self.act(rstd.ap, rt.ap, AF.Exp, [rt.buf], [rstd.buf], scale=-0.5)
            for m in range(8):
                self.stt(dst[m].ap[:, dl], src[m].ap[:, sl], self.ppc(l, gname, m), rstd.ap, ALU.mult, ALU.mult,
                         [src[m].buf, rstd.buf, self.pp_t.buf], [dst[m].buf])

    def layer(self, l):
        P = self.P
        A = self.arena
        P.barrier()
        tmp = Bump(A, [[self.R2 + 8192, self.R3]])
        self.prenorm(l, "nmp", self.xT, self.hT, range(NTG), tmp)
        if l > 0:
            for m in range(8):
                self.dma(self.xsp_d[m], self.xT[m].ap, [self.xT[m].buf], [self.xspbuf(m)], self.msem())
        xsrc = self.x_d if l == 0 else self.xsp_d
        self.tap(f"hT{l}", self.hT)
        if self.stop_after == (l, "n1"):
            return
        first = True
        for mx in self.mixers:
            P.barrier()
            tmp = Bump(A, [[self.R0, self.R1], [self.R2 + 8192, self.R3]])
            o_k, free = getattr(self, "mix_" + mx)(l, tmp)
            self.tap(f"o_{mx}{l}", o_k)
            k = {"gmlp": 0, "lru": 1, "mla": 2, "gla": 3}[mx]
            P.barrier()
            self.merge(l, k, o_k, first, Bump(A, free))
            first = False
        self.tap(f"acc{l}", self.acc)
        if self.stop_after == (l, "m"):
            return
        P.barrier()
        self.phase_o(l, xsrc)
        self.tap(f"xo{l}", self.xT)
        if self.stop_after == (l, "o"):
            return
        for hh in range(2):
            P.barrier()
            self.phase_f(l, hh)
        self.tap(f"xf{l}", self.xT)

    def xspbuf(self, m):
        if not hasattr(self, "_xsp"):
            self._xsp = [Buf(f"xsp{m}") for m in range(8)]
        return self._xsp[m]

    def merge(self, l, k, o_k, first, tmp):
        sg = [tmp.f32(512, "sg0"), tmp.f32(512, "sg1")]
        tm = [tmp.f32(512, "tm0"), tmp.f32(512, "tm1")]
        it = 0
        for mp in range(4):
            gt = self.ws.next(("k", "w_in", l, None, 0, 128, 8, ((C_GATE + k * 1024 + mp * 256, 256),)), 2048)
            bt = self.ws.next(("k", "w_branch", l, k, 0, 128, 4, ((mp * 256, 256),)), 1024)
            for mi in range(2):
                m = 2 * mp + mi
                for tg in range(NTG):
                    sl = slice(tg * TG, (tg + 1) * TG)
                    gb = self.PB[(it * 2) % 6]
                    bb = self.PB[(it * 2 + 1) % 6]
                    s_ = sg[it % 2]
                    t_ = tm[it % 2]
                    it += 1
                    for kc in range(8):
                        self.mm(gb.ap, gt.ap[:, kc * 256 + mi * 128: kc * 256 + mi * 128 + 128], self.hT[kc].ap[:, sl],
                                kc == 0, kc == 7, [gt.buf, self.hT[kc].buf], [gb.buf])
                    for kc in range(4):
                        self.mm(bb.ap, bt.ap[:, kc * 256 + mi * 128: kc * 256 + mi * 128 + 128], o_k[kc].ap[:, sl],
                                kc == 0, kc == 3, [bt.buf, o_k[kc].buf], [bb.buf])
                    self.act(s_.ap, gb.ap, AF.Sigmoid, [gb.buf], [s_.buf])
                    if first:
                        self.tt(self.acc[m].ap[:, sl], s_.ap, bb.ap, ALU.mult, [s_.buf, bb.buf], [self.acc[m].buf])
                    else:
                        self.tt(t_.ap, s_.ap, bb.ap, ALU.mult, [s_.buf, bb.buf], [t_.buf])
                        self.tt(self.acc[m].ap[:, sl], self.acc[m].ap[:, sl], t_.ap, ALU.add,
                                [self.acc[m].buf, t_.buf], [self.acc[m].buf], eng="pool")

    def phase_o(self, l, xsrc):
        A = self.arena
        tmp = Bump(A, [[self.R2 + 8192, self.R3]])
        mix = self.xT
        xo = [[A.f32(self.R2 + 8192 + b * 4096 + m * 512, 512, f"xo{b}_{m}") for m in range(8)] for b in range(2)]
        tmp = Bump(A, [[self.R1, self.R2]])
        it = 0
        for mp in range(4):
            wt = self.ws.next(("k", "w_out", l, None, 0, 128, 8, ((mp * 256, 256),)), 2048)
            for mi in range(2):
                m = 2 * mp + mi
                for tg in range(NTG):
                    sl = slice(tg * TG, (tg + 1) * TG)
                    b = self.PB[it % 6]
                    it += 1
                    for kc in range(8):
                        self.mm(b.ap, wt.ap[:, kc * 256 + mi * 128: kc * 256 + mi * 128 + 128], self.acc[kc].ap[:, sl],
                                kc == 0, kc == 7, [wt.buf, self.acc[kc].buf], [b.buf])
                    self.act(mix[m].ap[:, sl], b.ap, AF.Copy, [b.buf], [mix[m].buf])
        self.postnorm(l, "nmo", mix, range(NTG), tmp, xsrc, xo)

    def postnorm(self, l, gname, f, tgs, tmp, xsrc=None, xo=None, f_off=0, xres=None):
        sq = [tmp.bf16(512, f"sq{i}") for i in range(8)]
        rt = tmp.f32(512, "rt")
        rstd = tmp.f32(512, "rstd")
        bank = self.PB[7]
        for i, tg in enumerate(tgs):
            sl = slice(tg * TG, (tg + 1) * TG)
            fl = slice(tg * TG - f_off, (tg + 1) * TG - f_off)
            if xsrc is not None:
                xb = xo[i % 2]
                for m in range(8):
                    self.dma(xb[m].ap, xsrc[m][:, sl], [self.xspbuf(m)], [xb[m].buf], self.msem())
            for m in range(8):
                s = sq[m]
                self.act(s.ap, f[m].ap[:, fl], AF.Square, [f[m].buf], [s.buf])
            for m in range(8):
                s = sq[m]
                self.mm(bank.ap, self.ones_bf.ap, s.ap, m == 0, m == 7, [self.ones_bf.buf, s.buf], [bank.buf])
            self.act(rt.ap, bank.ap, AF.Ln, [bank.buf, self.cst_t.buf], [rt.buf], bias=self.cst(CS_EPS), scale=1.0 / D)
            self.act(rstd.ap, rt.ap, AF.Exp, [rt.buf], [rstd.buf], scale=-0.5)
            for m in range(8):
                if xsrc is not None:
                    self.stt(f[m].ap[:, fl], f[m].ap[:, fl], self.ppc(l, gname, m), rstd.ap, ALU.mult, ALU.mult,
                             [f[m].buf, rstd.buf, self.pp_t.buf], [f[m].buf])
                    self.tt(f[m].ap[:, fl], f[m].ap[:, fl], xb[m].ap, ALU.add, [f[m].buf, xb[m].buf], [f[m].buf], eng="pool")
                else:
                    self.stt(f[m].ap[:, fl], f[m].ap[:, fl], self.ppc(l, gname, m), rstd.ap, ALU.mult, ALU.mult,
                             [f[m].buf, rstd.buf, self.pp_t.buf], [f[m].buf])
                    self.tt(xres[m].ap[:, sl], xres[m].ap[:, sl], f[m].ap[:, fl], ALU.add,
                            [xres[m].buf, f[m].buf], [xres[m].buf], eng="pool")

    def phase_f(self, l, hh):
        A = self.arena
        H = 1024
        t0 = hh * H
        h2 = [A.bf16(self.R1 + m * 512, H, f"h2_{m}") for m in range(8)]
        tmpn = Bump(A, [[self.R1 + 4096, self.R2]])
        self.prenorm(l, "nfp", self.xT, h2, [2 * hh, 2 * hh + 1], tmpn, dst_off=t0)
        g = [A.bf16(self.R2 + j * 512, H, f"g{j}") for j in range(22)]
        yo = self.R2 + 22 * 512
        ya = [A.f32(yo + b * 2048, H, f"ya{b}") for b in range(2)]
        yb = [A.f32(yo + b * 2048 + 1024, H, f"yb{b}") for b in range(2)]
        assert yo + 4096 <= self.R3
        for j in range(22):
            wt = self.ws.next(("k", "ffn_w_up", l, None, 0, 128, 8, ((j * 128, 128), (FFN + j * 128, 128))), 2048)
            pa = 2 * (j % 2)
            for ab in range(2):
                for tl in range(2):
                    bank = self.PB[(pa + ab) * 2 + tl]
                    sl = slice(tl * TG, (tl + 1) * TG)
                    for kc in range(8):
                        self.mm(bank.ap, wt.ap[:, kc * 256 + ab * 128: kc * 256 + ab * 128 + 128], h2[kc].ap[:, sl],
                                kc == 0, kc == 7, [wt.buf, h2[kc].buf], [bank.buf])
            ys = [ya[j % 2], yb[j % 2]]
            for ab in range(2):
                jj = j + 22 * ab
                z = self.PS2[pa + ab].ap
                zb = [self.PB[(pa + ab) * 2].buf, self.PB[(pa + ab) * 2 + 1].buf]
                y = ys[ab]
                w = lambda k: self.ppc(l, "fcw", jj * 3 + k)
                self.act(y.ap, z, AF.Identity, zb + [self.pp_t.buf], [y.buf], bias=self.ppc(l, "fcb", jj), scale=w(2))
                if hh == 0:
                    self.act(self.carry.ap[:, jj * 2: jj * 2 + 2], z[:, H - 2:H], AF.Copy, zb, [self.carrybufs[jj]])
                self.stt(y.ap[:, 1:H], z[:, 0:H - 1], w(1), y.ap[:, 1:H], ALU.mult, ALU.add, zb + [y.buf, self.pp_t.buf], [y.buf])
                self.stt(y.ap[:, 2:H], z[:, 0:H - 2], w(0), y.ap[:, 2:H], ALU.mult, ALU.add, zb + [y.buf, self.pp_t.buf], [y.buf])
                cr = self.carry.ap[:, jj * 2: jj * 2 + 2]
                if hh == 1:
                    self.stt(y.ap[:, 0:1], cr[:, 1:2], w(1), y.ap[:, 0:1], ALU.mult, ALU.add,
                             [self.carrybufs[jj], y.buf, self.pp_t.buf], [y.buf])
                    self.stt(y.ap[:, 0:2], cr[:, 0:2], w(0), y.ap[:, 0:2], ALU.mult, ALU.add,
                             [self.carrybufs[jj], y.buf, self.pp_t.buf], [y.buf])
            self.act(ys[0].ap, ys[0].ap, AF.Gelu_apprx_tanh, [ys[0].buf], [ys[0].buf])
            self.tt(g[j].ap, ys[0].ap, ys[1].ap, ALU.mult, [ys[0].buf, ys[1].buf], [g[j].buf], eng="pool")
        f = [A.f32(self.R1 + 4096 + m * 1024, H, f"f{m}") for m in range(4)] + \
            [A.f32(yo + (m - 4) * 1024, H, f"f{m}") for m in range(4, 8)]
        self.P.barrier()
        for m in range(8):
            banks = [self.PB[(m % 2) * 2], self.PB[(m % 2) * 2 + 1]]
            for half in range(2):
                wt = self.ws.next(("k", "ffn_w_down", l, None, half * 11 * 128, 128, 11, ((m * 128, 128),)), 11 * 128)
                for tl in range(2):
                    sl = slice(tl * TG, (tl + 1) * TG)
                    for jc in range(11):
                        j = half * 11 + jc
                        self.mm(banks[tl].ap, wt.ap[:, jc * 128:(jc + 1) * 128], g[j].ap[:, sl],
                                j == 0, j == 21, [wt.buf, g[j].buf], [banks[tl].buf])
            for tl in range(2):
                self.act(f[m].ap[:, tl * TG:(tl + 1) * TG], banks[tl].ap, AF.Copy, [banks[tl].buf], [f[m].buf])
        tmpn = Bump(A, [[self.R1, self.R1 + 4096]])
        self.postnorm(l, "nfo", f, [2 * hh, 2 * hh + 1], tmpn, f_off=t0, xres=self.xT)

    def rope_tables(self, cosT, sinT, tr):
        R = slice(64, 96)
        _pt = tr.f32(S, "posi")
        posi = T(_pt.ap.bitcast(I32), buf=_pt.buf)
        posf = tr.f32(S, "posf")
        ang = tr.f32(S, "ang")
        ta = tr.f32(S, "ta")
        tb = tr.f32(S, "tb")
        self.dma(posi.ap[R, :], self.pos_d[0, :].partition_broadcast(32), [], [posi.buf], self.msem())
        self.cp(posf.ap[R, :], posi.ap[R, :], [posi.buf], [posf.buf])
        cb = self.cst_t.buf
        self.ts(ang.ap[R, :], posf.ap[R, :], self.cst(CS_INVF, 64, 96), None, ALU.mult, None, [posf.buf, cb], [ang.buf])
        MAGIC = 12582912.0
        INV2PI = float(np.float32(1.0 / (2 * np.pi)))
        C1 = 6.28125
        C2 = float(np.float32(2 * np.pi - 6.28125))
        PIS = 3.1415925
        for which, dst in (("sin", sinT), ("cos", cosT)):
            if which == "sin":
                self.ts(ta.ap[R, :], ang.ap[R, :], INV2PI, None, ALU.mult, None, [ang.buf], [ta.buf])
            else:
                self.ts(ta.ap[R, :], ang.ap[R, :], INV2PI, 0.25, ALU.mult, ALU.add, [ang.buf], [ta.buf])
            self.ts(tb.ap[R, :], ta.ap[R, :], MAGIC, None, ALU.add, None, [ta.buf], [tb.buf])
            self.ts(ta.ap[R, :], tb.ap[R, :], -MAGIC, None, ALU.add, None, [tb.buf], [ta.buf])
            self.stt(tb.ap[R, :], ta.ap[R, :], -C1, ang.ap[R, :], ALU.mult, ALU.add, [ta.buf, ang.buf], [tb.buf])
            self.stt(tb.ap[R, :], ta.ap[R, :], -C2, tb.ap[R, :], ALU.mult, ALU.add, [ta.buf, tb.buf], [tb.buf])
            if which == "cos":
                self.ts(tb.ap[R, :], tb.ap[R, :], float(np.float32(np.pi / 2)), None, ALU.add, None, [tb.buf], [tb.buf])
            self.ts(tb.ap[R, :], tb.ap[R, :], PIS, -PIS, ALU.min, ALU.max, [tb.buf], [tb.buf])
            if which == "sin":
                self.act(dst.ap[R, :], tb.ap[R, :], AF.Sin, [tb.buf, cb], [dst.buf], scale=self.cst(CS_SIGN, 64, 96))
            else:
                self.act(dst.ap[R, :], tb.ap[R, :], AF.Sin, [tb.buf], [dst.buf])

    def mix_mla(self, l, tmp):
        P = self.P
        A = self.arena
        o_c = [tmp.bf16(S, f"oc{i}") for i in range(4)]
        free = [list(r) for r in tmp.regions]
        cosT = tmp.bf16(S, "cos")
        sinT = tmp.bf16(S, "sin")
        tr = Bump(A, [list(r) for r in tmp.regions])
        self.rope_tables(cosT, sinT, tr)
        P.barrier()
        cqn = [tmp.bf16(S, f"cqn{i}") for i in range(3)]
        ckvn = [tmp.bf16(S, f"ckvn{i}") for i in range(2)]
        QT = [tmp.bf16(S, f"QT{i}") for i in range(2)]
        KT = [tmp.bf16(S, f"KT{i}") for i in range(2)]
        VA = [tmp.bf16(16 * 128, f"VA{i}") for i in range(2)]
        Pb = [tmp.bf16(512, f"Pb{i}") for i in range(4)]
        rawq = [tmp.f32(512, f"rawq{i}") for i in range(3)]
        rawk = [tmp.f32(512, f"rawk{i}") for i in range(2)]
        sq = [tmp.bf16(512, f"sq{i}") for i in range(2)]
        rt = tmp.f32(512, "rt")
        rstd = tmp.f32(512, "rstd")
        t1 = tmp.f32(512, "t1")
        t2 = tmp.f32(512, "t2")
        rec = tmp.f32(512, "rec")
        rec2 = tmp.f32(512, "rec2")
        cb = self.cst_t.buf
        ppb = self.pp_t.buf
        RR = slice(64, 96)
        VAv = [v.ap.rearrange("p (k c) -> p k c", c=128) for v in VA]
        self.memset(VAv[0][:, :, 64:128], 1.0, [VA[0].buf])
        self.memset(VAv[1][:, :, 0:64], 1.0, [VA[1].buf])

        def normgroup(tiles_cols, raws, dsts, gname, nfeat, ssq_bank, tg):
            sl = slice(tg * TG, (tg + 1) * TG)
            n = len(tiles_cols)
            for c, (wt, co, ks) in enumerate(tiles_cols):
                bank = self.PB[c % 2]
                for kc in range(8):
                    self.mm(bank.ap, wt.ap[:, kc * ks + co: kc * ks + co + 128], self.hT[kc].ap[:, sl], kc == 0, kc == 7,
                            [wt.buf, self.hT[kc].buf], [bank.buf])
                s = sq[c % 2]
                self.act(s.ap, bank.ap, AF.Square, [bank.buf], [s.buf])
                self.mm(ssq_bank.ap, self.ones_bf.ap, s.ap, c == 0, c == n - 1, [self.ones_bf.buf, s.buf], [ssq_bank.buf])
                self.cp(raws[c].ap, bank.ap, [bank.buf], [raws[c].buf])
            self.act(rt.ap, ssq_bank.ap, AF.Ln, [ssq_bank.buf, cb], [rt.buf], bias=self.cst(CS_EPS), scale=1.0 / nfeat)
            self.act(rstd.ap, rt.ap, AF.Exp, [rt.buf], [rstd.buf], scale=-0.5)
            for c in range(n):
                self.stt(dsts[c].ap[:, sl], raws[c].ap, self.ppc(l, gname, c), rstd.ap, ALU.mult, ALU.mult,
                         [raws[c].buf, rstd.buf, ppb], [dsts[c].buf])

        for tg in range(NTG):
            sl = slice(tg * TG, (tg + 1) * TG)
            tA = self.ws.next(("k", "w_in", l, None, 0, 128, 8, ((C_CQ, 256),)), 2048)
            tB = self.ws.next(("k", "w_in", l, None, 0, 128, 8, ((C_CQ + 256, 128), (C_CKV, 128))), 2048)
            normgroup([(tA, 0, 256), (tA, 128, 256), (tB, 0, 256)], rawq, cqn, "mqn", 384, self.PB[7], tg)
            bank0 = self.PB[2]
            for kc in range(8):
                self.mm(bank0.ap, tB.ap[:, kc * 256 + 128: kc * 256 + 256], self.hT[kc].ap[:, sl], kc == 0, kc == 7,
                        [tB.buf, self.hT[kc].buf], [bank0.buf])
            s = sq[0]
            self.act(s.ap, bank0.ap, AF.Square, [bank0.buf], [s.buf])
            self.mm(self.PB[6].ap, self.ones_bf.ap, s.ap, True, False, [self.ones_bf.buf, s.buf], [self.PB[6].buf])
            self.cp(rawk[0].ap, bank0.ap, [bank0.buf], [rawk[0].buf])
            tC = self.ws.next(("k", "w_in", l, None, 0, 128, 8, ((C_CKV + 128, 128), (C_KR - 64, 96))), 8 * 224)
            bank1 = self.PB[3]
            for kc in range(8):
                self.mm(bank1.ap, tC.ap[:, kc * 224: kc * 224 + 128], self.hT[kc].ap[:, sl], kc == 0, kc == 7,
                        [tC.buf, self.hT[kc].buf], [bank1.buf])
            s = sq[1]
            self.act(s.ap, bank1.ap, AF.Square, [bank1.buf], [s.buf])
            self.mm(self.PB[6].ap, self.ones_bf.ap, s.ap, False, True, [self.ones_bf.buf, s.buf], [self.PB[6].buf])
            self.cp(rawk[1].ap, bank1.ap, [bank1.buf], [rawk[1].buf])
            self.act(rt.ap, self.PB[6].ap, AF.Ln, [self.PB[6].buf, cb], [rt.buf], bias=self.cst(CS_EPS), scale=1.0 / 256)
            self.act(rstd.ap, rt.ap, AF.Exp, [rt.buf], [rstd.buf], scale=-0.5)
            for c in range(2):
                self.stt(ckvn[c].ap[:, sl], rawk[c].ap, self.ppc(l, "mkn", c), rstd.ap, ALU.mult, ALU.mult,
                         [rawk[c].buf, rstd.buf, ppb], [ckvn[c].buf])
            bA = self.PB[4]
            for kc in range(8):
                self.mm(bA.ap[0:96, :], tC.ap[:, kc * 224 + 128: kc * 224 + 224], self.hT[kc].ap[:, sl], kc == 0, kc == 7,
                        [tC.buf, self.hT[kc].buf], [bA.buf])
            tD = self.ws.next(("k", "w_in", l, None, 0, 128, 8, ((C_KR - 64, 64), (C_KR + 16, 16), (C_KR, 16))), 8 * 96)
            bB = self.PB[5]
            for kc in range(8):
                self.mm(bB.ap[0:96, :], tD.ap[:, kc * 96: kc * 96 + 96], self.hT[kc].ap[:, sl], kc == 0, kc == 7,
                        [tD.buf, self.hT[kc].buf], [bB.buf])
            self.tt(t1.ap[RR, :], bA.ap[RR, :], cosT.ap[RR, sl], ALU.mult, [bA.buf, cosT.buf], [t1.buf])
            self.tt(t2.ap[RR, :], bB.ap[RR, :], sinT.ap[RR, sl], ALU.mult, [bB.buf, sinT.buf], [t2.buf])
            self.tt(KT[0].ap[RR, sl], t1.ap[RR, :], t2.ap[RR, :], ALU.add, [t1.buf, t2.buf], [KT[0].buf])
            self.cp(KT[1].ap[RR, sl], KT[0].ap[RR, sl], [KT[0].buf], [KT[1].buf])

        SCALE = float(96 ** -0.5)
        for h in range(8):
            par = h % 2
            QTh, KTh, VAh, VAhv = QT[par], KT[par], VA[par], VAv[par]
            voff = 0 if par == 0 else 64
            tq = self.ws.next(("k", "mla_w_uq", l, None, 0, 128, 3,
                               ((h * 96, 96), (h * 96, 64), (h * 96 + 80, 16), (h * 96 + 64, 16))), 3 * 192)
            tkv = self.ws.next(("k", "mla_w_ukv", l, None, 0, 128, 2, ((h * 128, 128),)), 256)
            for tg in range(NTG):
                sl = slice(tg * TG, (tg + 1) * TG)
                bA, bB, bK = self.PB[4], self.PB[5], self.PB[7]
                for kc in range(3):
                    self.mm(bA.ap[0:96, :], tq.ap[:, kc * 192: kc * 192 + 96], cqn[kc].ap[:, sl], kc == 0, kc == 2,
                            [tq.buf, cqn[kc].buf], [bA.buf])
                for kc in range(3):
                    self.mm(bB.ap[0:96, :], tq.ap[:, kc * 192 + 96: kc * 192 + 192], cqn[kc].ap[:, sl], kc == 0, kc == 2,
                            [tq.buf, cqn[kc].buf], [bB.buf])
                for kc in range(2):
                    self.mm(bK.ap[0:64, :], tkv.ap[:, kc * 128: kc * 128 + 64], ckvn[kc].ap[:, sl], kc == 0, kc == 1,
                            [tkv.buf, ckvn[kc].buf], [bK.buf])
                self.cp(QTh.ap[0:64, sl], bA.ap[0:64, :], [bA.buf], [QTh.buf])
                self.tt(t1.ap[RR, :], bA.ap[RR, :], cosT.ap[RR, sl], ALU.mult, [bA.buf, cosT.buf], [t1.buf])
                self.tt(t2.ap[RR, :], bB.ap[RR, :], sinT.ap[RR, sl], ALU.mult, [bB.buf, sinT.buf], [t2.buf])
                self.tt(QTh.ap[RR, sl], t1.ap[RR, :], t2.ap[RR, :], ALU.add, [t1.buf, t2.buf], [QTh.buf])
                self.cp(KTh.ap[0:64, sl], bK.ap[0:64, :], [bK.buf], [KTh.buf])
            for kh in range(2):
                bV = self.PB[7]
                for k8 in range(8):
                    kt = kh * 8 + k8
                    for kc in range(2):
                        self.mm(bV.ap[:, k8 * 64:(k8 + 1) * 64], ckvn[kc].ap[:, kt * 128:(kt + 1) * 128],
                                tkv.ap[:, kc * 128 + 64: kc * 128 + 128], kc == 0, kc == 1,
                                [tkv.buf, ckvn[kc].buf], [bV.buf])
                self.cp(VAhv[:, kh * 8:(kh + 1) * 8, voff:voff + 64], bV.ap.rearrange("p (k c) -> p k c", c=64),
                        [bV.buf], [VAh.buf])
            m = h // 2
            pairs = [(g, j) for g in range(4) for j in range(4 * g + 4)]
            SB = [self.PB[0], self.PB[1], self.PB[6]]
            LOOK = 3

            def emitS(i):
                g, j = pairs[i]
                Sb = SB[i % 3]
                pb = Pb[i % 4]
                c0 = max(0, j - 4 * g) * 128
                self.mm(Sb.ap[:, c0:512], KTh.ap[0:96, j * 128:(j + 1) * 128], QTh.ap[0:96, g * 512 + c0:(g + 1) * 512],
                        True, True, [KTh.buf, QTh.buf], [Sb.buf])
                self.act(pb.ap[:, c0:512], Sb.ap[:, c0:512], AF.Exp, [Sb.buf], [pb.buf], scale=SCALE)
                if j >= 4 * g:
                    self.memset(pb.ap[64:128, c0:c0 + 64], 0.0, [pb.buf])

            def emitPV(i):
                g, j = pairs[i]
                pb = Pb[i % 4]
                O = self.PB[2 + g % 2]
                nj = 4 * g + 4
                c0 = max(0, j - 4 * g) * 128
                self.mm(O.ap[:, c0:512], VAhv[:, j, :], pb.ap[:, c0:512], j == 0, j == nj - 1,
                        [VAh.buf, pb.buf], [O.buf], skip=True)
                if j == nj - 1:
                    gs = slice(g * 512, (g + 1) * 512)
                    if par == 0:
                        self.act(rec.ap[64:128, :], O.ap[64:128, :], AF.Ln, [O.buf], [rec.buf])
                        self.act(rec.ap[64:128, :], rec.ap[64:128, :], AF.Exp, [rec.buf], [rec.buf], scale=-1.0)
                        self.cp(rec2.ap[0:64, :], rec.ap[64:128, :], [rec.buf], [rec2.buf])
                        self.tt(o_c[m].ap[0:64, gs], O.ap[0:64, :], rec2.ap[0:64, :], ALU.mult, [O.buf, rec2.buf], [o_c[m].buf])
                    else:
                        self.act(rec.ap[0:64, :], O.ap[0:64, :], AF.Ln, [O.buf], [rec.buf])
                        self.act(rec.ap[0:64, :], rec.ap[0:64, :], AF.Exp, [rec.buf], [rec.buf], scale=-1.0)
                        self.cp(rec2.ap[64:128, :], rec.ap[0:64, :], [rec.buf], [rec2.buf])
                        self.tt(o_c[m].ap[64:128, gs], O.ap[64:128, :], rec2.ap[64:128, :], ALU.mult, [O.buf, rec2.buf],
                                [o_c[m].buf])

            for i in range(len(pairs) + LOOK):
                if i < len(pairs):
                    emitS(i)
                if i - LOOK >= 0:
                    emitPV(i - LOOK)
        return o_c, free

    def mix_gmlp(self, l, tmp):
        o_a = [tmp.bf16(S, f"oa{i}") for i in range(4)]
        free = [list(r) for r in tmp.regions]
        grow = tmp.f32(512, "grow")
        brow = tmp.f32(512, "brow")
        self.dma(grow.ap, self.rows_d[l, 0, :].partition_broadcast(128), [], [grow.buf], self.msem())
        self.dma(brow.ap, self.rows_d[l, 1, :].partition_broadcast(128), [], [brow.buf], self.msem())
        vn = tmp.bf16(16 * 512, "vn")
        vnv = vn.ap.rearrange("p (n c) -> p n c", c=512)
        junk = tmp.bf16(512, "junk")
        ssq = tmp.f32(16, "ssq")
        rt16 = tmp.f32(16, "rt16")
        rsd = tmp.f32(16, "rsd")
        wm = tmp.bf16(512, "wm")
        ug = [tmp.f32(512, f"ug{i}") for i in range(2)]
        svb = [tmp.f32(512, f"svb{i}") for i in range(2)]
        cb = self.cst_t.buf
        tv = [self.ws.next(("k", "w_in", l, None, 0, 128, 8, ((512 + hf * 256, 256),)), 2048) for hf in range(2)]
        gvall = tmp.f32(16 * 512, "gvall")
        gvv = gvall.ap.rearrange("p (n c) -> p n c", c=512)
        for n in range(16):
            bank = self.PB[n % 2]
            for hf in range(2):
                for kc in range(8):
                    self.mm(bank.ap[:, hf * 256:(hf + 1) * 256], self.hT[kc].ap[:, n * 128:(n + 1) * 128],
                            tv[hf].ap[:, kc * 256:(kc + 1) * 256], kc == 0, kc == 7,
                            [tv[hf].buf, self.hT[kc].buf], [bank.buf])
            self.act(gvv[:, n, :], bank.ap, AF.Gelu_apprx_tanh, [bank.buf], [gvall.buf])
        for n in range(16):
            self.act(junk.ap, gvv[:, n, :], AF.Square, [gvall.buf], [junk.buf, ssq.buf], accum=ssq.ap[:, n:n + 1])
        self.act(rt16.ap, ssq.ap, AF.Ln, [ssq.buf, cb], [rt16.buf], bias=self.cst(CS_EPS), scale=1.0 / 512)
        self.act(rsd.ap, rt16.ap, AF.Exp, [rt16.buf], [rsd.buf], scale=-0.5)
        for n in range(16):
            self.stt(vnv[:, n, :], gvv[:, n, :], rsd.ap[:, n:n + 1], grow.ap, ALU.mult, ALU.mult,
                     [gvall.buf, rsd.buf, grow.buf], [vn.buf])
        tw = self.ws.next(("blkT", "gmlp_w_s", l), 512)
        self.cp(wm.ap, tw.ap[:, 0:512], [tw.buf], [wm.buf])
        self.memset(wm.ap.rearrange("p (h t) -> p h t", t=128)[64:128, :, 0:64], 0.0, [wm.buf])
        it = 0
        for hp in range(2):
            tu = self.ws.next(("k", "w_in", l, None, 0, 128, 8, ((hp * 256, 256),)), 2048)
            for hi in range(2):
                h = 2 * hp + hi
                for tg in range(NTG):
                    sl = slice(tg * TG, (tg + 1) * TG)
                    ub = self.PB[2 + it % 2]
                    sb = self.PB[4 + it % 2]
                    u_ = ug[it % 2]
                    s_ = svb[it % 2]
                    it += 1
                    for kc in range(8):
                        self.mm(ub.ap, tu.ap[:, kc * 256 + hi * 128: kc * 256 + hi * 128 + 128], self.hT[kc].ap[:, sl],
                                kc == 0, kc == 7, [tu.buf, self.hT[kc].buf], [ub.buf])
                    for i in range(4):
                        n = 4 * tg + i
                        self.mm(sb.ap[:, i * 128:(i + 1) * 128], vnv[:, n, h * 128:(h + 1) * 128], wm.ap[:, h * 128:(h + 1) * 128],
                                True, True, [vn.buf, wm.buf], [sb.buf])
                    self.act(u_.ap, ub.ap, AF.Gelu_apprx_tanh, [ub.buf], [u_.buf])
                    bb = brow.ap[:, h * 128:(h + 1) * 128].unsqueeze(1).to_broadcast([128, 4, 128])
                    self.tt(s_.ap.rearrange("p (a b) -> p a b", b=128), sb.ap.rearrange("p (a b) -> p a b", b=128), bb,
                            ALU.add, [sb.buf, brow.buf], [s_.buf])
                    self.tt(o_a[h].ap[:, sl], s_.ap, u_.ap, ALU.mult, [s_.buf, u_.buf], [o_a[h].buf])
        return o_a, free

    def mix_lru(self, l, tmp):
        ppb = self.pp_t.buf
        cb = self.cst_t.buf
        d = self.lrud
        lam = self.pp_t.ap[:, l * NPP + PP["llam"]: l * NPP + PP["llam"] + 4]
        self.act(d.ap[:, 0:4], lam, AF.Exp, [ppb], [d.buf], scale=-1.0)
        self.act(d.ap[:, 4:8], d.ap[:, 0:4], AF.Ln, [d.buf, cb], [d.buf], bias=self.cst(CS_ONE))
        self.ts(d.ap[:, 8:12], d.ap[:, 4:8], -8.0, None, ALU.mult, None, [d.buf], [d.buf])
        o_b = [tmp.bf16(S, f"ob{i}") for i in range(4)]
        free = [list(r) for r in tmp.regions]
        wa = tmp.bf16(512, "wa")
        wi = tmp.bf16(512, "wi")
        t_ = self.ws.next(("blk", "lru_w_a", l), 512)
        self.cp(wa.ap, t_.ap[:, 0:512], [t_.buf], [wa.buf])
        t_ = self.ws.next(("blk", "lru_w_i", l), 512)
        self.cp(wi.ap, t_.ap[:, 0:512], [t_.buf], [wi.buf])
        uraw = tmp.f32(S + 4, "uraw")
        up = tmp.f32(S, "up")
        upb = tmp.bf16(S, "upb")
        ra = tmp.f32(S, "ra")
        ig = tmp.f32(S, "ig")
        sq_ = tmp.f32(S, "sq_")
        hh_ = tmp.f32(S, "hh")
        yg = [tmp.f32(512, f"yg{i}") for i in range(2)]
        self.memset(uraw.ap[:, 0:3], 0.0, [uraw.buf])
        it = 0
        for c in range(4):
            tu = self.ws.next(("k", "w_in", l, None, 0, 128, 8, ((C_UB + c * 128, 128),)), 1024)
            for tg in range(NTG):
                sl = slice(tg * TG, (tg + 1) * TG)
                b = self.PB[tg % 2]
                for kc in range(8):
                    self.mm(b.ap, tu.ap[:, kc * 128:(kc + 1) * 128], self.hT[kc].ap[:, sl], kc == 0, kc == 7,
                            [tu.buf, self.hT[kc].buf], [b.buf])
                self.act(uraw.ap[:, 3 + tg * TG: 3 + (tg + 1) * TG], b.ap, AF.Copy, [b.buf], [uraw.buf])
            w = lambda k: self.ppc(l, "lcw", c * 4 + k)
            self.ts(up.ap, uraw.ap[:, 0:S], w(0), self.ppc(l, "lcb", c), ALU.mult, ALU.add, [uraw.buf, ppb], [up.buf])
            for k in range(1, 4):
                self.stt(up.ap, uraw.ap[:, k:k + S], w(k), up.ap, ALU.mult, ALU.add, [uraw.buf, up.buf, ppb], [up.buf])
            self.act(upb.ap, up.ap, AF.Copy, [up.buf], [upb.buf])
            for tg in range(NTG):
                sl = slice(tg * TG, (tg + 1) * TG)
                b = self.PB[2 + tg % 2]
                self.mm(b.ap, wa.ap[:, c * 128:(c + 1) * 128], upb.ap[:, sl], True, True, [wa.buf, upb.buf], [b.buf])
                self.act(ra.ap[:, sl], b.ap, AF.Sigmoid, [b.buf, ppb], [ra.buf], bias=self.ppc(l, "lba", c))
                b2 = self.PB[4 + tg % 2]
                self.mm(b2.ap, wi.ap[:, c * 128:(c + 1) * 128], upb.ap[:, sl], True, True, [wi.buf, upb.buf], [b2.buf])
                self.act(ig.ap[:, sl], b2.ap, AF.Sigmoid, [b2.buf, ppb], [ig.buf], bias=self.ppc(l, "lbi", c))
            self.act(ra.ap, ra.ap, AF.Exp, [ra.buf, d.buf], [ra.buf], scale=d.ap[:, 8 + c: 9 + c])
            self.stt(sq_.ap, ra.ap, -1.0, ra.ap, ALU.mult, ALU.mult, [ra.buf], [sq_.buf])
            self.act(sq_.ap, sq_.ap, AF.Sqrt, [sq_.buf, cb], [sq_.buf], bias=self.cst(CS_ONE))
            self.tt(ig.ap, ig.ap, up.ap, ALU.mult, [ig.buf, up.buf], [ig.buf])
            self.tt(ig.ap, ig.ap, sq_.ap, ALU.mult, [ig.buf, sq_.buf], [ig.buf])
            self.P.add("dve", lambda e, o=hh_.ap, a=ra.ap, x=ig.ap: e.tensor_tensor_scan(out=o, data0=a, data1=x, initial=0.0,
                                                                                         op0=ALU.mult, op1=ALU.add),
                       [ra.buf, ig.buf], [hh_.buf])
            ty = self.ws.next(("k", "w_in", l, None, 0, 128, 8, ((C_YB + c * 128, 128),)), 1024)
            for tg in range(NTG):
                sl = slice(tg * TG, (tg + 1) * TG)
                b = self.PB[6 + tg % 2]
                y_ = yg[it % 2]
                it += 1
                for kc in range(8):
                    self.mm(b.ap, ty.ap[:, kc * 128:(kc + 1) * 128], self.hT[kc].ap[:, sl], kc == 0, kc == 7,
                            [ty.buf, self.hT[kc].buf], [b.buf])
                self.act(y_.ap, b.ap, AF.Gelu_apprx_tanh, [b.buf], [y_.buf])
                self.tt(o_b[c].ap[:, sl], hh_.ap[:, sl], y_.ap, ALU.mult, [hh_.buf, y_.buf], [o_b[c].buf])
        return o_b, free

    def mix_gla(self, l, tmp):
        P = self.P
        A = self.arena
        ppb = self.pp_t.buf
        cb = self.cst_t.buf
        baseA = Bump(A, [list(r) for r in tmp.regions])
        ogl_words = 4 * S
        glr = tmp.f32(S, "glr")
        Cc = tmp.f32(S, "Cc")
        spb = tmp.f32(S, "spb")
        rmask = tmp.f32(S, "rmask")
        ogl_all = baseA.f32(4 * S, "ogl")
        ogl = [T(ogl_all.ap[:, h * S:(h + 1) * S], buf=ogl_all.buf) for h in range(4)]
        wg2 = tmp.f32(256, "wg2")
        nbg = tmp.f32(2, "nbg")
        qk_off = tmp._take(4096)
        free = [[qk_off, qk_off + 4096]]
        sub = Bump(A, [[qk_off, qk_off + 4096]])
        qin = [sub.bf16(S, f"qin{i}") for i in range(2)]
        kin = [sub.bf16(S, f"kin{i}") for i in range(2)]
        kstT = [tmp.bf16(512, f"kstT{i}") for i in range(2)]
        ksttok = tmp.bf16(16 * 256, "ksttok")
        ksv = ksttok.ap.rearrange("p (n c) -> p n c", c=256)
        vtok = tmp.bf16(16 * 512, "vtok")
        vtv = vtok.ap.rearrange("p (n c) -> p n c", c=512)
        et = [tmp.f32(512, f"et{i}") for i in range(3)]
        dec = tmp.f32(64, "dec")
        Sf = tmp.f32(256, "Sf")
        Sb_ = tmp.bf16(256, "Sb")
        attm = [tmp.bf16(512, f"attm{i}") for i in range(2)]
        sq = [tmp.bf16(512, f"gsq{i}") for i in range(2)]
        rt = tmp.f32(512, "grt")
        rstd = tmp.f32(512, "grstd")
        sg = tmp.f32(512, "gsg")
        tq_ = tmp.f32(512, "gtq")
        self.dma(wg2.ap[0:16, :], self.wg2_d[l], [], [wg2.buf], self.msem())
        self.dma(rmask.ap, self.rm_d, [], [rmask.buf], self.msem())
        gb = self.pp_t.ap[:, l * NPP + PP["gbg"]: l * NPP + PP["gbg"] + 2]
        self.ts(nbg.ap, gb, -1.0, None, ALU.mult, None, [ppb], [nbg.buf])
        tg_ = self.ws.next(("k", "w_in", l, None, 0, 128, 8, ((C_GD, 16),)), 128)
        for tg in range(NTG):
            sl = slice(tg * TG, (tg + 1) * TG)
            b = self.PB[tg % 2]
            for kc in range(8):
                self.mm(b.ap[0:16, :], tg_.ap[:, kc * 16:(kc + 1) * 16], self.hT[kc].ap[:, sl], kc == 0, kc == 7,
                        [tg_.buf, self.hT[kc].buf], [b.buf])
            self.act(glr.ap[0:16, sl], b.ap[0:16, :], AF.Copy, [b.buf], [glr.buf])
        it = 0
        for m in range(2):
            for tg in range(NTG):
                sl = slice(tg * TG, (tg + 1) * TG)
                b = self.PB[2 + tg % 2]
                self.mm(b.ap, wg2.ap[0:16, m * 128:(m + 1) * 128], glr.ap[0:16, sl], True, True, [wg2.buf, glr.buf], [b.buf])
                e_ = et[tg % 2]
                self.act(e_.ap, b.ap, AF.Exp, [b.buf, nbg.buf], [e_.buf], bias=nbg.ap[:, m:m + 1], scale=-1.0)
                self.act(spb.ap[:, sl], e_.ap, AF.Ln, [e_.buf, cb], [spb.buf], bias=self.cst(CS_ONE))
            P.add("dve", lambda e, o=Cc.ap, a=rmask.ap, x=spb.ap: e.tensor_tensor_scan(out=o, data0=a, data1=x, initial=0.0,
                                                                                      op0=ALU.mult, op1=ALU.add),
                  [rmask.buf, spb.buf], [Cc.buf])
            Cv = Cc.ap.rearrange("p (c t) -> p c t", t=64)
            self.act(dec.ap[:, m * 32:(m + 1) * 32], Cv[:, :, 63], AF.Exp, [Cc.buf], [dec.buf], scale=-1.0 / 16)
            tq = self.ws.next(("k", "w_in", l, None, 0, 128, 8, ((C_QD + m * 128, 128),)), 1024)
            for tg in range(NTG):
                sl = slice(tg * TG, (tg + 1) * TG)
                b = self.PB[4 + tg % 2]
                for kc in range(8):
                    self.mm(b.ap, tq.ap[:, kc * 128:(kc + 1) * 128], self.hT[kc].ap[:, sl], kc == 0, kc == 7,
                            [tq.buf, self.hT[kc].buf], [b.buf])
                e_ = et[it % 3]
                it += 1
                self.act(e_.ap, Cc.ap[:, sl], AF.Exp, [Cc.buf], [e_.buf], scale=-1.0 / 16)
                self.stt(qin[m].ap[:, sl], b.ap, 0.125, e_.ap, ALU.mult, ALU.mult, [b.buf, e_.buf], [qin[m].buf])
            tk = self.ws.next(("k", "w_in", l, None, 0, 128, 8, ((C_KD + m * 128, 128),)), 1024)
            for tg in range(NTG):
                sl = slice(tg * TG, (tg + 1) * TG)
                b = self.PB[6 + tg % 2]
                for kc in range(8):
                    self.mm(b.ap, tk.ap[:, kc * 128:(kc + 1) * 128], self.hT[kc].ap[:, sl], kc == 0, kc == 7,
                            [tk.buf, self.hT[kc].buf], [b.buf])
                e_ = et[it % 3]
                it += 1
                self.act(e_.ap, Cc.ap[:, sl], AF.Exp, [Cc.buf], [e_.buf], scale=1.0 / 16)
                self.tt(kin[m].ap[:, sl], b.ap, e_.ap, ALU.mult, [b.buf, e_.buf], [kin[m].buf])
                e2 = et[it % 3]
                it += 1
                Cs = Cc.ap[:, sl].rearrange("p (c t) -> p c t", t=64)
                self.tt(e2.ap.rearrange("p (c t) -> p c t", t=64), Cs[:, :, 63:64].to_broadcast([128, 8, 64]), Cs, ALU.subtract,
                        [Cc.buf], [e2.buf])
                self.act(e2.ap, e2.ap, AF.Exp, [e2.buf], [e2.buf], scale=-1.0 / 16)
                ks = kstT[tg % 2]
                self.tt(ks.ap, b.ap, e2.ap, ALU.mult, [b.buf, e2.buf], [ks.buf])
                bt = self.PB[tg % 2]
                for i in range(4):
                    self.mm(bt.ap[:, i * 128:(i + 1) * 128], ks.ap[:, i * 128:(i + 1) * 128], self.ident_bf.ap, True, True,
                            [ks.buf, self.ident_bf.buf], [bt.buf])
                self.act(ksv[:, tg * 4:(tg + 1) * 4, m * 128:(m + 1) * 128], bt.ap.rearrange("p (n c) -> p n c", c=128), AF.Copy,
                         [bt.buf], [ksttok.buf])
        tvs = [self.ws.next(("k", "w_in", l, None, 0, 128, 8, ((C_VD + hf * 256, 256),)), 2048) for hf in range(2)]
        for n in range(16):
            bank = self.PB[2 + n % 2]
            for hf in range(2):
                for kc in range(8):
                    self.mm(bank.ap[:, hf * 256:(hf + 1) * 256], self.hT[kc].ap[:, n * 128:(n + 1) * 128],
                            tvs[hf].ap[:, kc * 256:(kc + 1) * 256], kc == 0, kc == 7,
                            [tvs[hf].buf, self.hT[kc].buf], [bank.buf])
            self.act(vtv[:, n, :], bank.ap, AF.Copy, [bank.buf], [vtok.buf])
        self.tap(f"gla_qin{l}", qin)
        self.tap(f"gla_kin{l}", kin)
        P.barrier()
        self.memset(Sf.ap, 0.0, [Sf.buf])
        self.memset(Sb_.ap, 0.0, [Sb_.buf])
        Sfv = Sf.ap.rearrange("p (m v) -> p m v", v=128)
        Sbv = Sb_.ap.rearrange("p (m v) -> p m v", v=128)
        gm = self.gmask_bf.ap.unsqueeze(1).to_broadcast([128, 4, 128])
        ab = [self.PB[0], self.PB[1]]
        gm2 = self.gmask_bf.ap.unsqueeze(1).to_broadcast([128, 2, 128])
        oglv = ogl_all.ap.rearrange("p (h t) -> p h t", t=S)

        def emitA(n):
            nsl = slice(n * 128, (n + 1) * 128)
            for h in range(4):
                m, po, hs = h // 2, (h % 2) * 64, (h // 2) * 128
                self.mm(ab[h % 2].ap[:, hs:hs + 128], kin[m].ap[po:po + 64, nsl], qin[m].ap[po:po + 64, nsl], True, True,
                        [kin[m].buf, qin[m].buf], [ab[h % 2].buf])

        def emitK(c):
            n, tp = c // 2, (c % 2) * 64
            kvb = self.PB[6 + c % 2]
            for h in range(4):
                m, po = h // 2, (h % 2) * 64
                self.mm(kvb.ap[po:po + 64, m * 128:(m + 1) * 128], ksv[tp:tp + 64, n, h * 64:(h + 1) * 64],
                        vtv[tp:tp + 64, n, h * 128:(h + 1) * 128], True, True, [ksttok.buf, vtok.buf], [kvb.buf])

        emitA(0)
        emitK(0)
        emitK(1)
        for n in range(16):
            ob = [self.PB[2 + 2 * (n % 2)], self.PB[3 + 2 * (n % 2)]]
            nsl = slice(n * 128, (n + 1) * 128)
            am = attm[n % 2]
            amv = am.ap.rearrange("p (h t) -> p h t", t=128)
            for par in range(2):
                self.tt(amv[:, par::2, :], ab[par].ap[:, 0:256].rearrange("p (h t) -> p h t", t=128), gm2, ALU.mult,
                        [ab[par].buf, self.gmask_bf.buf], [am.buf])
            for h in range(4):
                hs = (h // 2) * 128
                self.mm(ob[h % 2].ap[:, hs:hs + 128], vtv[:, n, h * 128:(h + 1) * 128], am.ap[:, h * 128:(h + 1) * 128],
                        h < 2, False, [vtok.buf, am.buf], [ob[h % 2].buf], skip=True)
            if n + 1 < 16:
                emitA(n + 1)
            for cc in range(2):
                c = 2 * n + cc
                csl = slice(c * 64, (c + 1) * 64)
                for h in range(4):
                    m, po, hs = h // 2, (h % 2) * 64, (h // 2) * 128
                    self.mm(ob[h % 2].ap[:, hs + cc * 64: hs + cc * 64 + 64], Sbv[po:po + 64, m, :], qin[m].ap[po:po + 64, csl],
                            False, (cc == 1 and h >= 2), [Sb_.buf, qin[m].buf], [ob[h % 2].buf], skip=True)
                kvb = self.PB[6 + c % 2]
                for m in range(2):
                    self.stt(Sfv[:, m, :], Sfv[:, m, :], dec.ap[:, m * 32 + c: m * 32 + c + 1], kvb.ap[:, m * 128:(m + 1) * 128],
                             ALU.mult, ALU.add, [Sf.buf, dec.buf, kvb.buf], [Sf.buf])
                self.act(Sb_.ap, Sf.ap, AF.Copy, [Sf.buf], [Sb_.buf])
                if c + 2 < 32:
                    emitK(c + 2)
            for par in range(2):
                self.act(oglv[:, par::2, nsl], ob[par].ap[:, 0:256].rearrange("p (h t) -> p h t", t=128), AF.Copy,
                         [ob[par].buf], [ogl_all.buf])
        self.tap(f"gla_ogl{l}", ogl)
        P.barrier()
        o_d = [A.bf16(vtok.off + h * (S // 2), S, f"od{h}") for h in range(4)]
        it = 0
        for h in range(4):
            to = self.ws.next(("k", "w_in", l, None, 0, 128, 8, ((C_OG + h * 128, 128),)), 1024)
            for tg in range(NTG):
                sl = slice(tg * TG, (tg + 1) * TG)
                s = sq[it % 2]
                bq = self.PB[it % 2]
                bo = self.PB[2 + it % 2]
                it += 1
                self.act(s.ap, ogl[h].ap[:, sl], AF.Square, [ogl[h].buf], [s.buf])
                self.mm(bq.ap, self.ones_bf.ap, s.ap, True, True, [self.ones_bf.buf, s.buf], [bq.buf])
                for kc in range(8):
                    self.mm(bo.ap, to.ap[:, kc * 128:(kc + 1) * 128], self.hT[kc].ap[:, sl], kc == 0, kc == 7,
                            [to.buf, self.hT[kc].buf], [bo.buf])
                self.act(rt.ap, bq.ap, AF.Ln, [bq.buf, cb], [rt.buf], bias=self.cst(CS_EPS), scale=1.0 / 128)
                self.act(rstd.ap, rt.ap, AF.Exp, [rt.buf], [rstd.buf], scale=-0.5)
                self.act(sg.ap, bo.ap, AF.Silu, [bo.buf], [sg.buf])
                self.stt(tq_.ap, ogl[h].ap[:, sl], self.ppc(l, "gon", h), rstd.ap, ALU.mult, ALU.mult,
                         [ogl[h].buf, rstd.buf, ppb], [tq_.buf])
                self.tt(o_d[h].ap[:, sl], tq_.ap, sg.ap, ALU.mult, [tq_.buf, sg.buf], [o_d[h].buf])
        return o_d, free


_CACHE = {}


def _get_kern(**kw):
    key = tuple(sorted((k, str(v)) for k, v in kw.items()))
    if key not in _CACHE:
        _CACHE[key] = Kern(**kw)
    return _CACHE[key]


def _pack_shared(kern, inp):
    wpack = np.zeros((128, kern.wtot), np.float32)
    for key, (off, n) in kern.tile_table.items():
        t = _host_tile(inp, key)
        assert t.shape == (128, n), (key, t.shape, n)
        wpack[:, off:off + n] = t
    pp = np.concatenate([_host_params(inp, l) for l in range(DEPTH)], axis=1)
    rows = np.stack([np.stack([inp["gmlp_v_norm"][l].reshape(512),
                               inp["gmlp_b_s"][l].reshape(512)]) for l in range(DEPTH)]).astype(np.float32)
    return {
        "wpack": wpack,
        "pp": np.ascontiguousarray(pp, dtype=np.float32),
        "cst": _host_consts(),
        "rmask": _host_resetmask(),
        "rows": np.ascontiguousarray(rows),
        "wg2": np.ascontiguousarray(inp["gla_w_g2"], dtype=np.float32),
    }


def _core_inputs(inp, b, shared):
    xT = np.ascontiguousarray(np.asarray(inp["x"][b], dtype=np.float32).T.reshape(8, 128, S))
    d = dict(shared)
    d["xT"] = xT
    d["pos"] = np.ascontiguousarray(np.asarray(inp["positions"][b], dtype=np.int32).reshape(1, S))
    return d


def kernel(**inputs):
    inp = {k: np.asarray(v) for k, v in inputs.items()}
    kern = _get_kern()
    shared = _pack_shared(kern, inp)
    in_maps = [_core_inputs(inp, b, shared) for b in range(NCORE)]
    res = run_bass_kernel_spmd(kern.nc, in_maps, core_ids=list(range(NCORE)))
    out = np.empty((NCORE, S, D), np.float32)
    for b in range(NCORE):
        out[b] = res.results[b]["outT"].reshape(D, S).T
    return out
```

```python
import numpy as np
from contextlib import ExitStack
import concourse.bass as bass
import concourse.mybir as mybir
from concourse.bass_utils import run_bass_kernel_spmd

F32 = mybir.dt.float32
BF16 = mybir.dt.bfloat16
I32 = mybir.dt.int32
AF = mybir.ActivationFunctionType
ALU = mybir.AluOpType

D = 1024
S = 2048
DEPTH = 4
NCORE = 8
TG = 512
NTG = 4
IN_COLS = 8368
FFN = 2816
EPS = 1e-6

C_ZA = 0
C_YB = 1024
C_UB = 1536
C_CQ = 2048
C_CKV = 2432
C_KR = 2688
C_QD = 2720
C_KD = 2976
C_VD = 3232
C_GD = 3744
C_OG = 3760
C_GATE = 4272

PP = {}
_o = 0
for _n, _w in [("nmp", 8), ("nmo", 8), ("nfp", 8), ("nfo", 8), ("lcw", 16), ("lcb", 4), ("lba", 4), ("lbi", 4),
               ("llam", 4), ("mqn", 3), ("mkn", 2), ("gbg", 2), ("gon", 4), ("fcw", 132), ("fcb", 44)]:
    PP[_n] = _o
    _o += _w
NPP = _o

CS_IDENT = 0
CS_ONES = 128
CS_GMASK = 256
CS_INVF = 384
CS_SIGN = 385
CS_EPS = 386
CS_ONE = 387
CS_HALFPI = 388
CS_ZERO = 389
NCST = 392


def _host_consts():
    c = np.zeros((128, NCST), np.float32)
    c[:, CS_IDENT:CS_IDENT + 128] = np.eye(128, dtype=np.float32)
    c[:, CS_ONES:CS_ONES + 128] = 1.0
    s = np.arange(128)[:, None]
    t = np.arange(128)[None, :]
    c[:, CS_GMASK:CS_GMASK + 128] = ((s // 64 == t // 64) & (s <= t)).astype(np.float32)
    invf = (10000.0 ** (-np.arange(0, 32, 2, dtype=np.float32) / np.float32(32))).astype(np.float32)
    for p in range(64, 96):
        c[p, CS_INVF] = invf[(p - 64) % 16]
        c[p, CS_SIGN] = -1.0 if p < 80 else 1.0
    c[:, CS_EPS] = EPS
    c[:, CS_ONE] = 1.0
    c[:, CS_HALFPI] = np.float32(np.pi / 2)
    return c


def _host_resetmask():
    m = np.ones((128, S), np.float32)
    m[:, 0::64] = 0.0
    return m


def _host_params(inp, l):
    p = np.zeros((128, NPP), np.float32)

    def put(name, arr):
        p[:, PP[name]:PP[name] + arr.shape[1]] = arr
    put("nmp", inp["norm_mix_pre"][l].reshape(8, 128).T)
    put("nmo", inp["norm_mix_post"][l].reshape(8, 128).T)
    put("nfp", inp["norm_ffn_pre"][l].reshape(8, 128).T)
    put("nfo", inp["norm_ffn_post"][l].reshape(8, 128).T)
    put("lcw", inp["lru_conv_w"][l].reshape(4, 4, 128).transpose(2, 1, 0).reshape(128, 16))
    put("lcb", inp["lru_conv_b"][l].reshape(4, 128).T)
    put("lba", inp["lru_b_a"][l].reshape(4, 128).T)
    put("lbi", inp["lru_b_i"][l].reshape(4, 128).T)
    put("llam", inp["lru_lambda"][l].reshape(4, 128).T)
    put("mqn", inp["mla_q_norm"][l].reshape(3, 128).T)
    put("mkn", inp["mla_kv_norm"][l].reshape(2, 128).T)
    put("gbg", inp["gla_b_g"][l].reshape(2, 128).T)
    put("gon", inp["gla_o_norm"][l].reshape(4, 128).T)
    put("fcw", inp["ffn_conv_w"][l].reshape(3, 44, 128).transpose(2, 1, 0).reshape(128, 132))
    put("fcb", inp["ffn_conv_b"][l].reshape(44, 128).T)
    return p


def _host_tile(inp, key):
    kind = key[0]
    if kind == "k":
        _, name, l, sub, row0, pn, kc, cols = key
        w = inp[name][l]
        if sub is not None:
            w = w[sub]
        idx = np.concatenate([np.arange(s, s + n) for (s, n) in cols])
        blk = w[row0:row0 + kc * pn][:, idx]
        t = blk.reshape(kc, pn, len(idx)).transpose(1, 0, 2).reshape(pn, kc * len(idx))
        if pn < 128:
            t = np.concatenate([t, np.zeros((128 - pn, t.shape[1]), np.float32)], 0)
        return np.ascontiguousarray(t, dtype=np.float32)
    if kind == "blk":
        _, name, l = key
        return np.ascontiguousarray(inp[name][l].transpose(1, 0, 2).reshape(128, 512), dtype=np.float32)
    if kind == "blkT":
        _, name, l = key
        return np.ascontiguousarray(inp[name][l].transpose(2, 0, 1).reshape(128, 512), dtype=np.float32)
    raise ValueError(key)


class Buf:
    __slots__ = ("name", "writers", "readers", "excl", "lo", "hi", "live")

    def __init__(self, name="", excl=False, lo=None, hi=None):
        self.name = name
        self.excl = excl
        self.lo = lo
        self.hi = hi
        self.live = lo is None
        self.writers = {}
        self.readers = {}


class DSem:
    __slots__ = ("h", "count", "last")

    def __init__(self, h):
        self.h = h
        self.count = 0
        self.last = None


class Op:
    __slots__ = ("eng", "fn", "deps", "is_dma", "idx", "sem", "tick", "signal", "dsem")


ENGS = ["pe", "act", "dve", "pool", "sp"]
SEM_LIMIT = 12000
SAME_ENG_SYNC = True
import os as _os
USE_BARRIERS = bool(int(_os.environ.get('KERN_BARRIERS', '0')))


class Prog:
    def __init__(self, nc, es):
        self.nc = nc
        self.es = es
        self.ops = []
        self.dry = False
        self.nsem = 0
        self.last_op = {}
        self.dmas_since_bar = []
        self.bar_deps = []
        self.bar_pending = set()
        self.live = []

    def touch(self, b):
        if b.live:
            return
        keep = []
        for o in self.live:
            if o.lo < b.hi and b.lo < o.hi:
                for k, op in o.writers.items():
                    if k not in b.writers or b.writers[k].idx < op.idx:
                        b.writers[k] = op
                for k, op in o.readers.items():
                    if k not in b.readers or b.readers[k].idx < op.idx:
                        b.readers[k] = op
                o.writers = {}
                o.readers = {}
                o.live = False
            else:
                keep.append(o)
        keep.append(b)
        b.live = True
        self.live = keep

    def new_sem(self, name="s"):
        self.nsem += 1
        return self.es.enter_context(self.nc.semaphore(f"{name}{self.nsem}"))

    def new_dsem(self):
        return DSem(self.new_sem("d"))

    def barrier(self, force=False):
        if self.dry or not (force or USE_BARRIERS):
            return
        deps = list(self.last_op.values()) + self.dmas_since_bar
        if self.bar_pending:
            deps += self.bar_deps
        self.bar_deps = deps
        self.bar_pending = set(ENGS)
        self.dmas_since_bar = []

    def add(self, eng, fn, reads=(), writes=(), dma=False, dsem=None):
        if self.dry:
            return
        op = Op()
        op.eng = eng
        op.fn = fn
        op.is_dma = dma
        op.idx = len(self.ops)
        op.signal = False
        op.sem = None
        op.tick = 0
        op.dsem = dsem
        key = ("d", op.idx) if dma else eng
        deps = {}
        for b in reads:
            self.touch(b)
        for b in writes:
            self.touch(b)
        ex = [b for b in reads if b.excl and b not in writes]
        if ex:
            writes = list(writes) + ex
            reads = [b for b in reads if not b.excl]

        def need(o):
            if o.is_dma or dma or o.eng != eng or (SAME_ENG_SYNC and eng != "pe"):
                deps[o.idx] = o
        for b in reads:
            for o in b.writers.values():
                need(o)
        for b in writes:
            for o in b.writers.values():
                need(o)
            for o in b.readers.values():
                need(o)
        if eng in self.bar_pending:
            for o in self.bar_deps:
                need(o)
            self.bar_pending.discard(eng)
        if dma:
            assert dsem is not None
            if dsem.last is not None:
                need(dsem.last)
            dsem.last = op
            dsem.count += 1
            op.sem = dsem.h
            op.tick = 16 * dsem.count
            self.dmas_since_bar.append(op)
        for b in writes:
            b.writers = {key: op}
            b.readers = {}
        for b in reads:
            if b not in writes:
                b.readers[key] = op
        op.deps = list(deps.values())
        if not dma:
            self.last_op[eng] = op
        self.ops.append(op)
        return op

    def emit(self, block):
        for op in self.ops:
            for d in op.deps:
                d.signal = True
        cur = {}
        cnt = {}
        for op in self.ops:
            if op.is_dma or not op.signal:
                continue
            e = op.eng
            if e not in cur or cnt[e] >= SEM_LIMIT:
                cur[e] = self.new_sem(e)
                cnt[e] = 0
            cnt[e] += 1
            op.sem = cur[e]
            op.tick = cnt[e]
        import os
        mx = int(os.environ.get("KERN_MAXOPS", "0"))
        ops_all = self.ops[:mx] if mx > 0 else self.ops
        if os.environ.get("KERN_DUMP"):
            for o in ops_all:
                print("OP", o.idx, o.eng, "dma" if o.is_dma else "", "deps", [d.idx for d in o.deps], flush=True)
        streams = {e: [o for o in ops_all if o.eng == e] for e in ENGS}

        def run(eng_handle, ops):
            waited = {}
            for op in ops:
                for d in op.deps:
                    k = id(d.sem)
                    if waited.get(k, 0) < d.tick:
                        eng_handle.wait_ge(d.sem, d.tick)
                        waited[k] = d.tick
                if op.fn is None:
                    continue
                ins = op.fn(eng_handle)
                if op.is_dma:
                    ins.then_inc(op.sem, 16)
                elif op.signal:
                    ins.then_inc(op.sem, 1)

        @block.tensor
        def _(e):
            run(e, streams["pe"])

        @block.scalar
        def _(e):
            run(e, streams["act"])

        @block.vector
        def _(e):
            run(e, streams["dve"])

        @block.gpsimd
        def _(e):
            run(e, streams["pool"])

        @block.sync
        def _(e):
            run(e, streams["sp"])


class T:
    __slots__ = ("ap", "buf", "off", "words")

    def __init__(self, ap, buf=None, name="", off=None, words=None):
        self.ap = ap
        self.off = off
        self.words = words
        if buf is None:
            buf = Buf(name, lo=off, hi=(off + words) if off is not None else None)
        self.buf = buf


class Arena:
    def __init__(self, ap_f32, total_words):
        self.a = ap_f32
        self.total = total_words

    def f32(self, off, n, name=""):
        assert off + n <= self.total, (off, n, self.total)
        return T(self.a[:, off:off + n], name=name, off=off, words=n)

    def bf16(self, off_words, n_bf, name=""):
        assert n_bf % 2 == 0 and off_words + n_bf // 2 <= self.total, (off_words, n_bf, self.total)
        return T(self.a[:, off_words:off_words + n_bf // 2].bitcast(BF16), name=name, off=off_words, words=n_bf // 2)

    def i32(self, off, n, name=""):
        return T(self.a[:, off:off + n].bitcast(I32), name=name, off=off, words=n)


class Bump:
    def __init__(self, arena, regions):
        self.arena = arena
        self.regions = [list(r) for r in regions]

    def _take(self, words):
        for r in self.regions:
            if r[1] - r[0] >= words:
                off = r[0]
                r[0] += words
                return off
        raise MemoryError(f"bump alloc {words} words; regions {self.regions}")

    def f32(self, n, name=""):
        return self.arena.f32(self._take(n), n, name)

    def bf16(self, n, name=""):
        return self.arena.bf16(self._take((n + 1) // 2), n, name)


class WStream:
    NSLOT = 4
    KEEP = 2
    SLOT_WORDS = 1024

    def __init__(self, P, arena, off_words, wpack_ap_getter):
        self.P = P
        self.seq = []
        self.tiles = {}
        self.tot = 0
        self.pos = 0
        self.issued = 0
        self.slots = [arena.bf16(off_words + i * self.SLOT_WORDS, 2 * self.SLOT_WORDS, f"wslot{i}") for i in range(self.NSLOT)]
        self.sems = None
        self.get_wpack = wpack_ap_getter

    def reset_real(self):
        self.pos = 0
        self.issued = 0
        self.sems = [self.P.new_dsem() for _ in range(self.NSLOT)]

    def _issue(self, i):
        key, n = self.seq[i]
        off = self.tiles[key][0]
        slot = i % self.NSLOT
        st = self.slots[slot]
        src = self.get_wpack()[:, off:off + n]
        dst = st.ap[:, 0:n]
        self.P.add("pool", lambda e, dst=dst, src=src: e.dma_start(out=dst, in_=src), reads=[], writes=[st.buf],
                   dma=True, dsem=self.sems[slot])

    def next(self, key, n):
        assert n <= 2 * self.SLOT_WORDS
        if self.P.dry:
            self.seq.append((key, n))
            if key not in self.tiles:
                self.tiles[key] = (self.tot, n)
                self.tot += n
            return self.slots[0]
        assert self.seq[self.pos] == (key, n), (self.pos, self.seq[self.pos], key, n)
        hi = min(len(self.seq), self.pos + self.NSLOT - self.KEEP + 1)
        while self.issued < hi:
            self._issue(self.issued)
            self.issued += 1
        st = self.slots[self.pos % self.NSLOT]
        self.pos += 1
        return st


class Kern:
    def __init__(self, n_layers=DEPTH, taps=(), stop_after=None, mixers=("mla", "gmlp", "lru", "gla")):
        self.n_layers = n_layers
        self.taps = set(taps)
        self.stop_after = stop_after
        self.mixers = mixers
        self.nc = bass.Bass("TRN2", target_bir_lowering=False)
        self.tap_names = []
        self._build()

    def mm(self, out, lhsT, rhs, start, stop, reads, writes, skip=False):
        self.P.add("pe", lambda e: e.matmul(out, lhsT, rhs, start=start, stop=stop, skip_group_check=skip),
                   reads, writes)

    def act(self, out, in_, func, reads, writes, bias=None, scale=None, accum=None):
        kw = {}
        if bias is not None:
            kw["bias"] = bias
        if scale is not None:
            kw["scale"] = scale
        if accum is not None:
            kw["accum_out"] = accum
        self.P.add("act", lambda e: e.activation(out=out, in_=in_, func=func, **kw), reads, writes)

    def ts(self, out, in0, s1, s2, op0, op1, reads, writes, eng="dve"):
        if op1 is None:
            self.P.add(eng, lambda e: e.tensor_scalar(out=out, in0=in0, scalar1=s1, scalar2=None, op0=op0), reads, writes)
        else:
            self.P.add(eng, lambda e: e.tensor_scalar(out=out, in0=in0, scalar1=s1, scalar2=s2, op0=op0, op1=op1), reads, writes)

    def tt(self, out, in0, in1, op, reads, writes, eng="dve"):
        self.P.add(eng, lambda e: e.tensor_tensor(out=out, in0=in0, in1=in1, op=op), reads, writes)

    def stt(self, out, in0, scalar, in1, op0, op1, reads, writes):
        self.P.add("dve", lambda e: e.scalar_tensor_tensor(out=out, in0=in0, scalar=scalar, in1=in1, op0=op0, op1=op1),
                   reads, writes)

    def cp(self, out, in_, reads, writes, eng="dve"):
        self.P.add(eng, lambda e: e.tensor_copy(out=out, in_=in_), reads, writes)

    def recip(self, out, in_, reads, writes):
        self.P.add("dve", lambda e: e.reciprocal(out=out, in_=in_), reads, writes)

    def memset(self, ap, val, writes, eng="dve"):
        self.P.add(eng, lambda e: e.memset(ap, val), [], writes)

    def dma(self, out, in_, reads, writes, dsem, q="sp"):
        self.P.add(q, lambda e: e.dma_start(out=out, in_=in_), reads, writes, dma=True, dsem=dsem)

    def cst(self, col, p0=0, p1=128):
        return self.cst_t.ap[p0:p1, col:col + 1]

    def ppc(self, l, name, idx=0, p0=0, p1=128):
        c = l * NPP + PP[name] + idx
        return self.pp_t.ap[p0:p1, c:c + 1]

    def tap(self, name, tiles, width=S):
        if name not in self.taps or self.P.dry:
            return
        n = len(tiles)
        dt = self.nc.dram_tensor(name, [n * 128, width], F32, kind="ExternalOutput").ap()
        self.tap_names.append(name)
        self.P.barrier(force=True)
        stg = [self.arena.f32(self.r3_free + i * 512, 512, f"tapstg{i}") for i in range(2)]
        k = 0
        for i, t in enumerate(tiles):
            for c0 in range(0, width, 512):
                s_ = stg[k % 2]
                k += 1
                self.cp(s_.ap, t.ap[:, c0:c0 + 512], [t.buf], [s_.buf])
                ob = Buf("tapout")
                self.outbufs.append(ob)
                self.dma(dt[i * 128:(i + 1) * 128, c0:c0 + 512], s_.ap, [s_.buf], [ob], self.msem())
        self.P.barrier(force=True)

    def _build(self):
        nc = self.nc
        self.x_d = nc.dram_tensor("xT", [8, 128, S], F32, kind="ExternalInput").ap()
        self.pos_d = nc.dram_tensor("pos", [1, S], I32, kind="ExternalInput").ap()
        self.pp_d = nc.dram_tensor("pp", [128, DEPTH * NPP], F32, kind="ExternalInput").ap()
        self.cst_d = nc.dram_tensor("cst", [128, NCST], F32, kind="ExternalInput").ap()
        self.rm_d = nc.dram_tensor("rmask", [128, S], F32, kind="ExternalInput").ap()
        self.rows_d = nc.dram_tensor("rows", [DEPTH, 2, 512], F32, kind="ExternalInput").ap()
        self.wg2_d = nc.dram_tensor("wg2", [DEPTH, 16, 256], F32, kind="ExternalInput").ap()
        self.out_d = nc.dram_tensor("outT", [8, 128, S], F32, kind="ExternalOutput").ap()
        self.xsp_d = nc.dram_tensor("xspill", [8, 128, S], F32, kind="Internal").ap()
        self.wpack_d = None

        es0 = ExitStack()
        self.P = Prog(nc, es0)
        self.P.dry = True
        self._alloc(es0, dry=True)
        self._program()
        seq, tiles, tot = self.ws.seq, self.ws.tiles, self.ws.tot
        taps_dry = list(self.tap_names)
        es0.close()
        self.tile_table = tiles
        self.wtot = tot
        self.tap_names = []

        tot = max(tot, 64)
        self.wtot = tot
        self.wpack_d = nc.dram_tensor("wpack", [128, tot], F32, kind="ExternalInput").ap()
        with ExitStack() as es:
            self.P = Prog(nc, es)
            self._alloc(es, dry=False)
            self.ws.seq, self.ws.tiles, self.ws.tot = seq, tiles, tot
            self.ws.reset_real()
            self._program()
            assert self.ws.pos == len(seq), (self.ws.pos, len(seq))
            block = es.enter_context(nc.Block())
            self.P.emit(block)
        self.n_ops = len(self.P.ops)

    ARENA_WORDS = 47 * 1024
    R0 = 0
    R1 = 16 * 1024
    R2 = 24 * 1024
    R3 = 40 * 1024

    def _alloc(self, es, dry):
        nc = self.nc
        if dry and hasattr(self, "_arena_dry"):
            pass
        at = es.enter_context(nc.sbuf_tensor("arena" + ("d" if dry else ""), [128, self.ARENA_WORDS], F32))
        self.arena = Arena(at[:, :], self.ARENA_WORDS)
        A = self.arena
        ps = [es.enter_context(nc.psum_tensor(f"ps{i}" + ("d" if dry else ""), [128, 1024], F32)) for i in range(4)]
        self.PS2 = [T(p[:, :], name=f"ps2_{i}") for i, p in enumerate(ps)]
        self.PB = []
        for i, p in enumerate(ps):
            self.PB.append(T(p[:, 0:512], buf=Buf(f"pb{2 * i}", excl=True)))
            self.PB.append(T(p[:, 512:1024], buf=Buf(f"pb{2 * i + 1}", excl=True)))
        o = self.R3
        self.ws = WStream(self.P, A, o, lambda: self.wpack_d)
        o += WStream.NSLOT * WStream.SLOT_WORDS
        self.pp_t = A.f32(o, DEPTH * NPP, "pp")
        o += DEPTH * NPP
        self.cst_t = A.f32(o, NCST, "cst")
        o += NCST
        self.ident_bf = A.bf16(o, 128, "ident")
        o += 64
        self.ones_bf = A.bf16(o, 128, "ones")
        o += 64
        self.gmask_bf = A.bf16(o, 128, "gmask")
        o += 64
        self.carry = A.f32(o, 88, "carry")
        o += 88
        self.lrud = A.f32(o, 16, "lrud")
        o += 16
        self.TAP_OFF = None
        self.r3_free = o
        assert o <= self.ARENA_WORDS, o
        self.xT = [A.f32(self.R0 + m * S, S, f"xT{m}") for m in range(8)]
        self.hT = [A.bf16(self.R1 + m * (S // 2), S, f"hT{m}") for m in range(8)]
        self.acc = [A.bf16(self.R2 + m * (S // 2), S, f"acc{m}") for m in range(8)]
        self.misc_sems = [self.P.new_dsem() for _ in range(6)] if not dry else [None] * 6
        self.misc_i = 0
        self.outbufs = []

    def msem(self):
        s = self.misc_sems[self.misc_i % len(self.misc_sems)]
        self.misc_i += 1
        return s

    def _program(self):
        P = self.P
        A = self.arena
        self.dma(self.pp_t.ap, self.pp_d, [], [self.pp_t.buf], self.msem())
        self.dma(self.cst_t.ap, self.cst_d, [], [self.cst_t.buf], self.msem())
        for m in range(8):
            self.dma(self.xT[m].ap, self.x_d[m], [], [self.xT[m].buf], self.msem())
        c = self.cst_t
        self.cp(self.ident_bf.ap, c.ap[:, CS_IDENT:CS_IDENT + 128], [c.buf], [self.ident_bf.buf])
        self.cp(self.ones_bf.ap, c.ap[:, CS_ONES:CS_ONES + 128], [c.buf], [self.ones_bf.buf])
        self.cp(self.gmask_bf.ap, c.ap[:, CS_GMASK:CS_GMASK + 128], [c.buf], [self.gmask_bf.buf])
        self.carrybufs = [Buf(f"carry{i}") for i in range(44)]

        for l in range(self.n_layers):
            self.layer(l)
            if self.stop_after is not None and self.stop_after[0] == l:
                break
        P.barrier()
        for m in range(8):
            ob = Buf("outb")
            self.outbufs.append(ob)
            self.dma(self.out_d[m], self.xT[m].ap, [self.xT[m].buf], [ob], self.msem())
        P.add("sp", None, reads=list(self.outbufs), writes=[])

    def prenorm(self, l, gname, src, dst, tgs, tmp, dst_off=0):
        sq = [tmp.bf16(512, f"sq{i}") for i in range(8)]
        rt = tmp.f32(512, "rt")
        rstd = tmp.f32(512, "rstd")
        bank = self.PB[7]
        for tg in tgs:
            sl = slice(tg * TG, (tg + 1) * TG)
            dl = slice(tg * TG - dst_off, (tg + 1) * TG - dst_off)
            for m in range(8):
                s = sq[m]
                self.act(s.ap, src[m].ap[:, sl], AF.Square, [src[m].buf], [s.buf])
            for m in range(8):
                s = sq[m]
                self.mm(bank.ap, self.ones_bf.ap, s.ap, m == 0, m == 7, [self.ones_bf.buf, s.buf], [bank.buf])
            self.act(rt.ap, bank.ap, AF.Ln, [bank.buf, self.cst_t.buf], [rt.buf], bias=self.cst(CS_EPS), scale=1.0 / D)
            self.act(rstd.ap, rt.ap, AF.Exp, [rt.buf], [rstd.buf], scale=-0.5)
            for m in range(8):
                self.stt(dst[m].ap[:, dl], src[m].ap[:, sl], self.ppc(l, gname, m), rstd.ap, ALU.mult, ALU.mult,
                         [src[m].buf, rstd.buf, self.pp_t.buf], [dst[m].buf])

    def layer(self, l):
        P = self.P
        A = self.arena
        P.barrier()
        tmp = Bump(A, [[self.R2 + 8192, self.R3]])
        self.prenorm(l, "nmp", self.xT, self.hT, range(NTG), tmp)
        if l > 0:
            for m in range(8):
                self.dma(self.xsp_d[m], self.xT[m].ap, [self.xT[m].buf], [self.xspbuf(m)], self.msem())
        xsrc = self.x_d if l == 0 else self.xsp_d
        self.tap(f"hT{l}", self.hT)
        if self.stop_after == (l, "n1"):
            return
        first = True
        for mx in self.mixers:
            P.barrier()
            tmp = Bump(A, [[self.R0, self.R1], [self.R2 + 8192, self.R3]])
            o_k, free = getattr(self, "mix_" + mx)(l, tmp)
            self.tap(f"o_{mx}{l}", o_k)
            k = {"gmlp": 0, "lru": 1, "mla": 2, "gla": 3}[mx]
            P.barrier()
            self.merge(l, k, o_k, first, Bump(A, free))
            first = False
        self.tap(f"acc{l}", self.acc)
        if self.stop_after == (l, "m"):
            return
        P.barrier()
        self.phase_o(l, xsrc)
        self.tap(f"xo{l}", self.xT)
        if self.stop_after == (l, "o"):
            return
        for hh in range(2):
            P.barrier()
            self.phase_f(l, hh)
        self.tap(f"xf{l}", self.xT)

    def xspbuf(self, m):
        if not hasattr(self, "_xsp"):
            self._xsp = [Buf(f"xsp{m}") for m in range(8)]
        return self._xsp[m]

    def merge(self, l, k, o_k, first, tmp):
        sg = [tmp.f32(512, "sg0"), tmp.f32(512, "sg1")]
        tm = [tmp.f32(512, "tm0"), tmp.f32(512, "tm1")]
        it = 0
        for mp in range(4):
            gt = self.ws.next(("k", "w_in", l, None, 0, 128, 8, ((C_GATE + k * 1024 + mp * 256, 256),)), 2048)
            bt = self.ws.next(("k", "w_branch", l, k, 0, 128, 4, ((mp * 256, 256),)), 1024)
            for mi in range(2):
                m = 2 * mp + mi
                for tg in range(NTG):
                    sl = slice(tg * TG, (tg + 1) * TG)
                    gb = self.PB[(it * 2) % 6]
                    bb = self.PB[(it * 2 + 1) % 6]
                    s_ = sg[it % 2]
                    t_ = tm[it % 2]
                    it += 1
                    for kc in range(8):
                        self.mm(gb.ap, gt.ap[:, kc * 256 + mi * 128: kc * 256 + mi * 128 + 128], self.hT[kc].ap[:, sl],
                                kc == 0, kc == 7, [gt.buf, self.hT[kc].buf], [gb.buf])
                    for kc in range(4):
                        self.mm(bb.ap, bt.ap[:, kc * 256 + mi * 128: kc * 256 + mi * 128 + 128], o_k[kc].ap[:, sl],
                                kc == 0, kc == 3, [bt.buf, o_k[kc].buf], [bb.buf])
                    self.act(s_.ap, gb.ap, AF.Sigmoid, [gb.buf], [s_.buf])
                    if first:
                        self.tt(self.acc[m].ap[:, sl], s_.ap, bb.ap, ALU.mult, [s_.buf, bb.buf], [self.acc[m].buf])
                    else:
                        self.tt(t_.ap, s_.ap, bb.ap, ALU.mult, [s_.buf, bb.buf], [t_.buf])
                        self.tt(self.acc[m].ap[:, sl], self.acc[m].ap[:, sl], t_.ap, ALU.add,
                                [self.acc[m].buf, t_.buf], [self.acc[m].buf], eng="pool")

    def phase_o(self, l, xsrc):
        A = self.arena
        tmp = Bump(A, [[self.R2 + 8192, self.R3]])
        mix = self.xT
        xo = [[A.f32(self.R2 + 8192 + b * 4096 + m * 512, 512, f"xo{b}_{m}") for m in range(8)] for b in range(2)]
        tmp = Bump(A, [[self.R1, self.R2]])
        it = 0
        for mp in range(4):
            wt = self.ws.next(("k", "w_out", l, None, 0, 128, 8, ((mp * 256, 256),)), 2048)
            for mi in range(2):
                m = 2 * mp + mi
                for tg in range(NTG):
                    sl = slice(tg * TG, (tg + 1) * TG)
                    b = self.PB[it % 6]
                    it += 1
                    for kc in range(8):
                        self.mm(b.ap, wt.ap[:, kc * 256 + mi * 128: kc * 256 + mi * 128 + 128], self.acc[kc].ap[:, sl],
                                kc == 0, kc == 7, [wt.buf, self.acc[kc].buf], [b.buf])
                    self.act(mix[m].ap[:, sl], b.ap, AF.Copy, [b.buf], [mix[m].buf])
        self.postnorm(l, "nmo", mix, range(NTG), tmp, xsrc, xo)

    def postnorm(self, l, gname, f, tgs, tmp, xsrc=None, xo=None, f_off=0, xres=None):
        sq = [tmp.bf16(512, f"sq{i}") for i in range(8)]
        rt = tmp.f32(512, "rt")
        rstd = tmp.f32(512, "rstd")
        bank = self.PB[7]
        for i, tg in enumerate(tgs):
            sl = slice(tg * TG, (tg + 1) * TG)
            fl = slice(tg * TG - f_off, (tg + 1) * TG - f_off)
            if xsrc is not None:
                xb = xo[i % 2]
                for m in range(8):
                    self.dma(xb[m].ap, xsrc[m][:, sl], [self.xspbuf(m)], [xb[m].buf], self.msem())
            for m in range(8):
                s = sq[m]
                self.act(s.ap, f[m].ap[:, fl], AF.Square, [f[m].buf], [s.buf])
            for m in range(8):
                s = sq[m]
                self.mm(bank.ap, self.ones_bf.ap, s.ap, m == 0, m == 7, [self.ones_bf.buf, s.buf], [bank.buf])
            self.act(rt.ap, bank.ap, AF.Ln, [bank.buf, self.cst_t.buf], [rt.buf], bias=self.cst(CS_EPS), scale=1.0 / D)
            self.act(rstd.ap, rt.ap, AF.Exp, [rt.buf], [rstd.buf], scale=-0.5)
            for m in range(8):
                if xsrc is not None:
                    self.stt(f[m].ap[:, fl], f[m].ap[:, fl], self.ppc(l, gname, m), rstd.ap, ALU.mult, ALU.mult,
                             [f[m].buf, rstd.buf, self.pp_t.buf], [f[m].buf])
                    self.tt(f[m].ap[:, fl], f[m].ap[:, fl], xb[m].ap, ALU.add, [f[m].buf, xb[m].buf], [f[m].buf], eng="pool")
                else:
                    self.stt(f[m].ap[:, fl], f[m].ap[:, fl], self.ppc(l, gname, m), rstd.ap, ALU.mult, ALU.mult,
                             [f[m].buf, rstd.buf, self.pp_t.buf], [f[m].buf])
                    self.tt(xres[m].ap[:, sl], xres[m].ap[:, sl], f[m].ap[:, fl], ALU.add,
                            [xres[m].buf, f[m].buf], [xres[m].buf], eng="pool")

    def phase_f(self, l, hh):
        A = self.arena
        H = 1024
        t0 = hh * H
        h2 = [A.bf16(self.R1 + m * 512, H, f"h2_{m}") for m in range(8)]
        tmpn = Bump(A, [[self.R1 + 4096, self.R2]])
        self.prenorm(l, "nfp", self.xT, h2, [2 * hh, 2 * hh + 1], tmpn, dst_off=t0)
        g = [A.bf16(self.R2 + j * 512, H, f"g{j}") for j in range(22)]
        yo = self.R2 + 22 * 512
        ya = [A.f32(yo + b * 2048, H, f"ya{b}") for b in range(2)]
        yb = [A.f32(yo + b * 2048 + 1024, H, f"yb{b}") for b in range(2)]
        assert yo + 4096 <= self.R3
        for j in range(22):
            wt = self.ws.next(("k", "ffn_w_up", l, None, 0, 128, 8, ((j * 128, 128), (FFN + j * 128, 128))), 2048)
            pa = 2 * (j % 2)
            for ab in range(2):
                for tl in range(2):
                    bank = self.PB[(pa + ab) * 2 + tl]
                    sl = slice(tl * TG, (tl + 1) * TG)
                    for kc in range(8):
                        self.mm(bank.ap, wt.ap[:, kc * 256 + ab * 128: kc * 256 + ab * 128 + 128], h2[kc].ap[:, sl],
                                kc == 0, kc == 7, [wt.buf, h2[kc].buf], [bank.buf])
            ys = [ya[j % 2], yb[j % 2]]
            for ab in range(2):
                jj = j + 22 * ab
                z = self.PS2[pa + ab].ap
                zb = [self.PB[(pa + ab) * 2].buf, self.PB[(pa + ab) * 2 + 1].buf]
                y = ys[ab]
                w = lambda k: self.ppc(l, "fcw", jj * 3 + k)
                self.act(y.ap, z, AF.Identity, zb + [self.pp_t.buf], [y.buf], bias=self.ppc(l, "fcb", jj), scale=w(2))
                if hh == 0:
                    self.act(self.carry.ap[:, jj * 2: jj * 2 + 2], z[:, H - 2:H], AF.Copy, zb, [self.carrybufs[jj]])
                self.stt(y.ap[:, 1:H], z[:, 0:H - 1], w(1), y.ap[:, 1:H], ALU.mult, ALU.add, zb + [y.buf, self.pp_t.buf], [y.buf])
                self.stt(y.ap[:, 2:H], z[:, 0:H - 2], w(0), y.ap[:, 2:H], ALU.mult, ALU.add, zb + [y.buf, self.pp_t.buf], [y.buf])
                cr = self.carry.ap[:, jj * 2: jj * 2 + 2]
                if hh == 1:
                    self.stt(y.ap[:, 0:1], cr[:, 1:2], w(1), y.ap[:, 0:1], ALU.mult, ALU.add,
                             [self.carrybufs[jj], y.buf, self.pp_t.buf], [y.buf])
                    self.stt(y.ap[:, 0:2], cr[:, 0:2], w(0), y.ap[:, 0:2], ALU.mult, ALU.add,
                             [self.carrybufs[jj], y.buf, self.pp_t.buf], [y.buf])
            self.act(ys[0].ap, ys[0].ap, AF.Gelu_apprx_tanh, [ys[0].buf], [ys[0].buf])
            self.tt(g[j].ap, ys[0].ap, ys[1].ap, ALU.mult, [ys[0].buf, ys[1].buf], [g[j].buf], eng="pool")
        f = [A.f32(self.R1 + 4096 + m * 1024, H, f"f{m}") for m in range(4)] + \
            [A.f32(yo + (m - 4) * 1024, H, f"f{m}") for m in range(4, 8)]
        self.P.barrier()
        for m in range(8):
            banks = [self.PB[(m % 2) * 2], self.PB[(m % 2) * 2 + 1]]
            for half in range(2):
                wt = self.ws.next(("k", "ffn_w_down", l, None, half * 11 * 128, 128, 11, ((m * 128, 128),)), 11 * 128)
                for tl in range(2):
                    sl = slice(tl * TG, (tl + 1) * TG)
                    for jc in range(11):
                        j = half * 11 + jc
                        self.mm(banks[tl].ap, wt.ap[:, jc * 128:(jc + 1) * 128], g[j].ap[:, sl],
                                j == 0, j == 21, [wt.buf, g[j].buf], [banks[tl].buf])
            for tl in range(2):
                self.act(f[m].ap[:, tl * TG:(tl + 1) * TG], banks[tl].ap, AF.Copy, [banks[tl].buf], [f[m].buf])
        tmpn = Bump(A, [[self.R1, self.R1 + 4096]])
        self.postnorm(l, "nfo", f, [2 * hh, 2 * hh + 1], tmpn, f_off=t0, xres=self.xT)

    def rope_tables(self, cosT, sinT, tr):
        R = slice(64, 96)
        _pt = tr.f32(S, "posi")
        posi = T(_pt.ap.bitcast(I32), buf=_pt.buf)
        posf = tr.f32(S, "posf")
        ang = tr.f32(S, "ang")
        ta = tr.f32(S, "ta")
        tb = tr.f32(S, "tb")
        self.dma(posi.ap[R, :], self.pos_d[0, :].partition_broadcast(32), [], [posi.buf], self.msem())
        self.cp(posf.ap[R, :], posi.ap[R, :], [posi.buf], [posf.buf])
        cb = self.cst_t.buf
        self.ts(ang.ap[R, :], posf.ap[R, :], self.cst(CS_INVF, 64, 96), None, ALU.mult, None, [posf.buf, cb], [ang.buf])
        MAGIC = 12582912.0
        INV2PI = float(np.float32(1.0 / (2 * np.pi)))
        C1 = 6.28125
        C2 = float(np.float32(2 * np.pi - 6.28125))
        PIS = 3.1415925
        for which, dst in (("sin", sinT), ("cos", cosT)):
            if which == "sin":
                self.ts(ta.ap[R, :], ang.ap[R, :], INV2PI, None, ALU.mult, None, [ang.buf], [ta.buf])
            else:
                self.ts(ta.ap[R, :], ang.ap[R, :], INV2PI, 0.25, ALU.mult, ALU.add, [ang.buf], [ta.buf])
            self.ts(tb.ap[R, :], ta.ap[R, :], MAGIC, None, ALU.add, None, [ta.buf], [tb.buf])
            self.ts(ta.ap[R, :], tb.ap[R, :], -MAGIC, None, ALU.add, None, [tb.buf], [ta.buf])
            self.stt(tb.ap[R, :], ta.ap[R, :], -C1, ang.ap[R, :], ALU.mult, ALU.add, [ta.buf, ang.buf], [tb.buf])
            self.stt(tb.ap[R, :], ta.ap[R, :], -C2, tb.ap[R, :], ALU.mult, ALU.add, [ta.buf, tb.buf], [tb.buf])
            if which == "cos":
                self.ts(tb.ap[R, :], tb.ap[R, :], float(np.float32(np.pi / 2)), None, ALU.add, None, [tb.buf], [tb.buf])
            self.ts(tb.ap[R, :], tb.ap[R, :], PIS, -PIS, ALU.min, ALU.max, [tb.buf], [tb.buf])
            if which == "sin":
                self.act(dst.ap[R, :], tb.ap[R, :], AF.Sin, [tb.buf, cb], [dst.buf], scale=self.cst(CS_SIGN, 64, 96))
            else:
                self.act(dst.ap[R, :], tb.ap[R, :], AF.Sin, [tb.buf], [dst.buf])

    def mix_mla(self, l, tmp):
        P = self.P
        A = self.arena
        o_c = [tmp.bf16(S, f"oc{i}") for i in range(4)]
        free = [list(r) for r in tmp.regions]
        cosT = tmp.bf16(S, "cos")
        sinT = tmp.bf16(S, "sin")
        tr = Bump(A, [list(r) for r in tmp.regions])
        self.rope_tables(cosT, sinT, tr)
        P.barrier()
        cqn = [tmp.bf16(S, f"cqn{i}") for i in range(3)]
        ckvn = [tmp.bf16(S, f"ckvn{i}") for i in range(2)]
        QT = [tmp.bf16(S, f"QT{i}") for i in range(2)]
        KT = [tmp.bf16(S, f"KT{i}") for i in range(2)]
        VA = [tmp.bf16(16 * 128, f"VA{i}") for i in range(2)]
        Pb = [tmp.bf16(512, f"Pb{i}") for i in range(4)]
        rawq = [tmp.f32(512, f"rawq{i}") for i in range(3)]
        rawk = [tmp.f32(512, f"rawk{i}") for i in range(2)]
        sq = [tmp.bf16(512, f"sq{i}") for i in range(2)]
        rt = tmp.f32(512, "rt")
        rstd = tmp.f32(512, "rstd")
        t1 = tmp.f32(512, "t1")
        t2 = tmp.f32(512, "t2")
        rec = tmp.f32(512, "rec")
        rec2 = tmp.f32(512, "rec2")
        cb = self.cst_t.buf
        ppb = self.pp_t.buf
        RR = slice(64, 96)
        VAv = [v.ap.rearrange("p (k c) -> p k c", c=128) for v in VA]
        self.memset(VAv[0][:, :, 64:128], 1.0, [VA[0].buf])
        self.memset(VAv[1][:, :, 0:64], 1.0, [VA[1].buf])

        def normgroup(tiles_cols, raws, dsts, gname, nfeat, ssq_bank, tg):
            sl = slice(tg * TG, (tg + 1) * TG)
            n = len(tiles_cols)
            for c, (wt, co, ks) in enumerate(tiles_cols):
                bank = self.PB[c % 2]
                for kc in range(8):
                    self.mm(bank.ap, wt.ap[:, kc * ks + co: kc * ks + co + 128], self.hT[kc].ap[:, sl], kc == 0, kc == 7,
                            [wt.buf, self.hT[kc].buf], [bank.buf])
                s = sq[c % 2]
                self.act(s.ap, bank.ap, AF.Square, [bank.buf], [s.buf])
                self.mm(ssq_bank.ap, self.ones_bf.ap, s.ap, c == 0, c == n - 1, [self.ones_bf.buf, s.buf], [ssq_bank.buf])
                self.cp(raws[c].ap, bank.ap, [bank.buf], [raws[c].buf])
            self.act(rt.ap, ssq_bank.ap, AF.Ln, [ssq_bank.buf, cb], [rt.buf], bias=self.cst(CS_EPS), scale=1.0 / nfeat)
            self.act(rstd.ap, rt.ap, AF.Exp, [rt.buf], [rstd.buf], scale=-0.5)
            for c in range(n):
                self.stt(dsts[c].ap[:, sl], raws[c].ap, self.ppc(l, gname, c), rstd.ap, ALU.mult, ALU.mult,
                         [raws[c].buf, rstd.buf, ppb], [dsts[c].buf])

        for tg in range(NTG):
            sl = slice(tg * TG, (tg + 1) * TG)
            tA = self.ws.next(("k", "w_in", l, None, 0, 128, 8, ((C_CQ, 256),)), 2048)
            tB = self.ws.next(("k", "w_in", l, None, 0, 128, 8, ((C_CQ + 256, 128), (C_CKV, 128))), 2048)
            normgroup([(tA, 0, 256), (tA, 128, 256), (tB, 0, 256)], rawq, cqn, "mqn", 384, self.PB[7], tg)
            bank0 = self.PB[2]
            for kc in range(8):
                self.mm(bank0.ap, tB.ap[:, kc * 256 + 128: kc * 256 + 256], self.hT[kc].ap[:, sl], kc == 0, kc == 7,
                        [tB.buf, self.hT[kc].buf], [bank0.buf])
            s = sq[0]
            self.act(s.ap, bank0.ap, AF.Square, [bank0.buf], [s.buf])
            self.mm(self.PB[6].ap, self.ones_bf.ap, s.ap, True, False, [self.ones_bf.buf, s.buf], [self.PB[6].buf])
            self.cp(rawk[0].ap, bank0.ap, [bank0.buf], [rawk[0].buf])
            tC = self.ws.next(("k", "w_in", l, None, 0, 128, 8, ((C_CKV + 128, 128), (C_KR - 64, 96))), 8 * 224)
            bank1 = self.PB[3]
            for kc in range(8):
                self.mm(bank1.ap, tC.ap[:, kc * 224: kc * 224 + 128], self.hT[kc].ap[:, sl], kc == 0, kc == 7,
                        [tC.buf, self.hT[kc].buf], [bank1.buf])
            s = sq[1]
            self.act(s.ap, bank1.ap, AF.Square, [bank1.buf], [s.buf])
            self.mm(self.PB[6].ap, self.ones_bf.ap, s.ap, False, True, [self.ones_bf.buf, s.buf], [self.PB[6].buf])
            self.cp(rawk[1].ap, bank1.ap, [bank1.buf], [rawk[1].buf])
            self.act(rt.ap, self.PB[6].ap, AF.Ln, [self.PB[6].buf, cb], [rt.buf], bias=self.cst(CS_EPS), scale=1.0 / 256)
            self.act(rstd.ap, rt.ap, AF.Exp, [rt.buf], [rstd.buf], scale=-0.5)
            for c in range(2):
                self.stt(ckvn[c].ap[:, sl], rawk[c].ap, self.ppc(l, "mkn", c), rstd.ap, ALU.mult, ALU.mult,
                         [rawk[c].buf, rstd.buf, ppb], [ckvn[c].buf])
            bA = self.PB[4]
            for kc in range(8):
                self.mm(bA.ap[0:96, :], tC.ap[:, kc * 224 + 128: kc * 224 + 224], self.hT[kc].ap[:, sl], kc == 0, kc == 7,
                        [tC.buf, self.hT[kc].buf], [bA.buf])
            tD = self.ws.next(("k", "w_in", l, None, 0, 128, 8, ((C_KR - 64, 64), (C_KR + 16, 16), (C_KR, 16))), 8 * 96)
            bB = self.PB[5]
            for kc in range(8):
                self.mm(bB.ap[0:96, :], tD.ap[:, kc * 96: kc * 96 + 96], self.hT[kc].ap[:, sl], kc == 0, kc == 7,
                        [tD.buf, self.hT[kc].buf], [bB.buf])
            self.tt(t1.ap[RR, :], bA.ap[RR, :], cosT.ap[RR, sl], ALU.mult, [bA.buf, cosT.buf], [t1.buf])
            self.tt(t2.ap[RR, :], bB.ap[RR, :], sinT.ap[RR, sl], ALU.mult, [bB.buf, sinT.buf], [t2.buf])
            self.tt(KT[0].ap[RR, sl], t1.ap[RR, :], t2.ap[RR, :], ALU.add, [t1.buf, t2.buf], [KT[0].buf])
            self.cp(KT[1].ap[RR, sl], KT[0].ap[RR, sl], [KT[0].buf], [KT[1].buf])

        SCALE = float(96 ** -0.5)
        for h in range(8):
            par = h % 2
            QTh, KTh, VAh, VAhv = QT[par], KT[par], VA[par], VAv[par]
            voff = 0 if par == 0 else 64
            tq = self.ws.next(("k", "mla_w_uq", l, None, 0, 128, 3,
                               ((h * 96, 96), (h * 96, 64), (h * 96 + 80, 16), (h * 96 + 64, 16))), 3 * 192)
            tkv = self.ws.next(("k", "mla_w_ukv", l, None, 0, 128, 2, ((h * 128, 128),)), 256)
            for tg in range(NTG):
                sl = slice(tg * TG, (tg + 1) * TG)
                bA, bB, bK = self.PB[4], self.PB[5], self.PB[7]
                for kc in range(3):
                    self.mm(bA.ap[0:96, :], tq.ap[:, kc * 192: kc * 192 + 96], cqn[kc].ap[:, sl], kc == 0, kc == 2,
                            [tq.buf, cqn[kc].buf], [bA.buf])
                for kc in range(3):
                    self.mm(bB.ap[0:96, :], tq.ap[:, kc * 192 + 96: kc * 192 + 192], cqn[kc].ap[:, sl], kc == 0, kc == 2,
                            [tq.buf, cqn[kc].buf], [bB.buf])
                for kc in range(2):
                    self.mm(bK.ap[0:64, :], tkv.ap[:, kc * 128: kc * 128 + 64], ckvn[kc].ap[:, sl], kc == 0, kc == 1,
                            [tkv.buf, ckvn[kc].buf], [bK.buf])
                self.act(QTh.ap[0:64, sl], bA.ap[0:64, :], AF.Copy, [bA.buf], [QTh.buf])
                self.tt(t1.ap[RR, :], bA.ap[RR, :], cosT.ap[RR, sl], ALU.mult, [bA.buf, cosT.buf], [t1.buf])
                self.tt(t2.ap[RR, :], bB.ap[RR, :], sinT.ap[RR, sl], ALU.mult, [bB.buf, sinT.buf], [t2.buf])
                self.tt(QTh.ap[RR, sl], t1.ap[RR, :], t2.ap[RR, :], ALU.add, [t1.buf, t2.buf], [QTh.buf])
                self.act(KTh.ap[0:64, sl], bK.ap[0:64, :], AF.Copy, [bK.buf], [KTh.buf])
            for kh in range(2):
                bV = self.PB[7]
                for k8 in range(8):
                    kt = kh * 8 + k8
                    for kc in range(2):
                        self.mm(bV.ap[:, k8 * 64:(k8 + 1) * 64], ckvn[kc].ap[:, kt * 128:(kt + 1) * 128],
                                tkv.ap[:, kc * 128 + 64: kc * 128 + 128], kc == 0, kc == 1,
                                [tkv.buf, ckvn[kc].buf], [bV.buf])
                self.act(VAhv[:, kh * 8:(kh + 1) * 8, voff:voff + 64], bV.ap.rearrange("p (k c) -> p k c", c=64), AF.Copy,
                         [bV.buf], [VAh.buf])
            m = h // 2
            pairs = [(g, j) for g in range(4) for j in range(4 * g + 4)]
            SB = [self.PB[0], self.PB[1], self.PB[6]]
            LOOK = 2

            def emitS(i):
                g, j = pairs[i]
                Sb = SB[i % 3]
                pb = Pb[i % 4]
                c0 = max(0, j - 4 * g) * 128
                self.mm(Sb.ap[:, c0:512], KTh.ap[0:96, j * 128:(j + 1) * 128], QTh.ap[0:96, g * 512 + c0:(g + 1) * 512],
                        True, True, [KTh.buf, QTh.buf], [Sb.buf])
                self.act(pb.ap[:, c0:512], Sb.ap[:, c0:512], AF.Exp, [Sb.buf], [pb.buf], scale=SCALE)
                if j >= 4 * g:
                    self.memset(pb.ap[64:128, c0:c0 + 64], 0.0, [pb.buf])

            def emitPV(i):
                g, j = pairs[i]
                pb = Pb[i % 4]
                O = self.PB[2 + g % 2]
                nj = 4 * g + 4
                c0 = max(0, j - 4 * g) * 128
                self.mm(O.ap[:, c0:512], VAhv[:, j, :], pb.ap[:, c0:512], j == 0, j == nj - 1,
                        [VAh.buf, pb.buf], [O.buf], skip=True)
                if j == nj - 1:
                    gs = slice(g * 512, (g + 1) * 512)
                    if par == 0:
                        self.act(rec.ap[64:128, :], O.ap[64:128, :], AF.Ln, [O.buf], [rec.buf])
                        self.act(rec.ap[64:128, :], rec.ap[64:128, :], AF.Exp, [rec.buf], [rec.buf], scale=-1.0)
                        self.cp(rec2.ap[0:64, :], rec.ap[64:128, :], [rec.buf], [rec2.buf])
                        self.tt(o_c[m].ap[0:64, gs], O.ap[0:64, :], rec2.ap[0:64, :], ALU.mult, [O.buf, rec2.buf], [o_c[m].buf])
                    else:
                        self.act(rec.ap[0:64, :], O.ap[0:64, :], AF.Ln, [O.buf], [rec.buf])
                        self.act(rec.ap[0:64, :], rec.ap[0:64, :], AF.Exp, [rec.buf], [rec.buf], scale=-1.0)
                        self.cp(rec2.ap[64:128, :], rec.ap[0:64, :], [rec.buf], [rec2.buf])
                        self.tt(o_c[m].ap[64:128, gs], O.ap[64:128, :], rec2.ap[64:128, :], ALU.mult, [O.buf, rec2.buf],
                                [o_c[m].buf])

            for i in range(len(pairs) + LOOK):
                if i < len(pairs):
                    emitS(i)
                if i - LOOK >= 0:
                    emitPV(i - LOOK)
        return o_c, free

    def mix_gmlp(self, l, tmp):
        o_a = [tmp.bf16(S, f"oa{i}") for i in range(4)]
        free = [list(r) for r in tmp.regions]
        grow = tmp.f32(512, "grow")
        brow = tmp.f32(512, "brow")
        self.dma(grow.ap, self.rows_d[l, 0, :].partition_broadcast(128), [], [grow.buf], self.msem())
        self.dma(brow.ap, self.rows_d[l, 1, :].partition_broadcast(128), [], [brow.buf], self.msem())
        vn = tmp.bf16(16 * 512, "vn")
        vnv = vn.ap.rearrange("p (n c) -> p n c", c=512)
        junk = tmp.bf16(512, "junk")
        ssq = tmp.f32(16, "ssq")
        rt16 = tmp.f32(16, "rt16")
        rsd = tmp.f32(16, "rsd")
        wm = tmp.bf16(512, "wm")
        ug = [tmp.f32(512, f"ug{i}") for i in range(2)]
        svb = [tmp.f32(512, f"svb{i}") for i in range(2)]
        cb = self.cst_t.buf
        tv = [self.ws.next(("k", "w_in", l, None, 0, 128, 8, ((512 + hf * 256, 256),)), 2048) for hf in range(2)]
        gvall = tmp.f32(16 * 512, "gvall")
        gvv = gvall.ap.rearrange("p (n c) -> p n c", c=512)
        for n in range(16):
            bank = self.PB[n % 2]
            for hf in range(2):
                for kc in range(8):
                    self.mm(bank.ap[:, hf * 256:(hf + 1) * 256], self.hT[kc].ap[:, n * 128:(n + 1) * 128],
                            tv[hf].ap[:, kc * 256:(kc + 1) * 256], kc == 0, kc == 7,
                            [tv[hf].buf, self.hT[kc].buf], [bank.buf])
            self.act(gvv[:, n, :], bank.ap, AF.Gelu_apprx_tanh, [bank.buf], [gvall.buf])
        for n in range(16):
            self.act(junk.ap, gvv[:, n, :], AF.Square, [gvall.buf], [junk.buf, ssq.buf], accum=ssq.ap[:, n:n + 1])
        self.act(rt16.ap, ssq.ap, AF.Ln, [ssq.buf, cb], [rt16.buf], bias=self.cst(CS_EPS), scale=1.0 / 512)
        self.act(rsd.ap, rt16.ap, AF.Exp, [rt16.buf], [rsd.buf], scale=-0.5)
        for n in range(16):
            self.stt(vnv[:, n, :], gvv[:, n, :], rsd.ap[:, n:n + 1], grow.ap, ALU.mult, ALU.mult,
                     [gvall.buf, rsd.buf, grow.buf], [vn.buf])
        tw = self.ws.next(("blkT", "gmlp_w_s", l), 512)
        self.cp(wm.ap, tw.ap[:, 0:512], [tw.buf], [wm.buf])
        self.memset(wm.ap.rearrange("p (h t) -> p h t", t=128)[64:128, :, 0:64], 0.0, [wm.buf])
        it = 0
        for hp in range(2):
            tu = self.ws.next(("k", "w_in", l, None, 0, 128, 8, ((hp * 256, 256),)), 2048)
            for hi in range(2):
                h = 2 * hp + hi
                for tg in range(NTG):
                    sl = slice(tg * TG, (tg + 1) * TG)
                    ub = self.PB[2 + it % 2]
                    sb = self.PB[4 + it % 2]
                    u_ = ug[it % 2]
                    s_ = svb[it % 2]
                    it += 1
                    for kc in range(8):
                        self.mm(ub.ap, tu.ap[:, kc * 256 + hi * 128: kc * 256 + hi * 128 + 128], self.hT[kc].ap[:, sl],
                                kc == 0, kc == 7, [tu.buf, self.hT[kc].buf], [ub.buf])
                    for i in range(4):
                        n = 4 * tg + i
                        self.mm(sb.ap[:, i * 128:(i + 1) * 128], vnv[:, n, h * 128:(h + 1) * 128], wm.ap[:, h * 128:(h + 1) * 128],
                                True, True, [vn.buf, wm.buf], [sb.buf])
                    self.act(u_.ap, ub.ap, AF.Gelu_apprx_tanh, [ub.buf], [u_.buf])
                    bb = brow.ap[:, h * 128:(h + 1) * 128].unsqueeze(1).to_broadcast([128, 4, 128])
                    self.tt(s_.ap.rearrange("p (a b) -> p a b", b=128), sb.ap.rearrange("p (a b) -> p a b", b=128), bb,
                            ALU.add, [sb.buf, brow.buf], [s_.buf])
                    self.tt(o_a[h].ap[:, sl], s_.ap, u_.ap, ALU.mult, [s_.buf, u_.buf], [o_a[h].buf])
        return o_a, free

    def mix_lru(self, l, tmp):
        ppb = self.pp_t.buf
        cb = self.cst_t.buf
        d = self.lrud
        lam = self.pp_t.ap[:, l * NPP + PP["llam"]: l * NPP + PP["llam"] + 4]
        self.act(d.ap[:, 0:4], lam, AF.Exp, [ppb], [d.buf], scale=-1.0)
        self.act(d.ap[:, 4:8], d.ap[:, 0:4], AF.Ln, [d.buf, cb], [d.buf], bias=self.cst(CS_ONE))
        self.ts(d.ap[:, 8:12], d.ap[:, 4:8], -8.0, None, ALU.mult, None, [d.buf], [d.buf])
        o_b = [tmp.bf16(S, f"ob{i}") for i in range(4)]
        free = [list(r) for r in tmp.regions]
        wa = tmp.bf16(512, "wa")
        wi = tmp.bf16(512, "wi")
        t_ = self.ws.next(("blk", "lru_w_a", l), 512)
        self.cp(wa.ap, t_.ap[:, 0:512], [t_.buf], [wa.buf])
        t_ = self.ws.next(("blk", "lru_w_i", l), 512)
        self.cp(wi.ap, t_.ap[:, 0:512], [t_.buf], [wi.buf])
        uraws = [tmp.f32(S + 4, f"uraw{i}") for i in range(2)]
        ups = [tmp.f32(S, f"up{i}") for i in range(2)]
        upbs = [tmp.bf16(S, f"upb{i}") for i in range(2)]
        ra = tmp.f32(S, "ra")
        ig = tmp.f32(S, "ig")
        sq_ = tmp.f32(S, "sq_")
        hh_ = tmp.f32(S, "hh")
        yg = [tmp.f32(512, f"yg{i}") for i in range(2)]
        for u_ in uraws:
            self.memset(u_.ap[:, 0:3], 0.0, [u_.buf])
        itc = [0]

        def stageA(c):
            uraw, up, upb = uraws[c % 2], ups[c % 2], upbs[c % 2]
            tu = self.ws.next(("k", "w_in", l, None, 0, 128, 8, ((C_UB + c * 128, 128),)), 1024)
            for tg in range(NTG):
                sl = slice(tg * TG, (tg + 1) * TG)
                b = self.PB[tg % 2]
                for kc in range(8):
                    self.mm(b.ap, tu.ap[:, kc * 128:(kc + 1) * 128], self.hT[kc].ap[:, sl], kc == 0, kc == 7,
                            [tu.buf, self.hT[kc].buf], [b.buf])
                self.act(uraw.ap[:, 3 + tg * TG: 3 + (tg + 1) * TG], b.ap, AF.Copy, [b.buf], [uraw.buf])
            w = lambda k: self.ppc(l, "lcw", c * 4 + k)
            self.ts(up.ap, uraw.ap[:, 0:S], w(0), self.ppc(l, "lcb", c), ALU.mult, ALU.add, [uraw.buf, ppb], [up.buf])
            for k in range(1, 4):
                self.stt(up.ap, uraw.ap[:, k:k + S], w(k), up.ap, ALU.mult, ALU.add, [uraw.buf, up.buf, ppb], [up.buf])
            self.act(upb.ap, up.ap, AF.Copy, [up.buf], [upb.buf])

        def stageB(c):
            up, upb = ups[c % 2], upbs[c % 2]
            for tg in range(NTG):
                sl = slice(tg * TG, (tg + 1) * TG)
                b = self.PB[2 + tg % 2]
                self.mm(b.ap, wa.ap[:, c * 128:(c + 1) * 128], upb.ap[:, sl], True, True, [wa.buf, upb.buf], [b.buf])
                self.act(ra.ap[:, sl], b.ap, AF.Sigmoid, [b.buf, ppb], [ra.buf], bias=self.ppc(l, "lba", c))
                b2 = self.PB[4 + tg % 2]
                self.mm(b2.ap, wi.ap[:, c * 128:(c + 1) * 128], upb.ap[:, sl], True, True, [wi.buf, upb.buf], [b2.buf])
                self.act(ig.ap[:, sl], b2.ap, AF.Sigmoid, [b2.buf, ppb], [ig.buf], bias=self.ppc(l, "lbi", c))
            self.act(ra.ap, ra.ap, AF.Exp, [ra.buf, d.buf], [ra.buf], scale=d.ap[:, 8 + c: 9 + c])
            self.tt(ig.ap, ig.ap, up.ap, ALU.mult, [ig.buf, up.buf], [ig.buf], eng="pool")
            self.stt(sq_.ap, ra.ap, -1.0, ra.ap, ALU.mult, ALU.mult, [ra.buf], [sq_.buf])
            self.act(sq_.ap, sq_.ap, AF.Sqrt, [sq_.buf, cb], [sq_.buf], bias=self.cst(CS_ONE))
            self.tt(ig.ap, ig.ap, sq_.ap, ALU.mult, [ig.buf, sq_.buf], [ig.buf])
            self.P.add("dve", lambda e, o=hh_.ap, a=ra.ap, x=ig.ap: e.tensor_tensor_scan(out=o, data0=a, data1=x, initial=0.0,
                                                                                         op0=ALU.mult, op1=ALU.add),
                       [ra.buf, ig.buf], [hh_.buf])
            ty = self.ws.next(("k", "w_in", l, None, 0, 128, 8, ((C_YB + c * 128, 128),)), 1024)
            for tg in range(NTG):
                sl = slice(tg * TG, (tg + 1) * TG)
                b = self.PB[6 + tg % 2]
                y_ = yg[itc[0] % 2]
                itc[0] += 1
                for kc in range(8):
                    self.mm(b.ap, ty.ap[:, kc * 128:(kc + 1) * 128], self.hT[kc].ap[:, sl], kc == 0, kc == 7,
                            [ty.buf, self.hT[kc].buf], [b.buf])
                self.act(y_.ap, b.ap, AF.Gelu_apprx_tanh, [b.buf], [y_.buf])
                self.tt(o_b[c].ap[:, sl], hh_.ap[:, sl], y_.ap, ALU.mult, [hh_.buf, y_.buf], [o_b[c].buf])

        stageA(0)
        for c in range(4):
            if c + 1 < 4:
                stageA(c + 1)
            stageB(c)
        return o_b, free

    def mix_gla(self, l, tmp):
        P = self.P
        A = self.arena
        ppb = self.pp_t.buf
        cb = self.cst_t.buf
        baseA = Bump(A, [list(r) for r in tmp.regions])
        ogl_words = 4 * S
        glr = tmp.f32(S, "glr")
        Cc = tmp.f32(S, "Cc")
        spb = tmp.f32(S, "spb")
        rmask = tmp.f32(S, "rmask")
        ogl_all = baseA.f32(4 * S, "ogl")
        ogl = [T(ogl_all.ap[:, h * S:(h + 1) * S], buf=ogl_all.buf) for h in range(4)]
        wg2 = tmp.f32(256, "wg2")
        nbg = tmp.f32(2, "nbg")
        qk_off = tmp._take(4096)
        free = [[qk_off, qk_off + 4096]]
        sub = Bump(A, [[qk_off, qk_off + 4096]])
        qin = [sub.bf16(S, f"qin{i}") for i in range(2)]
        kin = [sub.bf16(S, f"kin{i}") for i in range(2)]
        kstT = [tmp.bf16(512, f"kstT{i}") for i in range(2)]
        ksttok = tmp.bf16(16 * 256, "ksttok")
        ksv = ksttok.ap.rearrange("p (n c) -> p n c", c=256)
        vtok = tmp.bf16(16 * 512, "vtok")
        vtv = vtok.ap.rearrange("p (n c) -> p n c", c=512)
        et = [tmp.f32(512, f"et{i}") for i in range(3)]
        dec = tmp.f32(64, "dec")
        Sf = tmp.f32(256, "Sf")
        Sb_ = tmp.bf16(256, "Sb")
        attm = [tmp.bf16(512, f"attm{i}") for i in range(2)]
        sq = [tmp.bf16(512, f"gsq{i}") for i in range(2)]
        rt = tmp.f32(512, "grt")
        rstd = tmp.f32(512, "grstd")
        sg = tmp.f32(512, "gsg")
        tq_ = tmp.f32(512, "gtq")
        self.dma(wg2.ap[0:16, :], self.wg2_d[l], [], [wg2.buf], self.msem())
        self.dma(rmask.ap, self.rm_d, [], [rmask.buf], self.msem())
        gb = self.pp_t.ap[:, l * NPP + PP["gbg"]: l * NPP + PP["gbg"] + 2]
        self.ts(nbg.ap, gb, -1.0, None, ALU.mult, None, [ppb], [nbg.buf])
        tg_ = self.ws.next(("k", "w_in", l, None, 0, 128, 8, ((C_GD, 16),)), 128)
        for tg in range(NTG):
            sl = slice(tg * TG, (tg + 1) * TG)
            b = self.PB[tg % 2]
            for kc in range(8):
                self.mm(b.ap[0:16, :], tg_.ap[:, kc * 16:(kc + 1) * 16], self.hT[kc].ap[:, sl], kc == 0, kc == 7,
                        [tg_.buf, self.hT[kc].buf], [b.buf])
            self.act(glr.ap[0:16, sl], b.ap[0:16, :], AF.Copy, [b.buf], [glr.buf])
        it = 0
        for m in range(2):
            for tg in range(NTG):
                sl = slice(tg * TG, (tg + 1) * TG)
                b = self.PB[2 + tg % 2]
                self.mm(b.ap, wg2.ap[0:16, m * 128:(m + 1) * 128], glr.ap[0:16, sl], True, True, [wg2.buf, glr.buf], [b.buf])
                e_ = et[tg % 2]
                self.act(e_.ap, b.ap, AF.Exp, [b.buf, nbg.buf], [e_.buf], bias=nbg.ap[:, m:m + 1], scale=-1.0)
                self.act(spb.ap[:, sl], e_.ap, AF.Ln, [e_.buf, cb], [spb.buf], bias=self.cst(CS_ONE))
            P.add("dve", lambda e, o=Cc.ap, a=rmask.ap, x=spb.ap: e.tensor_tensor_scan(out=o, data0=a, data1=x, initial=0.0,
                                                                                      op0=ALU.mult, op1=ALU.add),
                  [rmask.buf, spb.buf], [Cc.buf])
            Cv = Cc.ap.rearrange("p (c t) -> p c t", t=64)
            self.act(dec.ap[:, m * 32:(m + 1) * 32], Cv[:, :, 63], AF.Exp, [Cc.buf], [dec.buf], scale=-1.0 / 16)
            tq = self.ws.next(("k", "w_in", l, None, 0, 128, 8, ((C_QD + m * 128, 128),)), 1024)
            for tg in range(NTG):
                sl = slice(tg * TG, (tg + 1) * TG)
                b = self.PB[4 + tg % 2]
                for kc in range(8):
                    self.mm(b.ap, tq.ap[:, kc * 128:(kc + 1) * 128], self.hT[kc].ap[:, sl], kc == 0, kc == 7,
                            [tq.buf, self.hT[kc].buf], [b.buf])
                e_ = et[it % 3]
                it += 1
                self.act(e_.ap, Cc.ap[:, sl], AF.Exp, [Cc.buf], [e_.buf], scale=-1.0 / 16)
                self.stt(qin[m].ap[:, sl], b.ap, 0.125, e_.ap, ALU.mult, ALU.mult, [b.buf, e_.buf], [qin[m].buf])
            tk = self.ws.next(("k", "w_in", l, None, 0, 128, 8, ((C_KD + m * 128, 128),)), 1024)
            for tg in range(NTG):
                sl = slice(tg * TG, (tg + 1) * TG)
                b = self.PB[6 + tg % 2]
                for kc in range(8):
                    self.mm(b.ap, tk.ap[:, kc * 128:(kc + 1) * 128], self.hT[kc].ap[:, sl], kc == 0, kc == 7,
                            [tk.buf, self.hT[kc].buf], [b.buf])
                e_ = et[it % 3]
                it += 1
                self.act(e_.ap, Cc.ap[:, sl], AF.Exp, [Cc.buf], [e_.buf], scale=1.0 / 16)
                self.tt(kin[m].ap[:, sl], b.ap, e_.ap, ALU.mult, [b.buf, e_.buf], [kin[m].buf])
                e2 = et[it % 3]
                it += 1
                Cs = Cc.ap[:, sl].rearrange("p (c t) -> p c t", t=64)
                self.tt(e2.ap.rearrange("p (c t) -> p c t", t=64), Cs[:, :, 63:64].to_broadcast([128, 8, 64]), Cs, ALU.subtract,
                        [Cc.buf], [e2.buf])
                self.act(e2.ap, e2.ap, AF.Exp, [e2.buf], [e2.buf], scale=-1.0 / 16)
                ks = kstT[tg % 2]
                self.tt(ks.ap, b.ap, e2.ap, ALU.mult, [b.buf, e2.buf], [ks.buf])
                bt = self.PB[tg % 2]
                for i in range(4):
                    self.mm(bt.ap[:, i * 128:(i + 1) * 128], ks.ap[:, i * 128:(i + 1) * 128], self.ident_bf.ap, True, True,
                            [ks.buf, self.ident_bf.buf], [bt.buf])
                self.act(ksv[:, tg * 4:(tg + 1) * 4, m * 128:(m + 1) * 128], bt.ap.rearrange("p (n c) -> p n c", c=128), AF.Copy,
                         [bt.buf], [ksttok.buf])
        tvs = [self.ws.next(("k", "w_in", l, None, 0, 128, 8, ((C_VD + hf * 256, 256),)), 2048) for hf in range(2)]
        for n in range(16):
            bank = self.PB[2 + n % 2]
            for hf in range(2):
                for kc in range(8):
                    self.mm(bank.ap[:, hf * 256:(hf + 1) * 256], self.hT[kc].ap[:, n * 128:(n + 1) * 128],
                            tvs[hf].ap[:, kc * 256:(kc + 1) * 256], kc == 0, kc == 7,
                            [tvs[hf].buf, self.hT[kc].buf], [bank.buf])
            self.act(vtv[:, n, :], bank.ap, AF.Copy, [bank.buf], [vtok.buf])
        self.tap(f"gla_qin{l}", qin)
        self.tap(f"gla_kin{l}", kin)
        P.barrier()
        self.memset(Sf.ap, 0.0, [Sf.buf])
        self.memset(Sb_.ap, 0.0, [Sb_.buf])
        Sfv = Sf.ap.rearrange("p (m v) -> p m v", v=128)
        Sbv = Sb_.ap.rearrange("p (m v) -> p m v", v=128)
        gm = self.gmask_bf.ap.unsqueeze(1).to_broadcast([128, 4, 128])
        ab = [self.PB[0], self.PB[1]]
        gm2 = self.gmask_bf.ap.unsqueeze(1).to_broadcast([128, 2, 128])
        oglv = ogl_all.ap.rearrange("p (h t) -> p h t", t=S)

        def emitA(n):
            nsl = slice(n * 128, (n + 1) * 128)
            for h in range(4):
                m, po, hs = h // 2, (h % 2) * 64, (h // 2) * 128
                self.mm(ab[h % 2].ap[:, hs:hs + 128], kin[m].ap[po:po + 64, nsl], qin[m].ap[po:po + 64, nsl], True, True,
                        [kin[m].buf, qin[m].buf], [ab[h % 2].buf])

        def emitK(c):
            n, tp = c // 2, (c % 2) * 64
            kvb = self.PB[6 + c % 2]
            for h in range(4):
                m, po = h // 2, (h % 2) * 64
                self.mm(kvb.ap[po:po + 64, m * 128:(m + 1) * 128], ksv[tp:tp + 64, n, h * 64:(h + 1) * 64],
                        vtv[tp:tp + 64, n, h * 128:(h + 1) * 128], True, True, [ksttok.buf, vtok.buf], [kvb.buf])

        emitA(0)
        emitK(0)
        emitK(1)
        for n in range(16):
            ob = [self.PB[2 + 2 * (n % 2)], self.PB[3 + 2 * (n % 2)]]
            nsl = slice(n * 128, (n + 1) * 128)
            am = attm[n % 2]
            amv = am.ap.rearrange("p (h t) -> p h t", t=128)
            for par in range(2):
                self.tt(amv[:, par::2, :], ab[par].ap[:, 0:256].rearrange("p (h t) -> p h t", t=128), gm2, ALU.mult,
                        [ab[par].buf, self.gmask_bf.buf], [am.buf])
            for h in range(4):
                hs = (h // 2) * 128
                self.mm(ob[h % 2].ap[:, hs:hs + 128], vtv[:, n, h * 128:(h + 1) * 128], am.ap[:, h * 128:(h + 1) * 128],
                        h < 2, False, [vtok.buf, am.buf], [ob[h % 2].buf], skip=True)
            if n + 1 < 16:
                emitA(n + 1)
            for cc in range(2):
                c = 2 * n + cc
                csl = slice(c * 64, (c + 1) * 64)
                for h in range(4):
                    m, po, hs = h // 2, (h % 2) * 64, (h // 2) * 128
                    self.mm(ob[h % 2].ap[:, hs + cc * 64: hs + cc * 64 + 64], Sbv[po:po + 64, m, :], qin[m].ap[po:po + 64, csl],
                            False, (cc == 1 and h >= 2), [Sb_.buf, qin[m].buf], [ob[h % 2].buf], skip=True)
                kvb = self.PB[6 + c % 2]
                for m in range(2):
                    self.stt(Sfv[:, m, :], Sfv[:, m, :], dec.ap[:, m * 32 + c: m * 32 + c + 1], kvb.ap[:, m * 128:(m + 1) * 128],
                             ALU.mult, ALU.add, [Sf.buf, dec.buf, kvb.buf], [Sf.buf])
                self.act(Sb_.ap, Sf.ap, AF.Copy, [Sf.buf], [Sb_.buf])
                if c + 2 < 32:
                    emitK(c + 2)
            for par in range(2):
                self.act(oglv[:, par::2, nsl], ob[par].ap[:, 0:256].rearrange("p (h t) -> p h t", t=128), AF.Copy,
                         [ob[par].buf], [ogl_all.buf])
        self.tap(f"gla_ogl{l}", ogl)
        P.barrier()
        o_d = [A.bf16(vtok.off + h * (S // 2), S, f"od{h}") for h in range(4)]
        it = 0
        for h in range(4):
            to = self.ws.next(("k", "w_in", l, None, 0, 128, 8, ((C_OG + h * 128, 128),)), 1024)
            for tg in range(NTG):
                sl = slice(tg * TG, (tg + 1) * TG)
                s = sq[it % 2]
                bq = self.PB[it % 2]
                bo = self.PB[2 + it % 2]
                it += 1
                self.act(s.ap, ogl[h].ap[:, sl], AF.Square, [ogl[h].buf], [s.buf])
                self.mm(bq.ap, self.ones_bf.ap, s.ap, True, True, [self.ones_bf.buf, s.buf], [bq.buf])
                for kc in range(8):
                    self.mm(bo.ap, to.ap[:, kc * 128:(kc + 1) * 128], self.hT[kc].ap[:, sl], kc == 0, kc == 7,
                            [to.buf, self.hT[kc].buf], [bo.buf])
                self.act(rt.ap, bq.ap, AF.Ln, [bq.buf, cb], [rt.buf], bias=self.cst(CS_EPS), scale=1.0 / 128)
                self.act(rstd.ap, rt.ap, AF.Exp, [rt.buf], [rstd.buf], scale=-0.5)
                self.act(sg.ap, bo.ap, AF.Silu, [bo.buf], [sg.buf])
                self.stt(tq_.ap, ogl[h].ap[:, sl], self.ppc(l, "gon", h), rstd.ap, ALU.mult, ALU.mult,
                         [ogl[h].buf, rstd.buf, ppb], [tq_.buf])
                self.tt(o_d[h].ap[:, sl], tq_.ap, sg.ap, ALU.mult, [tq_.buf, sg.buf], [o_d[h].buf])
        return o_d, free


_CACHE = {}


def _get_kern(**kw):
    key = tuple(sorted((k, str(v)) for k, v in kw.items()))
    if key not in _CACHE:
        _CACHE[key] = Kern(**kw)
    return _CACHE[key]


def _pack_shared(kern, inp):
    wpack = np.zeros((128, kern.wtot), np.float32)
    for key, (off, n) in kern.tile_table.items():
        t = _host_tile(inp, key)
        assert t.shape == (128, n), (key, t.shape, n)
        wpack[:, off:off + n] = t
    pp = np.concatenate([_host_params(inp, l) for l in range(DEPTH)], axis=1)
    rows = np.stack([np.stack([inp["gmlp_v_norm"][l].reshape(512),
                               inp["gmlp_b_s"][l].reshape(512)]) for l in range(DEPTH)]).astype(np.float32)
    return {
        "wpack": wpack,
        "pp": np.ascontiguousarray(pp, dtype=np.float32),
        "cst": _host_consts(),
        "rmask": _host_resetmask(),
        "rows": np.ascontiguousarray(rows),
        "wg2": np.ascontiguousarray(inp["gla_w_g2"], dtype=np.float32),
    }


def _core_inputs(inp, b, shared):
    xT = np.ascontiguousarray(np.asarray(inp["x"][b], dtype=np.float32).T.reshape(8, 128, S))
    d = dict(shared)
    d["xT"] = xT
    d["pos"] = np.ascontiguousarray(np.asarray(inp["positions"][b], dtype=np.int32).reshape(1, S))
    return d


def kernel(**inputs):
    inp = {k: np.asarray(v) for k, v in inputs.items()}
    kern = _get_kern()
    shared = _pack_shared(kern, inp)
    in_maps = [_core_inputs(inp, b, shared) for b in range(NCORE)]
    res = run_bass_kernel_spmd(kern.nc, in_maps, core_ids=list(range(NCORE)))
    out = np.empty((NCORE, S, D), np.float32)
    for b in range(NCORE):
        out[b] = res.results[b]["outT"].reshape(D, S).T
    return out
```

```python
import numpy as np
from contextlib import ExitStack
import concourse.bass as bass
import concourse.mybir as mybir
from concourse.bass_utils import run_bass_kernel_spmd

F32 = mybir.dt.float32
BF16 = mybir.dt.bfloat16
I32 = mybir.dt.int32
AF = mybir.ActivationFunctionType
ALU = mybir.AluOpType

D = 1024
S = 2048
DEPTH = 4
NCORE = 8
TG = 512
NTG = 4
IN_COLS = 8368
FFN = 2816
EPS = 1e-6

C_ZA = 0
C_YB = 1024
C_UB = 1536
C_CQ = 2048
C_CKV = 2432
C_KR = 2688
C_QD = 2720
C_KD = 2976
C_VD = 3232
C_GD = 3744
C_OG = 3760
C_GATE = 4272

PP = {}
_o = 0
for _n, _w in [("nmp", 8), ("nmo", 8), ("nfp", 8), ("nfo", 8), ("lcw", 16), ("lcb", 4), ("lba", 4), ("lbi", 4),
               ("llam", 4), ("mqn", 3), ("mkn", 2), ("gbg", 2), ("gon", 4), ("fcw", 132), ("fcb", 44)]:
    PP[_n] = _o
    _o += _w
NPP = _o

CS_IDENT = 0
CS_ONES = 128
CS_GMASK = 256
CS_INVF = 384
CS_SIGN = 385
CS_EPS = 386
CS_ONE = 387
CS_HALFPI = 388
CS_ZERO = 389
NCST = 392


def _host_consts():
    c = np.zeros((128, NCST), np.float32)
    c[:, CS_IDENT:CS_IDENT + 128] = np.eye(128, dtype=np.float32)
    c[:, CS_ONES:CS_ONES + 128] = 1.0
    s = np.arange(128)[:, None]
    t = np.arange(128)[None, :]
    c[:, CS_GMASK:CS_GMASK + 128] = ((s // 64 == t // 64) & (s <= t)).astype(np.float32)
    invf = (10000.0 ** (-np.arange(0, 32, 2, dtype=np.float32) / np.float32(32))).astype(np.float32)
    for p in range(64, 96):
        c[p, CS_INVF] = invf[(p - 64) % 16]
        c[p, CS_SIGN] = -1.0 if p < 80 else 1.0
    c[:, CS_EPS] = EPS
    c[:, CS_ONE] = 1.0
    c[:, CS_HALFPI] = np.float32(np.pi / 2)
    return c


def _host_resetmask():
    m = np.ones((128, S), np.float32)
    m[:, 0::64] = 0.0
    return m


def _host_params(inp, l):
    p = np.zeros((128, NPP), np.float32)

    def put(name, arr):
        p[:, PP[name]:PP[name] + arr.shape[1]] = arr
    put("nmp", inp["norm_mix_pre"][l].reshape(8, 128).T)
    put("nmo", inp["norm_mix_post"][l].reshape(8, 128).T)
    put("nfp", inp["norm_ffn_pre"][l].reshape(8, 128).T)
    put("nfo", inp["norm_ffn_post"][l].reshape(8, 128).T)
    put("lcw", inp["lru_conv_w"][l].reshape(4, 4, 128).transpose(2, 1, 0).reshape(128, 16))
    put("lcb", inp["lru_conv_b"][l].reshape(4, 128).T)
    put("lba", inp["lru_b_a"][l].reshape(4, 128).T)
    put("lbi", inp["lru_b_i"][l].reshape(4, 128).T)
    put("llam", inp["lru_lambda"][l].reshape(4, 128).T)
    put("mqn", inp["mla_q_norm"][l].reshape(3, 128).T)
    put("mkn", inp["mla_kv_norm"][l].reshape(2, 128).T)
    put("gbg", inp["gla_b_g"][l].reshape(2, 128).T)
    put("gon", inp["gla_o_norm"][l].reshape(4, 128).T)
    put("fcw", inp["ffn_conv_w"][l].reshape(3, 44, 128).transpose(2, 1, 0).reshape(128, 132))
    put("fcb", inp["ffn_conv_b"][l].reshape(44, 128).T)
    return p


def _host_tile(inp, key):
    kind = key[0]
    if kind == "k":
        _, name, l, sub, row0, pn, kc, cols = key
        w = inp[name][l]
        if sub is not None:
            w = w[sub]
        idx = np.concatenate([np.arange(s, s + n) for (s, n) in cols])
        blk = w[row0:row0 + kc * pn][:, idx]
        t = blk.reshape(kc, pn, len(idx)).transpose(1, 0, 2).reshape(pn, kc * len(idx))
        if pn < 128:
            t = np.concatenate([t, np.zeros((128 - pn, t.shape[1]), np.float32)], 0)
        return np.ascontiguousarray(t, dtype=np.float32)
    if kind == "blk":
        _, name, l = key
        return np.ascontiguousarray(inp[name][l].transpose(1, 0, 2).reshape(128, 512), dtype=np.float32)
    if kind == "blkT":
        _, name, l = key
        return np.ascontiguousarray(inp[name][l].transpose(2, 0, 1).reshape(128, 512), dtype=np.float32)
    raise ValueError(key)


class Buf:
    __slots__ = ("name", "writers", "readers", "excl", "lo", "hi", "live")

    def __init__(self, name="", excl=False, lo=None, hi=None):
        self.name = name
        self.excl = excl
        self.lo = lo
        self.hi = hi
        self.live = lo is None
        self.writers = {}
        self.readers = {}


class DSem:
    __slots__ = ("h", "count", "last")

    def __init__(self, h):
        self.h = h
        self.count = 0
        self.last = None


class Op:
    __slots__ = ("eng", "fn", "deps", "is_dma", "idx", "sem", "tick", "signal", "dsem")


ENGS = ["pe", "act", "dve", "pool", "sp"]
SEM_LIMIT = 12000
SAME_ENG_SYNC = True
import os as _os
USE_BARRIERS = bool(int(_os.environ.get('KERN_BARRIERS', '0')))


class Prog:
    def __init__(self, nc, es):
        self.nc = nc
        self.es = es
        self.ops = []
        self.dry = False
        self.nsem = 0
        self.last_op = {}
        self.dmas_since_bar = []
        self.bar_deps = []
        self.bar_pending = set()
        self.live = []

    def touch(self, b):
        if b.live:
            return
        keep = []
        for o in self.live:
            if o.lo < b.hi and b.lo < o.hi:
                for k, op in o.writers.items():
                    if k not in b.writers or b.writers[k].idx < op.idx:
                        b.writers[k] = op
                for k, op in o.readers.items():
                    if k not in b.readers or b.readers[k].idx < op.idx:
                        b.readers[k] = op
                o.writers = {}
                o.readers = {}
                o.live = False
            else:
                keep.append(o)
        keep.append(b)
        b.live = True
        self.live = keep

    def new_sem(self, name="s"):
        self.nsem += 1
        return self.es.enter_context(self.nc.semaphore(f"{name}{self.nsem}"))

    def new_dsem(self):
        return DSem(self.new_sem("d"))

    def barrier(self, force=False):
        if self.dry or not (force or USE_BARRIERS):
            return
        deps = list(self.last_op.values()) + self.dmas_since_bar
        if self.bar_pending:
            deps += self.bar_deps
        self.bar_deps = deps
        self.bar_pending = set(ENGS)
        self.dmas_since_bar = []

    def add(self, eng, fn, reads=(), writes=(), dma=False, dsem=None):
        if self.dry:
            return
        op = Op()
        op.eng = eng
        op.fn = fn
        op.is_dma = dma
        op.idx = len(self.ops)
        op.signal = False
        op.sem = None
        op.tick = 0
        op.dsem = dsem
        key = ("d", op.idx) if dma else eng
        deps = {}
        for b in reads:
            self.touch(b)
        for b in writes:
            self.touch(b)
        ex = [b for b in reads if b.excl and b not in writes]
        if ex:
            writes = list(writes) + ex
            reads = [b for b in reads if not b.excl]

        def need(o):
            if o.is_dma or dma or o.eng != eng or (SAME_ENG_SYNC and eng != "pe"):
                deps[o.idx] = o
        for b in reads:
            for o in b.writers.values():
                need(o)
        for b in writes:
            for o in b.writers.values():
                need(o)
            for o in b.readers.values():
                need(o)
        if eng in self.bar_pending:
            for o in self.bar_deps:
                need(o)
            self.bar_pending.discard(eng)
        if dma:
            assert dsem is not None
            if dsem.last is not None:
                need(dsem.last)
            dsem.last = op
            dsem.count += 1
            op.sem = dsem.h
            op.tick = 16 * dsem.count
            self.dmas_since_bar.append(op)
        for b in writes:
            b.writers = {key: op}
            b.readers = {}
        for b in reads:
            if b not in writes:
                b.readers[key] = op
        op.deps = list(deps.values())
        if not dma:
            self.last_op[eng] = op
        self.ops.append(op)
        return op

    def emit(self, block):
        for op in self.ops:
            for d in op.deps:
                d.signal = True
        cur = {}
        cnt = {}
        for op in self.ops:
            if op.is_dma or not op.signal:
                continue
            e = op.eng
            if e not in cur or cnt[e] >= SEM_LIMIT:
                cur[e] = self.new_sem(e)
                cnt[e] = 0
            cnt[e] += 1
            op.sem = cur[e]
            op.tick = cnt[e]
        import os
        mx = int(os.environ.get("KERN_MAXOPS", "0"))
        ops_all = self.ops[:mx] if mx > 0 else self.ops
        if os.environ.get("KERN_DUMP"):
            for o in ops_all:
                print("OP", o.idx, o.eng, "dma" if o.is_dma else "", "deps", [d.idx for d in o.deps], flush=True)
        streams = {e: [o for o in ops_all if o.eng == e] for e in ENGS}

        def run(eng_handle, ops):
            waited = {}
            for op in ops:
                for d in op.deps:
                    k = id(d.sem)
                    if waited.get(k, 0) < d.tick:
                        eng_handle.wait_ge(d.sem, d.tick)
                        waited[k] = d.tick
                if op.fn is None:
                    continue
                ins = op.fn(eng_handle)
                if op.is_dma:
                    ins.then_inc(op.sem, 16)
                elif op.signal:
                    ins.then_inc(op.sem, 1)

        @block.tensor
        def _(e):
            run(e, streams["pe"])

        @block.scalar
        def _(e):
            run(e, streams["act"])

        @block.vector
        def _(e):
            run(e, streams["dve"])

        @block.gpsimd
        def _(e):
            run(e, streams["pool"])

        @block.sync
        def _(e):
            run(e, streams["sp"])


class T:
    __slots__ = ("ap", "buf", "off", "words")

    def __init__(self, ap, buf=None, name="", off=None, words=None):
        self.ap = ap
        self.off = off
        self.words = words
        if buf is None:
            buf = Buf(name, lo=off, hi=(off + words) if off is not None else None)
        self.buf = buf


class Arena:
    def __init__(self, ap_f32, total_words):
        self.a = ap_f32
        self.total = total_words

    def f32(self, off, n, name=""):
        assert off + n <= self.total, (off, n, self.total)
        return T(self.a[:, off:off + n], name=name, off=off, words=n)

    def bf16(self, off_words, n_bf, name=""):
        assert n_bf % 2 == 0 and off_words + n_bf // 2 <= self.total, (off_words, n_bf, self.total)
        return T(self.a[:, off_words:off_words + n_bf // 2].bitcast(BF16), name=name, off=off_words, words=n_bf // 2)

    def i32(self, off, n, name=""):
        return T(self.a[:, off:off + n].bitcast(I32), name=name, off=off, words=n)


class Bump:
    def __init__(self, arena, regions):
        self.arena = arena
        self.regions = [list(r) for r in regions]

    def _take(self, words):
        for r in self.regions:
            if r[1] - r[0] >= words:
                off = r[0]
                r[0] += words
                return off
        raise MemoryError(f"bump alloc {words} words; regions {self.regions}")

    def f32(self, n, name=""):
        return self.arena.f32(self._take(n), n, name)

    def bf16(self, n, name=""):
        return self.arena.bf16(self._take((n + 1) // 2), n, name)


class WStream:
    NSLOT = 6
    KEEP = 2
    SLOT_WORDS = 1024

    def __init__(self, P, arena, off_words, wpack_ap_getter):
        self.P = P
        self.seq = []
        self.tiles = {}
        self.tot = 0
        self.pos = 0
        self.issued = 0
        self.slots = [arena.bf16(off_words + i * self.SLOT_WORDS, 2 * self.SLOT_WORDS, f"wslot{i}") for i in range(self.NSLOT)]
        self.sems = None
        self.get_wpack = wpack_ap_getter

    def reset_real(self):
        self.pos = 0
        self.issued = 0
        self.sems = [self.P.new_dsem() for _ in range(self.NSLOT)]

    def _issue(self, i):
        key, n = self.seq[i]
        off = self.tiles[key][0]
        slot = i % self.NSLOT
        st = self.slots[slot]
        src = self.get_wpack()[:, off:off + n]
        dst = st.ap[:, 0:n]
        self.P.add("pool", lambda e, dst=dst, src=src: e.dma_start(out=dst, in_=src), reads=[], writes=[st.buf],
                   dma=True, dsem=self.sems[slot])

    def next(self, key, n):
        assert n <= 2 * self.SLOT_WORDS
        if self.P.dry:
            self.seq.append((key, n))
            if key not in self.tiles:
                self.tiles[key] = (self.tot, n)
                self.tot += n
            return self.slots[0]
        assert self.seq[self.pos] == (key, n), (self.pos, self.seq[self.pos], key, n)
        hi = min(len(self.seq), self.pos + self.NSLOT - self.KEEP + 1)
        while self.issued < hi:
            self._issue(self.issued)
            self.issued += 1
        st = self.slots[self.pos % self.NSLOT]
        self.pos += 1
        return st


class Kern:
    def __init__(self, n_layers=DEPTH, taps=(), stop_after=None, mixers=("mla", "gmlp", "lru", "gla")):
        self.n_layers = n_layers
        self.taps = set(taps)
        self.stop_after = stop_after
        self.mixers = mixers
        self.nc = bass.Bass("TRN2", target_bir_lowering=False)
        self.tap_names = []
        self._build()

    def mm(self, out, lhsT, rhs, start, stop, reads, writes, skip=False):
        self.P.add("pe", lambda e: e.matmul(out, lhsT, rhs, start=start, stop=stop, skip_group_check=skip),
                   reads, writes)

    def act(self, out, in_, func, reads, writes, bias=None, scale=None, accum=None):
        kw = {}
        if bias is not None:
            kw["bias"] = bias
        if scale is not None:
            kw["scale"] = scale
        if accum is not None:
            kw["accum_out"] = accum
        self.P.add("act", lambda e: e.activation(out=out, in_=in_, func=func, **kw), reads, writes)

    def ts(self, out, in0, s1, s2, op0, op1, reads, writes, eng="dve"):
        if op1 is None:
            self.P.add(eng, lambda e: e.tensor_scalar(out=out, in0=in0, scalar1=s1, scalar2=None, op0=op0), reads, writes)
        else:
            self.P.add(eng, lambda e: e.tensor_scalar(out=out, in0=in0, scalar1=s1, scalar2=s2, op0=op0, op1=op1), reads, writes)

    def tt(self, out, in0, in1, op, reads, writes, eng="dve"):
        self.P.add(eng, lambda e: e.tensor_tensor(out=out, in0=in0, in1=in1, op=op), reads, writes)

    def stt(self, out, in0, scalar, in1, op0, op1, reads, writes):
        self.P.add("dve", lambda e: e.scalar_tensor_tensor(out=out, in0=in0, scalar=scalar, in1=in1, op0=op0, op1=op1),
                   reads, writes)

    def cp(self, out, in_, reads, writes, eng="dve"):
        self.P.add(eng, lambda e: e.tensor_copy(out=out, in_=in_), reads, writes)

    def recip(self, out, in_, reads, writes):
        self.P.add("dve", lambda e: e.reciprocal(out=out, in_=in_), reads, writes)

    def memset(self, ap, val, writes, eng="dve"):
        self.P.add(eng, lambda e: e.memset(ap, val), [], writes)

    def dma(self, out, in_, reads, writes, dsem, q="sp"):
        self.P.add(q, lambda e: e.dma_start(out=out, in_=in_), reads, writes, dma=True, dsem=dsem)

    def cst(self, col, p0=0, p1=128):
        return self.cst_t.ap[p0:p1, col:col + 1]

    def ppc(self, l, name, idx=0, p0=0, p1=128):
        c = l * NPP + PP[name] + idx
        return self.pp_t.ap[p0:p1, c:c + 1]

    def tap(self, name, tiles, width=S):
        if name not in self.taps or self.P.dry:
            return
        n = len(tiles)
        dt = self.nc.dram_tensor(name, [n * 128, width], F32, kind="ExternalOutput").ap()
        self.tap_names.append(name)
        self.P.barrier(force=True)
        stg = [self.arena.f32(self.r3_free + i * 512, 512, f"tapstg{i}") for i in range(2)]
        k = 0
        for i, t in enumerate(tiles):
            for c0 in range(0, width, 512):
                s_ = stg[k % 2]
                k += 1
                self.cp(s_.ap, t.ap[:, c0:c0 + 512], [t.buf], [s_.buf])
                ob = Buf("tapout")
                self.outbufs.append(ob)
                self.dma(dt[i * 128:(i + 1) * 128, c0:c0 + 512], s_.ap, [s_.buf], [ob], self.msem())
        self.P.barrier(force=True)

    def _build(self):
        nc = self.nc
        self.x_d = nc.dram_tensor("xT", [8, 128, S], F32, kind="ExternalInput").ap()
        self.pos_d = nc.dram_tensor("pos", [1, S], I32, kind="ExternalInput").ap()
        self.pp_d = nc.dram_tensor("pp", [128, DEPTH * NPP], F32, kind="ExternalInput").ap()
        self.cst_d = nc.dram_tensor("cst", [128, NCST], F32, kind="ExternalInput").ap()
        self.rm_d = nc.dram_tensor("rmask", [128, S], F32, kind="ExternalInput").ap()
        self.rows_d = nc.dram_tensor("rows", [DEPTH, 2, 512], F32, kind="ExternalInput").ap()
        self.wg2_d = nc.dram_tensor("wg2", [DEPTH, 16, 256], F32, kind="ExternalInput").ap()
        self.out_d = nc.dram_tensor("outT", [8, 128, S], F32, kind="ExternalOutput").ap()
        self.xsp_d = nc.dram_tensor("xspill", [8, 128, S], F32, kind="Internal").ap()
        self.wpack_d = None

        es0 = ExitStack()
        self.P = Prog(nc, es0)
        self.P.dry = True
        self._alloc(es0, dry=True)
        self._program()
        seq, tiles, tot = self.ws.seq, self.ws.tiles, self.ws.tot
        taps_dry = list(self.tap_names)
        es0.close()
        self.tile_table = tiles
        self.wtot = tot
        self.tap_names = []

        tot = max(tot, 64)
        self.wtot = tot
        self.wpack_d = nc.dram_tensor("wpack", [128, tot], F32, kind="ExternalInput").ap()
        with ExitStack() as es:
            self.P = Prog(nc, es)
            self._alloc(es, dry=False)
            self.ws.seq, self.ws.tiles, self.ws.tot = seq, tiles, tot
            self.ws.reset_real()
            self._program()
            assert self.ws.pos == len(seq), (self.ws.pos, len(seq))
            block = es.enter_context(nc.Block())
            self.P.emit(block)
        self.n_ops = len(self.P.ops)

    ARENA_WORDS = 50 * 1024
    R0 = 0
    R1 = 16 * 1024
    R2 = 24 * 1024
    R3 = 40 * 1024

    def _alloc(self, es, dry):
        nc = self.nc
        if dry and hasattr(self, "_arena_dry"):
            pass
        at = es.enter_context(nc.sbuf_tensor("arena" + ("d" if dry else ""), [128, self.ARENA_WORDS], F32))
        self.arena = Arena(at[:, :], self.ARENA_WORDS)
        A = self.arena
        ps = [es.enter_context(nc.psum_tensor(f"ps{i}" + ("d" if dry else ""), [128, 1024], F32)) for i in range(4)]
        self.PS2 = [T(p[:, :], name=f"ps2_{i}") for i, p in enumerate(ps)]
        self.PB = []
        for i, p in enumerate(ps):
            self.PB.append(T(p[:, 0:512], buf=Buf(f"pb{2 * i}", excl=True)))
            self.PB.append(T(p[:, 512:1024], buf=Buf(f"pb{2 * i + 1}", excl=True)))
        o = self.R3
        self.ws = WStream(self.P, A, o, lambda: self.wpack_d)
        o += WStream.NSLOT * WStream.SLOT_WORDS
        self.pp_t = A.f32(o, DEPTH * NPP, "pp")
        o += DEPTH * NPP
        self.cst_t = A.f32(o, NCST, "cst")
        o += NCST
        self.ident_bf = A.bf16(o, 128, "ident")
        o += 64
        self.ones_bf = A.bf16(o, 128, "ones")
        o += 64
        self.gmask_bf = A.bf16(o, 128, "gmask")
        o += 64
        self.carry = A.f32(o, 88, "carry")
        o += 88
        self.lrud = A.f32(o, 16, "lrud")
        o += 16
        self.TAP_OFF = None
        self.r3_free = o
        assert o <= self.ARENA_WORDS, o
        self.xT = [A.f32(self.R0 + m * S, S, f"xT{m}") for m in range(8)]
        self.hT = [A.bf16(self.R1 + m * (S // 2), S, f"hT{m}") for m in range(8)]
        self.acc = [A.bf16(self.R2 + m * (S // 2), S, f"acc{m}") for m in range(8)]
        self.cosT = A.bf16(48 * 1024, S, "cosT")
        self.sinT = A.bf16(49 * 1024, S, "sinT")
        self.misc_sems = [self.P.new_dsem() for _ in range(6)] if not dry else [None] * 6
        self.misc_i = 0
        self.outbufs = []

    def msem(self):
        s = self.misc_sems[self.misc_i % len(self.misc_sems)]
        self.misc_i += 1
        return s

    def _program(self):
        P = self.P
        A = self.arena
        self.dma(self.pp_t.ap, self.pp_d, [], [self.pp_t.buf], self.msem())
        self.dma(self.cst_t.ap, self.cst_d, [], [self.cst_t.buf], self.msem())
        for m in range(8):
            self.dma(self.xT[m].ap, self.x_d[m], [], [self.xT[m].buf], self.msem())
        c = self.cst_t
        self.cp(self.ident_bf.ap, c.ap[:, CS_IDENT:CS_IDENT + 128], [c.buf], [self.ident_bf.buf])
        self.cp(self.ones_bf.ap, c.ap[:, CS_ONES:CS_ONES + 128], [c.buf], [self.ones_bf.buf])
        self.cp(self.gmask_bf.ap, c.ap[:, CS_GMASK:CS_GMASK + 128], [c.buf], [self.gmask_bf.buf])
        self.carrybufs = [Buf(f"carry{i}") for i in range(44)]

        for l in range(self.n_layers):
            self.layer(l)
            if self.stop_after is not None and self.stop_after[0] == l:
                break
        P.barrier()
        for m in range(8):
            ob = Buf("outb")
            self.outbufs.append(ob)
            self.dma(self.out_d[m], self.xT[m].ap, [self.xT[m].buf], [ob], self.msem())
        P.add("sp", None, reads=list(self.outbufs), writes=[])

    def prenorm(self, l, gname, src, dst, tgs, tmp, dst_off=0):
        sq = [tmp.bf16(512, f"sq{i}") for i in range(8)]
        rt = tmp.f32(512, "rt")
        rstd = tmp.f32(512, "rstd")
        bank = self.PB[7]
        for tg in tgs:
            sl = slice(tg * TG, (tg + 1) * TG)
            dl = slice(tg * TG - dst_off, (tg + 1) * TG - dst_off)
            for m in range(8):
                s = sq[m]
                self.act(s.ap, src[m].ap[:, sl], AF.Square, [src[m].buf], [s.buf])
            for m in range(8):
                s = sq[m]
                self.mm(bank.ap, self.ones_bf.ap, s.ap, m == 0, m == 7, [self.ones_bf.buf, s.buf], [bank.buf])
            self.act(rt.ap, bank.ap, AF.Ln, [bank.buf, self.cst_t.buf], [rt.buf], bias=self.cst(CS_EPS), scale=1.0 / D)
            self.act(rstd.ap, rt.ap, AF.Exp, [rt.buf], [rstd.buf], scale=-0.5)
            for m in range(8):
                self.stt(dst[m].ap[:, dl], src[m].ap[:, sl], self.ppc(l, gname, m), rstd.ap, ALU.mult, ALU.mult,
                         [src[m].buf, rstd.buf, self.pp_t.buf], [dst[m].buf])

    def layer(self, l):
        P = self.P
        A = self.arena
        P.barrier()
        tmp = Bump(A, [[self.R2 + 8192, self.R3]])
        self.prenorm(l, "nmp", self.xT, self.hT, range(NTG), tmp)
        if l > 0:
            for m in range(8):
                self.dma(self.xsp_d[m], self.xT[m].ap, [self.xT[m].buf], [self.xspbuf(m)], self.msem())
        xsrc = self.x_d if l == 0 else self.xsp_d
        self.tap(f"hT{l}", self.hT)
        if self.stop_after == (l, "n1"):
            return
        first = True
        for mx in self.mixers:
            P.barrier()
            tmp = Bump(A, [[self.R0, self.R1], [self.R2 + 8192, self.R3]])
            o_k, free = getattr(self, "mix_" + mx)(l, tmp)
            self.tap(f"o_{mx}{l}", o_k)
            k = {"gmlp": 0, "lru": 1, "mla": 2, "gla": 3}[mx]
            P.barrier()
            self.merge(l, k, o_k, first, Bump(A, free))
            first = False
        self.tap(f"acc{l}", self.acc)
        if self.stop_after == (l, "m"):
            return
        P.barrier()
        self.phase_o(l, xsrc)
        self.tap(f"xo{l}", self.xT)
        if self.stop_after == (l, "o"):
            return
        for hh in range(2):
            P.barrier()
            self.phase_f(l, hh)
        self.tap(f"xf{l}", self.xT)

    def xspbuf(self, m):
        if not hasattr(self, "_xsp"):
            self._xsp = [Buf(f"xsp{m}") for m in range(8)]
        return self._xsp[m]

    def merge(self, l, k, o_k, first, tmp):
        sg = [tmp.f32(512, "sg0"), tmp.f32(512, "sg1")]
        tm = [tmp.f32(512, "tm0"), tmp.f32(512, "tm1")]
        it = 0
        for mp in range(4):
            gt = self.ws.next(("k", "w_in", l, None, 0, 128, 8, ((C_GATE + k * 1024 + mp * 256, 256),)), 2048)
            bt = self.ws.next(("k", "w_branch", l, k, 0, 128, 4, ((mp * 256, 256),)), 1024)
            for mi in range(2):
                m = 2 * mp + mi
                for tg in range(NTG):
                    sl = slice(tg * TG, (tg + 1) * TG)
                    gb = self.PB[(it * 2) % 6]
                    bb = self.PB[(it * 2 + 1) % 6]
                    s_ = sg[it % 2]
                    t_ = tm[it % 2]
                    it += 1
                    for kc in range(8):
                        self.mm(gb.ap, gt.ap[:, kc * 256 + mi * 128: kc * 256 + mi * 128 + 128], self.hT[kc].ap[:, sl],
                                kc == 0, kc == 7, [gt.buf, self.hT[kc].buf], [gb.buf])
                    for kc in range(4):
                        self.mm(bb.ap, bt.ap[:, kc * 256 + mi * 128: kc * 256 + mi * 128 + 128], o_k[kc].ap[:, sl],
                                kc == 0, kc == 3, [bt.buf, o_k[kc].buf], [bb.buf])
                    self.act(s_.ap, gb.ap, AF.Sigmoid, [gb.buf], [s_.buf])
                    if first:
                        self.tt(self.acc[m].ap[:, sl], s_.ap, bb.ap, ALU.mult, [s_.buf, bb.buf], [self.acc[m].buf])
                    else:
                        self.tt(t_.ap, s_.ap, bb.ap, ALU.mult, [s_.buf, bb.buf], [t_.buf])
                        self.tt(self.acc[m].ap[:, sl], self.acc[m].ap[:, sl], t_.ap, ALU.add,
                                [self.acc[m].buf, t_.buf], [self.acc[m].buf], eng="pool")

    def phase_o(self, l, xsrc):
        A = self.arena
        tmp = Bump(A, [[self.R2 + 8192, self.R3]])
        mix = self.xT
        xo = [[A.f32(self.R2 + 8192 + b * 4096 + m * 512, 512, f"xo{b}_{m}") for m in range(8)] for b in range(2)]
        tmp = Bump(A, [[self.R1, self.R2]])
        it = 0
        for mp in range(4):
            wt = self.ws.next(("k", "w_out", l, None, 0, 128, 8, ((mp * 256, 256),)), 2048)
            for mi in range(2):
                m = 2 * mp + mi
                for tg in range(NTG):
                    sl = slice(tg * TG, (tg + 1) * TG)
                    b = self.PB[it % 6]
                    it += 1
                    for kc in range(8):
                        self.mm(b.ap, wt.ap[:, kc * 256 + mi * 128: kc * 256 + mi * 128 + 128], self.acc[kc].ap[:, sl],
                                kc == 0, kc == 7, [wt.buf, self.acc[kc].buf], [b.buf])
                    self.act(mix[m].ap[:, sl], b.ap, AF.Copy, [b.buf], [mix[m].buf])
        self.postnorm(l, "nmo", mix, range(NTG), tmp, xsrc, xo)

    def postnorm(self, l, gname, f, tgs, tmp, xsrc=None, xo=None, f_off=0, xres=None):
        sq = [tmp.bf16(512, f"sq{i}") for i in range(8)]
        rt = tmp.f32(512, "rt")
        rstd = tmp.f32(512, "rstd")
        bank = self.PB[7]
        for i, tg in enumerate(tgs):
            sl = slice(tg * TG, (tg + 1) * TG)
            fl = slice(tg * TG - f_off, (tg + 1) * TG - f_off)
            if xsrc is not None:
                xb = xo[i % 2]
                for m in range(8):
                    self.dma(xb[m].ap, xsrc[m][:, sl], [self.xspbuf(m)], [xb[m].buf], self.msem())
            for m in range(8):
                s = sq[m]
                self.act(s.ap, f[m].ap[:, fl], AF.Square, [f[m].buf], [s.buf])
            for m in range(8):
                s = sq[m]
                self.mm(bank.ap, self.ones_bf.ap, s.ap, m == 0, m == 7, [self.ones_bf.buf, s.buf], [bank.buf])
            self.act(rt.ap, bank.ap, AF.Ln, [bank.buf, self.cst_t.buf], [rt.buf], bias=self.cst(CS_EPS), scale=1.0 / D)
            self.act(rstd.ap, rt.ap, AF.Exp, [rt.buf], [rstd.buf], scale=-0.5)
            for m in range(8):
                if xsrc is not None:
                    self.stt(f[m].ap[:, fl], f[m].ap[:, fl], self.ppc(l, gname, m), rstd.ap, ALU.mult, ALU.mult,
                             [f[m].buf, rstd.buf, self.pp_t.buf], [f[m].buf])
                    self.tt(f[m].ap[:, fl], f[m].ap[:, fl], xb[m].ap, ALU.add, [f[m].buf, xb[m].buf], [f[m].buf], eng="pool")
                else:
                    self.stt(f[m].ap[:, fl], f[m].ap[:, fl], self.ppc(l, gname, m), rstd.ap, ALU.mult, ALU.mult,
                             [f[m].buf, rstd.buf, self.pp_t.buf], [f[m].buf])
                    self.tt(xres[m].ap[:, sl], xres[m].ap[:, sl], f[m].ap[:, fl], ALU.add,
                            [xres[m].buf, f[m].buf], [xres[m].buf], eng="pool")

    def phase_f(self, l, hh):
        A = self.arena
        H = 1024
        t0 = hh * H
        h2 = [A.bf16(self.R1 + m * 512, H, f"h2_{m}") for m in range(8)]
        tmpn = Bump(A, [[self.R1 + 4096, self.R2]])
        self.prenorm(l, "nfp", self.xT, h2, [2 * hh, 2 * hh + 1], tmpn, dst_off=t0)
        g = [A.bf16(self.R2 + j * 512, H, f"g{j}") for j in range(22)]
        yo = self.R2 + 22 * 512
        ya = [A.f32(yo + b * 2048, H, f"ya{b}") for b in range(2)]
        yb = [A.f32(yo + b * 2048 + 1024, H, f"yb{b}") for b in range(2)]
        assert yo + 4096 <= self.R3
        for j in range(22):
            wt = self.ws.next(("k", "ffn_w_up", l, None, 0, 128, 8, ((j * 128, 128), (FFN + j * 128, 128))), 2048)
            pa = 2 * (j % 2)
            for ab in range(2):
                for tl in range(2):
                    bank = self.PB[(pa + ab) * 2 + tl]
                    sl = slice(tl * TG, (tl + 1) * TG)
                    for kc in range(8):
                        self.mm(bank.ap, wt.ap[:, kc * 256 + ab * 128: kc * 256 + ab * 128 + 128], h2[kc].ap[:, sl],
                                kc == 0, kc == 7, [wt.buf, h2[kc].buf], [bank.buf])
            ys = [ya[j % 2], yb[j % 2]]
            for ab in range(2):
                jj = j + 22 * ab
                z = self.PS2[pa + ab].ap
                zb = [self.PB[(pa + ab) * 2].buf, self.PB[(pa + ab) * 2 + 1].buf]
                y = ys[ab]
                w = lambda k: self.ppc(l, "fcw", jj * 3 + k)
                self.act(y.ap, z, AF.Identity, zb + [self.pp_t.buf], [y.buf], bias=self.ppc(l, "fcb", jj), scale=w(2))
                if hh == 0:
                    self.act(self.carry.ap[:, jj * 2: jj * 2 + 2], z[:, H - 2:H], AF.Copy, zb, [self.carrybufs[jj]])
                self.stt(y.ap[:, 1:H], z[:, 0:H - 1], w(1), y.ap[:, 1:H], ALU.mult, ALU.add, zb + [y.buf, self.pp_t.buf], [y.buf])
                self.stt(y.ap[:, 2:H], z[:, 0:H - 2], w(0), y.ap[:, 2:H], ALU.mult, ALU.add, zb + [y.buf, self.pp_t.buf], [y.buf])
                cr = self.carry.ap[:, jj * 2: jj * 2 + 2]
                if hh == 1:
                    self.stt(y.ap[:, 0:1], cr[:, 1:2], w(1), y.ap[:, 0:1], ALU.mult, ALU.add,
                             [self.carrybufs[jj], y.buf, self.pp_t.buf], [y.buf])
                    self.stt(y.ap[:, 0:2], cr[:, 0:2], w(0), y.ap[:, 0:2], ALU.mult, ALU.add,
                             [self.carrybufs[jj], y.buf, self.pp_t.buf], [y.buf])
            self.act(ys[0].ap, ys[0].ap, AF.Gelu_apprx_tanh, [ys[0].buf], [ys[0].buf])
            self.tt(g[j].ap, ys[0].ap, ys[1].ap, ALU.mult, [ys[0].buf, ys[1].buf], [g[j].buf], eng="pool")
        f = [A.f32(self.R1 + 4096 + m * 1024, H, f"f{m}") for m in range(4)] + \
            [A.f32(yo + (m - 4) * 1024, H, f"f{m}") for m in range(4, 8)]
        self.P.barrier()
        for m in range(8):
            banks = [self.PB[(m % 2) * 2], self.PB[(m % 2) * 2 + 1]]
            for half in range(2):
                wt = self.ws.next(("k", "ffn_w_down", l, None, half * 11 * 128, 128, 11, ((m * 128, 128),)), 11 * 128)
                for tl in range(2):
                    sl = slice(tl * TG, (tl + 1) * TG)
                    for jc in range(11):
                        j = half * 11 + jc
                        self.mm(banks[tl].ap, wt.ap[:, jc * 128:(jc + 1) * 128], g[j].ap[:, sl],
                                j == 0, j == 21, [wt.buf, g[j].buf], [banks[tl].buf])
            for tl in range(2):
                self.act(f[m].ap[:, tl * TG:(tl + 1) * TG], banks[tl].ap, AF.Copy, [banks[tl].buf], [f[m].buf])
        tmpn = Bump(A, [[self.R1, self.R1 + 4096]])
        self.postnorm(l, "nfo", f, [2 * hh, 2 * hh + 1], tmpn, f_off=t0, xres=self.xT)

    def rope_tables(self, cosT, sinT, tr):
        R = slice(64, 96)
        _pt = tr.f32(S, "posi")
        posi = T(_pt.ap.bitcast(I32), buf=_pt.buf)
        posf = tr.f32(S, "posf")
        ang = tr.f32(S, "ang")
        ta = tr.f32(S, "ta")
        tb = tr.f32(S, "tb")
        self.dma(posi.ap[R, :], self.pos_d[0, :].partition_broadcast(32), [], [posi.buf], self.msem())
        self.cp(posf.ap[R, :], posi.ap[R, :], [posi.buf], [posf.buf])
        cb = self.cst_t.buf
        self.ts(ang.ap[R, :], posf.ap[R, :], self.cst(CS_INVF, 64, 96), None, ALU.mult, None, [posf.buf, cb], [ang.buf])
        MAGIC = 12582912.0
        INV2PI = float(np.float32(1.0 / (2 * np.pi)))
        C1 = 6.28125
        C2 = float(np.float32(2 * np.pi - 6.28125))
        PIS = 3.1415925
        for which, dst in (("sin", sinT), ("cos", cosT)):
            if which == "sin":
                self.ts(ta.ap[R, :], ang.ap[R, :], INV2PI, None, ALU.mult, None, [ang.buf], [ta.buf])
            else:
                self.ts(ta.ap[R, :], ang.ap[R, :], INV2PI, 0.25, ALU.mult, ALU.add, [ang.buf], [ta.buf])
            self.ts(tb.ap[R, :], ta.ap[R, :], MAGIC, None, ALU.add, None, [ta.buf], [tb.buf])
            self.ts(ta.ap[R, :], tb.ap[R, :], -MAGIC, None, ALU.add, None, [tb.buf], [ta.buf])
            self.stt(tb.ap[R, :], ta.ap[R, :], -C1, ang.ap[R, :], ALU.mult, ALU.add, [ta.buf, ang.buf], [tb.buf])
            self.stt(tb.ap[R, :], ta.ap[R, :], -C2, tb.ap[R, :], ALU.mult, ALU.add, [ta.buf, tb.buf], [tb.buf])
            if which == "cos":
                self.ts(tb.ap[R, :], tb.ap[R, :], float(np.float32(np.pi / 2)), None, ALU.add, None, [tb.buf], [tb.buf])
            self.ts(tb.ap[R, :], tb.ap[R, :], PIS, -PIS, ALU.min, ALU.max, [tb.buf], [tb.buf])
            if which == "sin":
                self.act(dst.ap[R, :], tb.ap[R, :], AF.Sin, [tb.buf, cb], [dst.buf], scale=self.cst(CS_SIGN, 64, 96))
            else:
                self.act(dst.ap[R, :], tb.ap[R, :], AF.Sin, [tb.buf], [dst.buf])

    def mix_mla(self, l, tmp):
        P = self.P
        A = self.arena
        o_c = [tmp.bf16(S, f"oc{i}") for i in range(4)]
        free = [list(r) for r in tmp.regions]
        cosT, sinT = self.cosT, self.sinT
        if l == 0:
            tr = Bump(A, [list(r) for r in tmp.regions])
            self.rope_tables(cosT, sinT, tr)
        P.barrier()
        cqn = [tmp.bf16(S, f"cqn{i}") for i in range(3)]
        ckvn = [tmp.bf16(S, f"ckvn{i}") for i in range(2)]
        QT = [tmp.bf16(S, f"QT{i}") for i in range(2)]
        KT = [tmp.bf16(S, f"KT{i}") for i in range(2)]
        VA = [tmp.bf16(16 * 128, f"VA{i}") for i in range(2)]
        Pb = [tmp.bf16(512, f"Pb{i}") for i in range(4)]
        rawq = [tmp.f32(512, f"rawq{i}") for i in range(3)]
        rawk = [tmp.f32(512, f"rawk{i}") for i in range(2)]
        sq = [tmp.bf16(512, f"sq{i}") for i in range(2)]
        rt = tmp.f32(512, "rt")
        rstd = tmp.f32(512, "rstd")
        t1 = tmp.f32(512, "t1")
        t2 = tmp.f32(512, "t2")
        rec = tmp.f32(512, "rec")
        rec2 = tmp.f32(512, "rec2")
        cb = self.cst_t.buf
        ppb = self.pp_t.buf
        RR = slice(64, 96)
        VAv = [v.ap.rearrange("p (k c) -> p k c", c=128) for v in VA]
        self.memset(VAv[0][:, :, 64:128], 1.0, [VA[0].buf])
        self.memset(VAv[1][:, :, 0:64], 1.0, [VA[1].buf])

        def normgroup(tiles_cols, raws, dsts, gname, nfeat, ssq_bank, tg):
            sl = slice(tg * TG, (tg + 1) * TG)
            n = len(tiles_cols)
            for c, (wt, co, ks) in enumerate(tiles_cols):
                bank = self.PB[c % 2]
                for kc in range(8):
                    self.mm(bank.ap, wt.ap[:, kc * ks + co: kc * ks + co + 128], self.hT[kc].ap[:, sl], kc == 0, kc == 7,
                            [wt.buf, self.hT[kc].buf], [bank.buf])
                s = sq[c % 2]
                self.act(s.ap, bank.ap, AF.Square, [bank.buf], [s.buf])
                self.mm(ssq_bank.ap, self.ones_bf.ap, s.ap, c == 0, c == n - 1, [self.ones_bf.buf, s.buf], [ssq_bank.buf])
                self.cp(raws[c].ap, bank.ap, [bank.buf], [raws[c].buf])
            self.act(rt.ap, ssq_bank.ap, AF.Ln, [ssq_bank.buf, cb], [rt.buf], bias=self.cst(CS_EPS), scale=1.0 / nfeat)
            self.act(rstd.ap, rt.ap, AF.Exp, [rt.buf], [rstd.buf], scale=-0.5)
            for c in range(n):
                self.stt(dsts[c].ap[:, sl], raws[c].ap, self.ppc(l, gname, c), rstd.ap, ALU.mult, ALU.mult,
                         [raws[c].buf, rstd.buf, ppb], [dsts[c].buf])

        for tg in range(NTG):
            sl = slice(tg * TG, (tg + 1) * TG)
            tA = self.ws.next(("k", "w_in", l, None, 0, 128, 8, ((C_CQ, 256),)), 2048)
            tB = self.ws.next(("k", "w_in", l, None, 0, 128, 8, ((C_CQ + 256, 128), (C_CKV, 128))), 2048)
            normgroup([(tA, 0, 256), (tA, 128, 256), (tB, 0, 256)], rawq, cqn, "mqn", 384, self.PB[7], tg)
            bank0 = self.PB[2]
            for kc in range(8):
                self.mm(bank0.ap, tB.ap[:, kc * 256 + 128: kc * 256 + 256], self.hT[kc].ap[:, sl], kc == 0, kc == 7,
                        [tB.buf, self.hT[kc].buf], [bank0.buf])
            s = sq[0]
            self.act(s.ap, bank0.ap, AF.Square, [bank0.buf], [s.buf])
            self.mm(self.PB[6].ap, self.ones_bf.ap, s.ap, True, False, [self.ones_bf.buf, s.buf], [self.PB[6].buf])
            self.cp(rawk[0].ap, bank0.ap, [bank0.buf], [rawk[0].buf])
            tC = self.ws.next(("k", "w_in", l, None, 0, 128, 8, ((C_CKV + 128, 128), (C_KR - 64, 96))), 8 * 224)
            bank1 = self.PB[3]
            for kc in range(8):
                self.mm(bank1.ap, tC.ap[:, kc * 224: kc * 224 + 128], self.hT[kc].ap[:, sl], kc == 0, kc == 7,
                        [tC.buf, self.hT[kc].buf], [bank1.buf])
            s = sq[1]
            self.act(s.ap, bank1.ap, AF.Square, [bank1.buf], [s.buf])
            self.mm(self.PB[6].ap, self.ones_bf.ap, s.ap, False, True, [self.ones_bf.buf, s.buf], [self.PB[6].buf])
            self.cp(rawk[1].ap, bank1.ap, [bank1.buf], [rawk[1].buf])
            self.act(rt.ap, self.PB[6].ap, AF.Ln, [self.PB[6].buf, cb], [rt.buf], bias=self.cst(CS_EPS), scale=1.0 / 256)
            self.act(rstd.ap, rt.ap, AF.Exp, [rt.buf], [rstd.buf], scale=-0.5)
            for c in range(2):
                self.stt(ckvn[c].ap[:, sl], rawk[c].ap, self.ppc(l, "mkn", c), rstd.ap, ALU.mult, ALU.mult,
                         [rawk[c].buf, rstd.buf, ppb], [ckvn[c].buf])
            bA = self.PB[4]
            for kc in range(8):
                self.mm(bA.ap[0:96, :], tC.ap[:, kc * 224 + 128: kc * 224 + 224], self.hT[kc].ap[:, sl], kc == 0, kc == 7,
                        [tC.buf, self.hT[kc].buf], [bA.buf])
            tD = self.ws.next(("k", "w_in", l, None, 0, 128, 8, ((C_KR - 64, 64), (C_KR + 16, 16), (C_KR, 16))), 8 * 96)
            bB = self.PB[5]
            for kc in range(8):
                self.mm(bB.ap[0:96, :], tD.ap[:, kc * 96: kc * 96 + 96], self.hT[kc].ap[:, sl], kc == 0, kc == 7,
                        [tD.buf, self.hT[kc].buf], [bB.buf])
            self.tt(t1.ap[RR, :], bA.ap[RR, :], cosT.ap[RR, sl], ALU.mult, [bA.buf, cosT.buf], [t1.buf])
            self.tt(t2.ap[RR, :], bB.ap[RR, :], sinT.ap[RR, sl], ALU.mult, [bB.buf, sinT.buf], [t2.buf])
            self.tt(KT[0].ap[RR, sl], t1.ap[RR, :], t2.ap[RR, :], ALU.add, [t1.buf, t2.buf], [KT[0].buf])
            self.cp(KT[1].ap[RR, sl], KT[0].ap[RR, sl], [KT[0].buf], [KT[1].buf])

        SCALE = float(96 ** -0.5)
        for h in range(8):
            par = h % 2
            QTh, KTh, VAh, VAhv = QT[par], KT[par], VA[par], VAv[par]
            voff = 0 if par == 0 else 64
            tq = self.ws.next(("k", "mla_w_uq", l, None, 0, 128, 3,
                               ((h * 96, 96), (h * 96, 64), (h * 96 + 80, 16), (h * 96 + 64, 16))), 3 * 192)
            tkv = self.ws.next(("k", "mla_w_ukv", l, None, 0, 128, 2, ((h * 128, 128),)), 256)
            for tg in range(NTG):
                sl = slice(tg * TG, (tg + 1) * TG)
                bA, bB, bK = self.PB[4], self.PB[5], self.PB[7]
                for kc in range(3):
                    self.mm(bA.ap[0:96, :], tq.ap[:, kc * 192: kc * 192 + 96], cqn[kc].ap[:, sl], kc == 0, kc == 2,
                            [tq.buf, cqn[kc].buf], [bA.buf])
                for kc in range(3):
                    self.mm(bB.ap[0:96, :], tq.ap[:, kc * 192 + 96: kc * 192 + 192], cqn[kc].ap[:, sl], kc == 0, kc == 2,
                            [tq.buf, cqn[kc].buf], [bB.buf])
                for kc in range(2):
                    self.mm(bK.ap[0:64, :], tkv.ap[:, kc * 128: kc * 128 + 64], ckvn[kc].ap[:, sl], kc == 0, kc == 1,
                            [tkv.buf, ckvn[kc].buf], [bK.buf])
                self.act(QTh.ap[0:64, sl], bA.ap[0:64, :], AF.Copy, [bA.buf], [QTh.buf])
                self.tt(t1.ap[RR, :], bA.ap[RR, :], cosT.ap[RR, sl], ALU.mult, [bA.buf, cosT.buf], [t1.buf])
                self.tt(t2.ap[RR, :], bB.ap[RR, :], sinT.ap[RR, sl], ALU.mult, [bB.buf, sinT.buf], [t2.buf])
                self.tt(QTh.ap[RR, sl], t1.ap[RR, :], t2.ap[RR, :], ALU.add, [t1.buf, t2.buf], [QTh.buf])
                self.act(KTh.ap[0:64, sl], bK.ap[0:64, :], AF.Copy, [bK.buf], [KTh.buf])
            for kh in range(2):
                bV = self.PB[7]
                for k8 in range(8):
                    kt = kh * 8 + k8
                    for kc in range(2):
                        self.mm(bV.ap[:, k8 * 64:(k8 + 1) * 64], ckvn[kc].ap[:, kt * 128:(kt + 1) * 128],
                                tkv.ap[:, kc * 128 + 64: kc * 128 + 128], kc == 0, kc == 1,
                                [tkv.buf, ckvn[kc].buf], [bV.buf])
                self.act(VAhv[:, kh * 8:(kh + 1) * 8, voff:voff + 64], bV.ap.rearrange("p (k c) -> p k c", c=64), AF.Copy,
                         [bV.buf], [VAh.buf])
            m = h // 2
            pairs = [(g, j) for g in range(4) for j in range(4 * g + 4)]
            SB = [self.PB[0], self.PB[1], self.PB[6]]
            LOOK = 2

            def emitS(i):
                g, j = pairs[i]
                Sb = SB[i % 3]
                pb = Pb[i % 4]
                c0 = max(0, j - 4 * g) * 128
                self.mm(Sb.ap[:, c0:512], KTh.ap[0:96, j * 128:(j + 1) * 128], QTh.ap[0:96, g * 512 + c0:(g + 1) * 512],
                        True, True, [KTh.buf, QTh.buf], [Sb.buf])
                self.act(pb.ap[:, c0:512], Sb.ap[:, c0:512], AF.Exp, [Sb.buf], [pb.buf], scale=SCALE)
                if j >= 4 * g:
                    self.memset(pb.ap[64:128, c0:c0 + 64], 0.0, [pb.buf])

            def emitPV(i):
                g, j = pairs[i]
                pb = Pb[i % 4]
                O = self.PB[2 + g % 2]
                nj = 4 * g + 4
                c0 = max(0, j - 4 * g) * 128
                self.mm(O.ap[:, c0:512], VAhv[:, j, :], pb.ap[:, c0:512], j == 0, j == nj - 1,
                        [VAh.buf, pb.buf], [O.buf], skip=True)
                if j == nj - 1:
                    gs = slice(g * 512, (g + 1) * 512)
                    if par == 0:
                        self.act(rec.ap[64:128, :], O.ap[64:128, :], AF.Ln, [O.buf], [rec.buf])
                        self.act(rec.ap[64:128, :], rec.ap[64:128, :], AF.Exp, [rec.buf], [rec.buf], scale=-1.0)
                        self.cp(rec2.ap[0:64, :], rec.ap[64:128, :], [rec.buf], [rec2.buf])
                        self.tt(o_c[m].ap[0:64, gs], O.ap[0:64, :], rec2.ap[0:64, :], ALU.mult, [O.buf, rec2.buf], [o_c[m].buf])
                    else:
                        self.act(rec.ap[0:64, :], O.ap[0:64, :], AF.Ln, [O.buf], [rec.buf])
                        self.act(rec.ap[0:64, :], rec.ap[0:64, :], AF.Exp, [rec.buf], [rec.buf], scale=-1.0)
                        self.cp(rec2.ap[64:128, :], rec.ap[0:64, :], [rec.buf], [rec2.buf])
                        self.tt(o_c[m].ap[64:128, gs], O.ap[64:128, :], rec2.ap[64:128, :], ALU.mult, [O.buf, rec2.buf],
                                [o_c[m].buf])

            for i in range(len(pairs) + LOOK):
                if i < len(pairs):
                    emitS(i)
                if i - LOOK >= 0:
                    emitPV(i - LOOK)
        return o_c, free

    def mix_gmlp(self, l, tmp):
        o_a = [tmp.bf16(S, f"oa{i}") for i in range(4)]
        free = [list(r) for r in tmp.regions]
        grow = tmp.f32(512, "grow")
        brow = tmp.f32(512, "brow")
        self.dma(grow.ap, self.rows_d[l, 0, :].partition_broadcast(128), [], [grow.buf], self.msem())
        self.dma(brow.ap, self.rows_d[l, 1, :].partition_broadcast(128), [], [brow.buf], self.msem())
        vn = tmp.bf16(16 * 512, "vn")
        vnv = vn.ap.rearrange("p (n c) -> p n c", c=512)
        junk = tmp.bf16(512, "junk")
        ssq = tmp.f32(16, "ssq")
        rt16 = tmp.f32(16, "rt16")
        rsd = tmp.f32(16, "rsd")
        wm = tmp.bf16(512, "wm")
        ug = [tmp.f32(512, f"ug{i}") for i in range(2)]
        svb = [tmp.f32(512, f"svb{i}") for i in range(2)]
        cb = self.cst_t.buf
        tv = [self.ws.next(("k", "w_in", l, None, 0, 128, 8, ((512 + hf * 256, 256),)), 2048) for hf in range(2)]
        gvall = tmp.f32(16 * 512, "gvall")
        gvv = gvall.ap.rearrange("p (n c) -> p n c", c=512)
        for n in range(16):
            bank = self.PB[n % 2]
            for hf in range(2):
                for kc in range(8):
                    self.mm(bank.ap[:, hf * 256:(hf + 1) * 256], self.hT[kc].ap[:, n * 128:(n + 1) * 128],
                            tv[hf].ap[:, kc * 256:(kc + 1) * 256], kc == 0, kc == 7,
                            [tv[hf].buf, self.hT[kc].buf], [bank.buf])
            self.act(gvv[:, n, :], bank.ap, AF.Gelu_apprx_tanh, [bank.buf], [gvall.buf])
        for n in range(16):
            self.act(junk.ap, gvv[:, n, :], AF.Square, [gvall.buf], [junk.buf, ssq.buf], accum=ssq.ap[:, n:n + 1])
        self.act(rt16.ap, ssq.ap, AF.Ln, [ssq.buf, cb], [rt16.buf], bias=self.cst(CS_EPS), scale=1.0 / 512)
        self.act(rsd.ap, rt16.ap, AF.Exp, [rt16.buf], [rsd.buf], scale=-0.5)
        for n in range(16):
            self.stt(vnv[:, n, :], gvv[:, n, :], rsd.ap[:, n:n + 1], grow.ap, ALU.mult, ALU.mult,
                     [gvall.buf, rsd.buf, grow.buf], [vn.buf])
        tw = self.ws.next(("blkT", "gmlp_w_s", l), 512)
        self.cp(wm.ap, tw.ap[:, 0:512], [tw.buf], [wm.buf])
        self.memset(wm.ap.rearrange("p (h t) -> p h t", t=128)[64:128, :, 0:64], 0.0, [wm.buf])
        it = 0
        for hp in range(2):
            tu = self.ws.next(("k", "w_in", l, None, 0, 128, 8, ((hp * 256, 256),)), 2048)
            for hi in range(2):
                h = 2 * hp + hi
                for tg in range(NTG):
                    sl = slice(tg * TG, (tg + 1) * TG)
                    ub = self.PB[2 + it % 2]
                    sb = self.PB[4 + it % 2]
                    u_ = ug[it % 2]
                    s_ = svb[it % 2]
                    it += 1
                    for kc in range(8):
                        self.mm(ub.ap, tu.ap[:, kc * 256 + hi * 128: kc * 256 + hi * 128 + 128], self.hT[kc].ap[:, sl],
                                kc == 0, kc == 7, [tu.buf, self.hT[kc].buf], [ub.buf])
                    for i in range(4):
                        n = 4 * tg + i
                        self.mm(sb.ap[:, i * 128:(i + 1) * 128], vnv[:, n, h * 128:(h + 1) * 128], wm.ap[:, h * 128:(h + 1) * 128],
                                True, True, [vn.buf, wm.buf], [sb.buf])
                    self.act(u_.ap, ub.ap, AF.Gelu_apprx_tanh, [ub.buf], [u_.buf])
                    bb = brow.ap[:, h * 128:(h + 1) * 128].unsqueeze(1).to_broadcast([128, 4, 128])
                    self.tt(s_.ap.rearrange("p (a b) -> p a b", b=128), sb.ap.rearrange("p (a b) -> p a b", b=128), bb,
                            ALU.add, [sb.buf, brow.buf], [s_.buf])
                    self.tt(o_a[h].ap[:, sl], s_.ap, u_.ap, ALU.mult, [s_.buf, u_.buf], [o_a[h].buf])
        return o_a, free

    def mix_lru(self, l, tmp):
        ppb = self.pp_t.buf
        cb = self.cst_t.buf
        d = self.lrud
        lam = self.pp_t.ap[:, l * NPP + PP["llam"]: l * NPP + PP["llam"] + 4]
        self.act(d.ap[:, 0:4], lam, AF.Exp, [ppb], [d.buf], scale=-1.0)
        self.act(d.ap[:, 4:8], d.ap[:, 0:4], AF.Ln, [d.buf, cb], [d.buf], bias=self.cst(CS_ONE))
        self.ts(d.ap[:, 8:12], d.ap[:, 4:8], -8.0, None, ALU.mult, None, [d.buf], [d.buf])
        o_b = [tmp.bf16(S, f"ob{i}") for i in range(4)]
        free = [list(r) for r in tmp.regions]
        wa = tmp.bf16(512, "wa")
        wi = tmp.bf16(512, "wi")
        t_ = self.ws.next(("blk", "lru_w_a", l), 512)
        self.cp(wa.ap, t_.ap[:, 0:512], [t_.buf], [wa.buf])
        t_ = self.ws.next(("blk", "lru_w_i", l), 512)
        self.cp(wi.ap, t_.ap[:, 0:512], [t_.buf], [wi.buf])
        uraw = tmp.f32(S + 4, "uraw")
        up = tmp.f32(S, "up")
        upb = tmp.bf16(S, "upb")
        ra = tmp.f32(S, "ra")
        ig = tmp.f32(S, "ig")
        sq_ = tmp.f32(S, "sq_")
        hh_ = tmp.f32(S, "hh")
        yg = [tmp.f32(512, f"yg{i}") for i in range(2)]
        self.memset(uraw.ap[:, 0:3], 0.0, [uraw.buf])
        it = 0
        for c in range(4):
            tu = self.ws.next(("k", "w_in", l, None, 0, 128, 8, ((C_UB + c * 128, 128),)), 1024)
            for tg in range(NTG):
                sl = slice(tg * TG, (tg + 1) * TG)
                b = self.PB[tg % 2]
                for kc in range(8):
                    self.mm(b.ap, tu.ap[:, kc * 128:(kc + 1) * 128], self.hT[kc].ap[:, sl], kc == 0, kc == 7,
                            [tu.buf, self.hT[kc].buf], [b.buf])
                self.act(uraw.ap[:, 3 + tg * TG: 3 + (tg + 1) * TG], b.ap, AF.Copy, [b.buf], [uraw.buf])
            w = lambda k: self.ppc(l, "lcw", c * 4 + k)
            self.ts(up.ap, uraw.ap[:, 0:S], w(0), self.ppc(l, "lcb", c), ALU.mult, ALU.add, [uraw.buf, ppb], [up.buf])
            for k in range(1, 4):
                self.stt(up.ap, uraw.ap[:, k:k + S], w(k), up.ap, ALU.mult, ALU.add, [uraw.buf, up.buf, ppb], [up.buf])
            self.act(upb.ap, up.ap, AF.Copy, [up.buf], [upb.buf])
            for tg in range(NTG):
                sl = slice(tg * TG, (tg + 1) * TG)
                b = self.PB[2 + tg % 2]
                self.mm(b.ap, wa.ap[:, c * 128:(c + 1) * 128], upb.ap[:, sl], True, True, [wa.buf, upb.buf], [b.buf])
                self.act(ra.ap[:, sl], b.ap, AF.Sigmoid, [b.buf, ppb], [ra.buf], bias=self.ppc(l, "lba", c))
                b2 = self.PB[4 + tg % 2]
                self.mm(b2.ap, wi.ap[:, c * 128:(c + 1) * 128], upb.ap[:, sl], True, True, [wi.buf, upb.buf], [b2.buf])
                self.act(ig.ap[:, sl], b2.ap, AF.Sigmoid, [b2.buf, ppb], [ig.buf], bias=self.ppc(l, "lbi", c))
            self.act(ra.ap, ra.ap, AF.Exp, [ra.buf, d.buf], [ra.buf], scale=d.ap[:, 8 + c: 9 + c])
            self.stt(sq_.ap, ra.ap, -1.0, ra.ap, ALU.mult, ALU.mult, [ra.buf], [sq_.buf])
            self.act(sq_.ap, sq_.ap, AF.Sqrt, [sq_.buf, cb], [sq_.buf], bias=self.cst(CS_ONE))
            self.tt(ig.ap, ig.ap, up.ap, ALU.mult, [ig.buf, up.buf], [ig.buf])
            self.tt(ig.ap, ig.ap, sq_.ap, ALU.mult, [ig.buf, sq_.buf], [ig.buf])
            self.P.add("dve", lambda e, o=hh_.ap, a=ra.ap, x=ig.ap: e.tensor_tensor_scan(out=o, data0=a, data1=x, initial=0.0,
                                                                                         op0=ALU.mult, op1=ALU.add),
                       [ra.buf, ig.buf], [hh_.buf])
            ty = self.ws.next(("k", "w_in", l, None, 0, 128, 8, ((C_YB + c * 128, 128),)), 1024)
            for tg in range(NTG):
                sl = slice(tg * TG, (tg + 1) * TG)
                b = self.PB[6 + tg % 2]
                y_ = yg[it % 2]
                it += 1
                for kc in range(8):
                    self.mm(b.ap, ty.ap[:, kc * 128:(kc + 1) * 128], self.hT[kc].ap[:, sl], kc == 0, kc == 7,
                            [ty.buf, self.hT[kc].buf], [b.buf])
                self.act(y_.ap, b.ap, AF.Gelu_apprx_tanh, [b.buf], [y_.buf])
                self.tt(o_b[c].ap[:, sl], hh_.ap[:, sl], y_.ap, ALU.mult, [hh_.buf, y_.buf], [o_b[c].buf])
        return o_b, free

    def mix_gla(self, l, tmp):
        P = self.P
        A = self.arena
        ppb = self.pp_t.buf
        cb = self.cst_t.buf
        baseA = Bump(A, [list(r) for r in tmp.regions])
        ogl_words = 4 * S
        glr = tmp.f32(S, "glr")
        Cc = tmp.f32(S, "Cc")
        spb = tmp.f32(S, "spb")
        rmask = tmp.f32(S, "rmask")
        ogl_all = baseA.f32(4 * S, "ogl")
        ogl = [T(ogl_all.ap[:, h * S:(h + 1) * S], buf=ogl_all.buf) for h in range(4)]
        wg2 = tmp.f32(256, "wg2")
        nbg = tmp.f32(2, "nbg")
        qk_off = tmp._take(4096)
        free = [[qk_off, qk_off + 4096]]
        sub = Bump(A, [[qk_off, qk_off + 4096]])
        qin = [sub.bf16(S, f"qin{i}") for i in range(2)]
        kin = [sub.bf16(S, f"kin{i}") for i in range(2)]
        kstT = [tmp.bf16(512, f"kstT{i}") for i in range(2)]
        ksttok = tmp.bf16(16 * 256, "ksttok")
        ksv = ksttok.ap.rearrange("p (n c) -> p n c", c=256)
        vtok = tmp.bf16(16 * 512, "vtok")
        vtv = vtok.ap.rearrange("p (n c) -> p n c", c=512)
        et = [tmp.f32(512, f"et{i}") for i in range(3)]
        dec = tmp.f32(64, "dec")
        Sf = tmp.f32(256, "Sf")
        Sb_ = tmp.bf16(256, "Sb")
        attm = [tmp.bf16(512, f"attm{i}") for i in range(2)]
        sq = [tmp.bf16(512, f"gsq{i}") for i in range(2)]
        rt = tmp.f32(512, "grt")
        rstd = tmp.f32(512, "grstd")
        sg = tmp.f32(512, "gsg")
        tq_ = tmp.f32(512, "gtq")
        self.dma(wg2.ap[0:16, :], self.wg2_d[l], [], [wg2.buf], self.msem())
        self.dma(rmask.ap, self.rm_d, [], [rmask.buf], self.msem())
        gb = self.pp_t.ap[:, l * NPP + PP["gbg"]: l * NPP + PP["gbg"] + 2]
        self.ts(nbg.ap, gb, -1.0, None, ALU.mult, None, [ppb], [nbg.buf])
        tg_ = self.ws.next(("k", "w_in", l, None, 0, 128, 8, ((C_GD, 16),)), 128)
        for tg in range(NTG):
            sl = slice(tg * TG, (tg + 1) * TG)
            b = self.PB[tg % 2]
            for kc in range(8):
                self.mm(b.ap[0:16, :], tg_.ap[:, kc * 16:(kc + 1) * 16], self.hT[kc].ap[:, sl], kc == 0, kc == 7,
                        [tg_.buf, self.hT[kc].buf], [b.buf])
            self.act(glr.ap[0:16, sl], b.ap[0:16, :], AF.Copy, [b.buf], [glr.buf])
        it = 0
        for m in range(2):
            for tg in range(NTG):
                sl = slice(tg * TG, (tg + 1) * TG)
                b = self.PB[2 + tg % 2]
                self.mm(b.ap, wg2.ap[0:16, m * 128:(m + 1) * 128], glr.ap[0:16, sl], True, True, [wg2.buf, glr.buf], [b.buf])
                e_ = et[tg % 2]
                self.act(e_.ap, b.ap, AF.Exp, [b.buf, nbg.buf], [e_.buf], bias=nbg.ap[:, m:m + 1], scale=-1.0)
                self.act(spb.ap[:, sl], e_.ap, AF.Ln, [e_.buf, cb], [spb.buf], bias=self.cst(CS_ONE))
            P.add("dve", lambda e, o=Cc.ap, a=rmask.ap, x=spb.ap: e.tensor_tensor_scan(out=o, data0=a, data1=x, initial=0.0,
                                                                                      op0=ALU.mult, op1=ALU.add),
                  [rmask.buf, spb.buf], [Cc.buf])
            Cv = Cc.ap.rearrange("p (c t) -> p c t", t=64)
            self.act(dec.ap[:, m * 32:(m + 1) * 32], Cv[:, :, 63], AF.Exp, [Cc.buf], [dec.buf], scale=-1.0 / 16)
            tq = self.ws.next(("k", "w_in", l, None, 0, 128, 8, ((C_QD + m * 128, 128),)), 1024)
            for tg in range(NTG):
                sl = slice(tg * TG, (tg + 1) * TG)
                b = self.PB[4 + tg % 2]
                for kc in range(8):
                    self.mm(b.ap, tq.ap[:, kc * 128:(kc + 1) * 128], self.hT[kc].ap[:, sl], kc == 0, kc == 7,
                            [tq.buf, self.hT[kc].buf], [b.buf])
                e_ = et[it % 3]
                it += 1
                self.act(e_.ap, Cc.ap[:, sl], AF.Exp, [Cc.buf], [e_.buf], scale=-1.0 / 16)
                self.stt(qin[m].ap[:, sl], b.ap, 0.125, e_.ap, ALU.mult, ALU.mult, [b.buf, e_.buf], [qin[m].buf])
            tk = self.ws.next(("k", "w_in", l, None, 0, 128, 8, ((C_KD + m * 128, 128),)), 1024)
            for tg in range(NTG):
                sl = slice(tg * TG, (tg + 1) * TG)
                b = self.PB[6 + tg % 2]
                for kc in range(8):
                    self.mm(b.ap, tk.ap[:, kc * 128:(kc + 1) * 128], self.hT[kc].ap[:, sl], kc == 0, kc == 7,
                            [tk.buf, self.hT[kc].buf], [b.buf])
                e_ = et[it % 3]
                it += 1
                self.act(e_.ap, Cc.ap[:, sl], AF.Exp, [Cc.buf], [e_.buf], scale=1.0 / 16)
                self.tt(kin[m].ap[:, sl], b.ap, e_.ap, ALU.mult, [b.buf, e_.buf], [kin[m].buf])
                e2 = et[it % 3]
                it += 1
                Cs = Cc.ap[:, sl].rearrange("p (c t) -> p c t", t=64)
                self.tt(e2.ap.rearrange("p (c t) -> p c t", t=64), Cs[:, :, 63:64].to_broadcast([128, 8, 64]), Cs, ALU.subtract,
                        [Cc.buf], [e2.buf])
                self.act(e2.ap, e2.ap, AF.Exp, [e2.buf], [e2.buf], scale=-1.0 / 16)
                ks = kstT[tg % 2]
                self.tt(ks.ap, b.ap, e2.ap, ALU.mult, [b.buf, e2.buf], [ks.buf])
                bt = self.PB[tg % 2]
                for i in range(4):
                    self.mm(bt.ap[:, i * 128:(i + 1) * 128], ks.ap[:, i * 128:(i + 1) * 128], self.ident_bf.ap, True, True,
                            [ks.buf, self.ident_bf.buf], [bt.buf])
                self.act(ksv[:, tg * 4:(tg + 1) * 4, m * 128:(m + 1) * 128], bt.ap.rearrange("p (n c) -> p n c", c=128), AF.Copy,
                         [bt.buf], [ksttok.buf])
        tvs = [self.ws.next(("k", "w_in", l, None, 0, 128, 8, ((C_VD + hf * 256, 256),)), 2048) for hf in range(2)]
        for n in range(16):
            bank = self.PB[2 + n % 2]
            for hf in range(2):
                for kc in range(8):
                    self.mm(bank.ap[:, hf * 256:(hf + 1) * 256], self.hT[kc].ap[:, n * 128:(n + 1) * 128],
                            tvs[hf].ap[:, kc * 256:(kc + 1) * 256], kc == 0, kc == 7,
                            [tvs[hf].buf, self.hT[kc].buf], [bank.buf])
            self.act(vtv[:, n, :], bank.ap, AF.Copy, [bank.buf], [vtok.buf])
        self.tap(f"gla_qin{l}", qin)
        self.tap(f"gla_kin{l}", kin)
        P.barrier()
        self.memset(Sf.ap, 0.0, [Sf.buf])
        self.memset(Sb_.ap, 0.0, [Sb_.buf])
        Sfv = Sf.ap.rearrange("p (m v) -> p m v", v=128)
        Sbv = Sb_.ap.rearrange("p (m v) -> p m v", v=128)
        gm = self.gmask_bf.ap.unsqueeze(1).to_broadcast([128, 4, 128])
        ab = [self.PB[0], self.PB[1]]
        gm2 = self.gmask_bf.ap.unsqueeze(1).to_broadcast([128, 2, 128])
        oglv = ogl_all.ap.rearrange("p (h t) -> p h t", t=S)

        def emitA(n):
            nsl = slice(n * 128, (n + 1) * 128)
            for h in range(4):
                m, po, hs = h // 2, (h % 2) * 64, (h // 2) * 128
                self.mm(ab[h % 2].ap[:, hs:hs + 128], kin[m].ap[po:po + 64, nsl], qin[m].ap[po:po + 64, nsl], True, True,
                        [kin[m].buf, qin[m].buf], [ab[h % 2].buf])

        def emitK(c):
            n, tp = c // 2, (c % 2) * 64
            kvb = self.PB[6 + c % 2]
            for h in range(4):
                m, po = h // 2, (h % 2) * 64
                self.mm(kvb.ap[po:po + 64, m * 128:(m + 1) * 128], ksv[tp:tp + 64, n, h * 64:(h + 1) * 64],
                        vtv[tp:tp + 64, n, h * 128:(h + 1) * 128], True, True, [ksttok.buf, vtok.buf], [kvb.buf])

        emitA(0)
        emitK(0)
        emitK(1)
        for n in range(16):
            ob = [self.PB[2 + 2 * (n % 2)], self.PB[3 + 2 * (n % 2)]]
            nsl = slice(n * 128, (n + 1) * 128)
            am = attm[n % 2]
            amv = am.ap.rearrange("p (h t) -> p h t", t=128)
            for par in range(2):
                self.tt(amv[:, par::2, :], ab[par].ap[:, 0:256].rearrange("p (h t) -> p h t", t=128), gm2, ALU.mult,
                        [ab[par].buf, self.gmask_bf.buf], [am.buf])
            for h in range(4):
                hs = (h // 2) * 128
                self.mm(ob[h % 2].ap[:, hs:hs + 128], vtv[:, n, h * 128:(h + 1) * 128], am.ap[:, h * 128:(h + 1) * 128],
                        h < 2, False, [vtok.buf, am.buf], [ob[h % 2].buf], skip=True)
            if n + 1 < 16:
                emitA(n + 1)
            for cc in range(2):
                c = 2 * n + cc
                csl = slice(c * 64, (c + 1) * 64)
                for h in range(4):
                    m, po, hs = h // 2, (h % 2) * 64, (h // 2) * 128
                    self.mm(ob[h % 2].ap[:, hs + cc * 64: hs + cc * 64 + 64], Sbv[po:po + 64, m, :], qin[m].ap[po:po + 64, csl],
                            False, (cc == 1 and h >= 2), [Sb_.buf, qin[m].buf], [ob[h % 2].buf], skip=True)
                kvb = self.PB[6 + c % 2]
                for m in range(2):
                    self.stt(Sfv[:, m, :], Sfv[:, m, :], dec.ap[:, m * 32 + c: m * 32 + c + 1], kvb.ap[:, m * 128:(m + 1) * 128],
                             ALU.mult, ALU.add, [Sf.buf, dec.buf, kvb.buf], [Sf.buf])
                self.act(Sb_.ap, Sf.ap, AF.Copy, [Sf.buf], [Sb_.buf])
                if c + 2 < 32:
                    emitK(c + 2)
            for par in range(2):
                self.act(oglv[:, par::2, nsl], ob[par].ap[:, 0:256].rearrange("p (h t) -> p h t", t=128), AF.Copy,
                         [ob[par].buf], [ogl_all.buf])
        self.tap(f"gla_ogl{l}", ogl)
        P.barrier()
        o_d = [A.bf16(vtok.off + h * (S // 2), S, f"od{h}") for h in range(4)]
        it = 0
        for h in range(4):
            to = self.ws.next(("k", "w_in", l, None, 0, 128, 8, ((C_OG + h * 128, 128),)), 1024)
            for tg in range(NTG):
                sl = slice(tg * TG, (tg + 1) * TG)
                s = sq[it % 2]
                bq = self.PB[it % 2]
                bo = self.PB[2 + it % 2]
                it += 1
                self.act(s.ap, ogl[h].ap[:, sl], AF.Square, [ogl[h].buf], [s.buf])
                self.mm(bq.ap, self.ones_bf.ap, s.ap, True, True, [self.ones_bf.buf, s.buf], [bq.buf])
                for kc in range(8):
                    self.mm(bo.ap, to.ap[:, kc * 128:(kc + 1) * 128], self.hT[kc].ap[:, sl], kc == 0, kc == 7,
                            [to.buf, self.hT[kc].buf], [bo.buf])
                self.act(rt.ap, bq.ap, AF.Ln, [bq.buf, cb], [rt.buf], bias=self.cst(CS_EPS), scale=1.0 / 128)
                self.act(rstd.ap, rt.ap, AF.Exp, [rt.buf], [rstd.buf], scale=-0.5)
                self.act(sg.ap, bo.ap, AF.Silu, [bo.buf], [sg.buf])
                self.stt(tq_.ap, ogl[h].ap[:, sl], self.ppc(l, "gon", h), rstd.ap, ALU.mult, ALU.mult,
                         [ogl[h].buf, rstd.buf, ppb], [tq_.buf])
                self.tt(o_d[h].ap[:, sl], tq_.ap, sg.ap, ALU.mult, [tq_.buf, sg.buf], [o_d[h].buf])
        return o_d, free


_CACHE = {}


def _get_kern(**kw):
    key = tuple(sorted((k, str(v)) for k, v in kw.items()))
    if key not in _CACHE:
        _CACHE[key] = Kern(**kw)
    return _CACHE[key]


def _pack_shared(kern, inp):
    wpack = np.zeros((128, kern.wtot), np.float32)
    for key, (off, n) in kern.tile_table.items():
        t = _host_tile(inp, key)
        assert t.shape == (128, n), (key, t.shape, n)
        wpack[:, off:off + n] = t
    pp = np.concatenate([_host_params(inp, l) for l in range(DEPTH)], axis=1)
    rows = np.stack([np.stack([inp["gmlp_v_norm"][l].reshape(512),
                               inp["gmlp_b_s"][l].reshape(512)]) for l in range(DEPTH)]).astype(np.float32)
    return {
        "wpack": wpack,
        "pp": np.ascontiguousarray(pp, dtype=np.float32),
        "cst": _host_consts(),
        "rmask": _host_resetmask(),
        "rows": np.ascontiguousarray(rows),
        "wg2": np.ascontiguousarray(inp["gla_w_g2"], dtype=np.float32),
    }


def _core_inputs(inp, b, shared):
    xT = np.ascontiguousarray(np.asarray(inp["x"][b], dtype=np.float32).T.reshape(8, 128, S))
    d = dict(shared)
    d["xT"] = xT
    d["pos"] = np.ascontiguousarray(np.asarray(inp["positions"][b], dtype=np.int32).reshape(1, S))
    return d


def kernel(**inputs):
    inp = {k: np.asarray(v) for k, v in inputs.items()}
    kern = _get_kern()
    shared = _pack_shared(kern, inp)
    in_maps = [_core_inputs(inp, b, shared) for b in range(NCORE)]
    res = run_bass_kernel_spmd(kern.nc, in_maps, core_ids=list(range(NCORE)))
    out = np.empty((NCORE, S, D), np.float32)
    for b in range(NCORE):
        out[b] = res.results[b]["outT"].reshape(D, S).T
    return out
```
